# Optimizing a Trainium2 kernel written in Bass

```python
import jax, jax.numpy as jnp
from jax import lax
import numpy as np

D_MODEL = 1024
BATCH = 32
SEQ = 2048
DEPTH = 4

N_BRANCH = 4
WIDTH = 512
EPS = 1e-6
NEG_INF = -1e30
BIG = 1e30

A_HEADS = 4
A_HEAD_DIM = WIDTH // A_HEADS
A_CHUNK = 64

B_HEADS = 8
B_KV_GROUPS = 2
B_HEAD_DIM = WIDTH // B_HEADS
B_KV_WIDTH = B_KV_GROUPS * B_HEAD_DIM
CMP_LEN = 32
CMP_STRIDE = 16
SEL_LEN = 64
SEL_TOPN = 8
WINDOW = 256
Q_BLOCK = 128

C_BLOCKS = 8
C_BLOCK_DIM = WIDTH // C_BLOCKS
CONV_WIDTH = 4
RG_C = 8.0

D_HEADS = 4
D_QK_DIM = WIDTH // (2 * D_HEADS)
D_V_DIM = WIDTH // D_HEADS
D_CHUNK = 64

SPLIT_SIZES = (
    WIDTH, WIDTH, WIDTH, WIDTH,
    WIDTH, B_KV_WIDTH, B_KV_WIDTH, B_KV_WIDTH, B_KV_WIDTH,
    B_KV_WIDTH, B_KV_WIDTH, 3 * B_HEADS, WIDTH,
    WIDTH, WIDTH,
    D_HEADS * D_QK_DIM, D_HEADS * D_QK_DIM, D_HEADS * D_V_DIM, WIDTH,
    N_BRANCH * D_MODEL,
)
N_IN = sum(SPLIT_SIZES)

kernel_name = 'hybrid_gated_hgrn2_nsa_rglru_retention'


def _rms(x, g):
    xf = x.astype(jnp.float32)
    y = xf * lax.rsqrt(jnp.mean(xf * xf, axis=-1, keepdims=True) + EPS)
    return (y * g.astype(jnp.float32)).astype(x.dtype)


def _group_norm(x, g):
    xc = x - jnp.mean(x, axis=-1, keepdims=True)
    var = jnp.mean(xc * xc, axis=-1, keepdims=True)
    return xc * lax.rsqrt(var + EPS) * g.astype(jnp.float32)


def _to_chunks(a, n_heads, chunk):
    b, s, _ = a.shape
    return a.reshape(b, s // chunk, chunk, n_heads, -1).transpose(0, 3, 1, 2, 4)


def _from_chunks(o):
    b, h, n, c, d = o.shape
    return o.transpose(0, 2, 3, 1, 4).reshape(b, n * c, h, d)


def _chunk_states(decay, kv):
    def step(state, inp):
        d, u = inp
        return d[..., None] * state + u, state
    s0 = jnp.zeros(kv.shape[:2] + kv.shape[3:], kv.dtype)
    _, prev = lax.scan(step, s0, (jnp.moveaxis(decay, 2, 0), jnp.moveaxis(kv, 2, 0)))
    return jnp.moveaxis(prev, 0, 2)


def _lin_combine(c1, c2):
    a1, b1 = c1
    a2, b2 = c2
    return a1 * a2, a2 * b1 + b2


def hgrn2_mixer(q, f_logit, i_in, lb):
    f = lb + (1.0 - lb) * jax.nn.sigmoid(f_logit)
    k = (1.0 - lb) * jax.nn.sigmoid(-f_logit)
    qc, kc, vc, lf = (_to_chunks(a, A_HEADS, A_CHUNK) for a in (q, k, i_in, jnp.log(f)))
    b = jnp.cumsum(lf, axis=3)
    b_ref = b[:, :, :, A_CHUNK // 2 - 1:A_CHUNK // 2]
    b_end = b[:, :, :, -1:]
    causal = jnp.tril(jnp.ones((A_CHUNK, A_CHUNK), bool))
    scores = jnp.einsum('bhntd,bhnsd->bhnts', qc * jnp.exp(b - b_ref), kc * jnp.exp(b_ref - b))
    scores = jnp.where(causal, scores, 0.0)
    o = jnp.einsum('bhnts,bhnsv->bhntv', scores, vc)
    kv = jnp.einsum('bhnsd,bhnsv->bhndv', kc * jnp.exp(b_end - b), vc)
    s_prev = _chunk_states(jnp.exp(b_end[:, :, :, 0]), kv)
    o = o + jnp.einsum('bhntd,bhndv->bhntv', qc * jnp.exp(b), s_prev)
    return _from_chunks(o)


def retention_mixer(q, k, v):
    pos = jnp.arange(D_CHUNK, dtype=jnp.float32)
    log_g = jnp.log1p(-jnp.exp2(-5.0 - jnp.arange(D_HEADS, dtype=jnp.float32)))
    qc = _to_chunks(q, D_HEADS, D_CHUNK) * (D_QK_DIM ** -0.5)
    kc = _to_chunks(k, D_HEADS, D_CHUNK)
    vc = _to_chunks(v, D_HEADS, D_CHUNK)
    rel = pos[:, None] - pos[None, :]
    decay = jnp.where(rel >= 0, jnp.exp(log_g[:, None, None] * jnp.maximum(rel, 0.0)), 0.0)
    scores = jnp.einsum('bhntd,bhnsd->bhnts', qc, kc) * decay[None, :, None]
    o = jnp.einsum('bhnts,bhnsv->bhntv', scores, vc)
    k_dec = kc * jnp.exp(log_g[:, None] * (D_CHUNK - 1.0 - pos))[None, :, None, :, None]
    kv = jnp.einsum('bhnsd,bhnsv->bhndv', k_dec, vc)
    chunk_decay = jnp.broadcast_to(jnp.exp(log_g * D_CHUNK)[None, :, None, None], kv.shape[:4])
    s_prev = _chunk_states(chunk_decay, kv)
    o = o + jnp.einsum('bhntd,bhndv->bhntv', qc, s_prev) * jnp.exp(log_g[:, None] * (pos + 1.0))[None, :, None, :, None]
    return _from_chunks(o)


def rglru_mixer(xr, conv_w, conv_b, w_ra, b_ra, w_ri, b_ri, lam):
    b_, s_, w_ = xr.shape
    xc = lax.conv_general_dilated(
        xr, conv_w.astype(jnp.float32)[:, None, :], (1,), [(CONV_WIDTH - 1, 0)],
        dimension_numbers=('NWC', 'WIO', 'NWC'), feature_group_count=w_) + conv_b
    xb = xc.reshape(b_, s_, C_BLOCKS, C_BLOCK_DIM)
    r = jax.nn.sigmoid(jnp.einsum('bsnc,ncd->bsnd', xb, w_ra).reshape(b_, s_, w_) + b_ra)
    ig = jax.nn.sigmoid(jnp.einsum('bsnc,ncd->bsnd', xb, w_ri).reshape(b_, s_, w_) + b_ri)
    log_a = -RG_C * r * jax.nn.softplus(-lam)
    a = jnp.exp(log_a)
    u = jnp.sqrt(-jnp.expm1(2.0 * log_a)) * (ig * xc)
    _, h = lax.associative_scan(_lin_combine, (a, u), axis=1)
    return h


def nsa_mixer(q, kc, vc, ks, vs, kw, vw, gate_logits, q_g, k_g, cmp_pos, w_ck, w_cv):
    b_, s_ = q.shape[:2]
    H, G, hd = B_HEADS, B_KV_GROUPS, B_HEAD_DIM
    R = H // G
    scale = hd ** -0.5
    slopes = jnp.exp2(-8.0 * (jnp.arange(H, dtype=jnp.float32) + 1.0) / H).reshape(G, R)
    heads = lambda a: a.reshape(b_, s_, G, hd)
    qh = _rms(q.reshape(b_, s_, H, hd), q_g)

    n_c = (s_ - CMP_LEN) // CMP_STRIDE + 1
    cmp_idx = jnp.arange(n_c)[:, None] * CMP_STRIDE + jnp.arange(CMP_LEN)[None]
    kblk = heads(kc)[:, cmp_idx] + cmp_pos[None, None, :, None, :]
    vblk = heads(vc)[:, cmp_idx] + cmp_pos[None, None, :, None, :]
    k_cmp = _rms(jnp.einsum('bnlgd,lde->bnge', kblk, w_ck.reshape(CMP_LEN, hd, hd)), k_g)
    v_cmp = jnp.einsum('bnlgd,lde->bnge', vblk, w_cv.reshape(CMP_LEN, hd, hd))
    cmp_end = cmp_idx[:, -1]

    n_sel = s_ // SEL_LEN
    top_n = min(SEL_TOPN, n_sel)
    ks_blk = _rms(heads(ks), k_g).reshape(b_, n_sel, SEL_LEN, G, hd).transpose(0, 3, 1, 2, 4)
    vs_blk = heads(vs).reshape(b_, n_sel, SEL_LEN, G, hd).transpose(0, 3, 1, 2, 4)
    cmp_start = jnp.arange(n_c) * CMP_STRIDE
    sel_start = jnp.arange(n_sel) * SEL_LEN
    overlap = ((cmp_start[:, None] < sel_start[None] + SEL_LEN)
               & (cmp_start[:, None] + CMP_LEN > sel_start[None])).astype(jnp.float32)

    pad = ((0, 0), (WINDOW, 0), (0, 0), (0, 0))
    kw_pad = jnp.pad(_rms(heads(kw), k_g), pad)
    vw_pad = jnp.pad(heads(vw), pad)

    n_qb = s_ // Q_BLOCK
    q_blocks = qh.reshape(b_, n_qb, Q_BLOCK, G, R, hd).transpose(1, 0, 2, 3, 4, 5)
    starts = jnp.arange(n_qb) * Q_BLOCK
    bi = jnp.arange(b_)[:, None, None, None]
    gi = jnp.arange(G)[None, :, None, None]
    jsel = jnp.arange(n_sel)

    def block(args):
        qb, start = args
        t = start + jnp.arange(Q_BLOCK)
        dist_c = t[:, None] - cmp_end[None]
        m_c = dist_c >= 0
        s_c = jnp.einsum('btgrd,bngd->bgrtn', qb, k_cmp) * scale - slopes[None, :, :, None, None] * dist_c
        p_c = jax.nn.softmax(jnp.where(m_c, s_c, NEG_INF), axis=-1) * jnp.any(m_c, axis=-1)[:, None]
        o_c = jnp.einsum('bgrtn,bngd->btgrd', p_c, v_cmp)
        imp = jnp.einsum('bgrtn,nj->bgtj', p_c, overlap)
        blk_t = t // SEL_LEN
        imp = jnp.where(jsel[None] == blk_t[:, None], BIG, jnp.where(jsel[None] < blk_t[:, None], imp, -BIG))
        _, idx = lax.top_k(imp, top_n)
        k_sel = ks_blk[bi, gi, idx]
        v_sel = vs_blk[bi, gi, idx]
        dist_s = t[None, None, :, None, None] - (idx[..., None] * SEL_LEN + jnp.arange(SEL_LEN))
        s_s = jnp.einsum('btgrd,bgtnld->bgrtnl', qb, k_sel) * scale - slopes[None, :, :, None, None, None] * dist_s[:, :, None]
        s_s = jnp.where(dist_s[:, :, None] >= 0, s_s, NEG_INF)
        p_s = jax.nn.softmax(s_s.reshape(s_s.shape[:4] + (top_n * SEL_LEN,)), axis=-1).reshape(s_s.shape)
        o_s = jnp.einsum('bgrtnl,bgtnld->btgrd', p_s, v_sel)
        k_w = lax.dynamic_slice_in_dim(kw_pad, start, Q_BLOCK + WINDOW, axis=1)
        v_w = lax.dynamic_slice_in_dim(vw_pad, start, Q_BLOCK + WINDOW, axis=1)
        spos = start - WINDOW + jnp.arange(Q_BLOCK + WINDOW)
        dist_w = t[:, None] - spos[None]
        m_w = (dist_w >= 0) & (dist_w < WINDOW) & (spos[None] >= 0)
        s_w = jnp.einsum('btgrd,bsgd->bgrts', qb, k_w) * scale - slopes[None, :, :, None, None] * dist_w
        p_w = jax.nn.softmax(jnp.where(m_w, s_w, NEG_INF), axis=-1)
        o_w = jnp.einsum('bgrts,bsgd->btgrd', p_w, v_w)
        return jnp.stack([o_c, o_s, o_w], axis=-2)

    o = lax.map(block, (q_blocks, starts))
    o = o.transpose(1, 0, 2, 3, 4, 5, 6).reshape(b_, s_, H, 3, hd)
    gates = jax.nn.sigmoid(gate_logits.reshape(b_, s_, H, 3))
    return jnp.einsum('bshc,bshcd->bshd', gates, o).reshape(b_, s_, WIDTH)


def setup_inputs(seed: int = 0) -> dict:
    key = jax.random.key(seed)
    k = jax.random.split(key, 21)
    f32 = jnp.float32
    nrm = lambda kk, shape, s: jax.random.normal(kk, shape, f32) * s
    hd = B_HEAD_DIM
    a_c = jax.random.uniform(k[14], (DEPTH, WIDTH), f32, 0.9, 0.999)
    sig_lam = a_c ** (1.0 / RG_C)
    return {
        'x': nrm(k[0], (BATCH, SEQ, D_MODEL), 1.0),
        'norm_g': 1.0 + nrm(k[1], (DEPTH, D_MODEL), 0.02),
        'w_in': nrm(k[2], (DEPTH, D_MODEL, N_IN), D_MODEL ** -0.5),
        'lb_logits': nrm(k[3], (DEPTH, WIDTH), 0.1),
        'a_norm_g': 1.0 + nrm(k[4], (DEPTH, WIDTH), 0.02),
        'b_q_norm_g': 1.0 + nrm(k[5], (DEPTH, hd), 0.02),
        'b_k_norm_g': 1.0 + nrm(k[6], (DEPTH, hd), 0.02),
        'b_cmp_pos': nrm(k[7], (DEPTH, CMP_LEN, hd), 0.02),
        'b_cmp_wk': nrm(k[8], (DEPTH, CMP_LEN * hd, hd), (CMP_LEN * hd) ** -0.5),
        'b_cmp_wv': nrm(k[9], (DEPTH, CMP_LEN * hd, hd), (CMP_LEN * hd) ** -0.5),
        'c_conv_w': nrm(k[10], (DEPTH, CONV_WIDTH, WIDTH), CONV_WIDTH ** -0.5),
        'c_conv_b': nrm(k[11], (DEPTH, WIDTH), 0.01),
        'c_w_ra': nrm(k[12], (DEPTH, C_BLOCKS, C_BLOCK_DIM, C_BLOCK_DIM), C_BLOCK_DIM ** -0.5),
        'c_b_ra': nrm(k[13], (DEPTH, WIDTH), 0.01),
        'c_w_ri': nrm(k[15], (DEPTH, C_BLOCKS, C_BLOCK_DIM, C_BLOCK_DIM), C_BLOCK_DIM ** -0.5),
        'c_b_ri': nrm(k[16], (DEPTH, WIDTH), 0.01),
        'c_lambda': jnp.log(sig_lam) - jnp.log1p(-sig_lam),
        'd_norm_g': 1.0 + nrm(k[17], (DEPTH, WIDTH), 0.02),
        'merge_b': nrm(k[18], (DEPTH, N_BRANCH, D_MODEL), 0.01),
        'w_branch': nrm(k[19], (DEPTH, N_BRANCH, WIDTH, D_MODEL), WIDTH ** -0.5),
        'w_out': nrm(k[20], (DEPTH, D_MODEL, D_MODEL), D_MODEL ** -0.5),
    }


def reference(x, norm_g, w_in, lb_logits, a_norm_g, b_q_norm_g, b_k_norm_g, b_cmp_pos, b_cmp_wk, b_cmp_wv,
              c_conv_w, c_conv_b, c_w_ra, c_b_ra, c_w_ri, c_b_ri, c_lambda, d_norm_g, merge_b, w_branch, w_out):
    b_, s_, _ = x.shape
    f32 = jnp.float32
    p_lb = jax.nn.softmax(lb_logits.astype(f32), axis=0)
    lower_bounds = jnp.cumsum(p_lb, axis=0) - p_lb[0:1]
    split_points = np.cumsum(SPLIT_SIZES)[:-1]
    for l in range(DEPTH):
        xn = _rms(x, norm_g[l])
        u = (xn @ w_in[l]).astype(f32)
        (a_q, a_f, a_i, a_z, b_q, b_kc, b_vc, b_ks, b_vs, b_kw, b_vw, b_g, b_z,
         c_x, c_z, d_q, d_k, d_v, d_z, m_g) = jnp.split(u, split_points, axis=-1)

        o_a = _rms(hgrn2_mixer(a_q, a_f, a_i, lower_bounds[l]), a_norm_g[l].reshape(A_HEADS, A_HEAD_DIM))
        o_a = o_a.reshape(b_, s_, WIDTH) * jax.nn.silu(a_z)
        o_b = nsa_mixer(b_q, b_kc, b_vc, b_ks, b_vs, b_kw, b_vw, b_g, b_q_norm_g[l], b_k_norm_g[l],
                        b_cmp_pos[l], b_cmp_wk[l], b_cmp_wv[l]) * jax.nn.silu(b_z)
        o_c = rglru_mixer(c_x, c_conv_w[l], c_conv_b[l], c_w_ra[l], c_b_ra[l], c_w_ri[l], c_b_ri[l],
                          c_lambda[l]) * jax.nn.silu(c_z)
        o_d = _group_norm(retention_mixer(d_q, d_k, d_v), d_norm_g[l].reshape(D_HEADS, D_V_DIM))
        o_d = o_d.reshape(b_, s_, WIDTH) * jax.nn.silu(d_z)

        merged = 0.0
        for br, o_k in enumerate((o_a, o_b, o_c, o_d)):
            gate = jax.nn.sigmoid(m_g[..., br * D_MODEL:(br + 1) * D_MODEL] + merge_b[l, br])
            merged = merged + gate * (o_k.astype(x.dtype) @ w_branch[l, br])
        x = x + merged.astype(x.dtype) @ w_out[l]
    return x
```

```python
import numpy as np
import concourse.bass as bass
import concourse.mybir as mybir
from concourse.bass_utils import run_bass_kernel_spmd
from contextlib import ExitStack

F32 = mybir.dt.float32
BF16 = mybir.dt.bfloat16
AF = mybir.ActivationFunctionType
ALU = mybir.AluOpType
AX = mybir.AxisListType

D = 1024
T = 2048
NIN = 10520
W = 512
EPS = 1e-6
NEG = -30000.0
ENGS = ("pe", "act", "dve", "pool", "sp")
SEM_LIM = 30000

C_AQ, C_AF, C_AI, C_AZ = 0, 512, 1024, 1536
C_BQ, C_BKC, C_BVC, C_BKS, C_BVS, C_BKW, C_BVW, C_BG, C_BZ = 2048, 2560, 2688, 2816, 2944, 3072, 3200, 3328, 3352
C_CX, C_CZ = 3864, 4376
C_DQ, C_DK, C_DV, C_DZ = 4888, 5144, 5400, 5912
C_MG = 6424


class Prog:
    def __init__(self, nc, n_dma_sems=8):
        self.nc = nc
        self.es = ExitStack()
        self.ops = {e: [] for e in ENGS}
        self.nsem = 0
        self.csem = {e: self._newsem() for e in ENGS}
        self.ccount = {e: 0 for e in ENGS}
        self.nd = n_dma_sems
        self.dsem = {q: [self._newsem() for _ in range(n_dma_sems)] for q in ("sp", "pool", "act")}
        self.dcnt = {q: [0] * n_dma_sems for q in ("sp", "pool", "act")}
        self.dnext = {q: 0 for q in ("sp", "pool", "act")}
        self.last_w = {}
        self.readers = {}
        self.waited = {e: {} for e in ENGS}
        self.skip = False
        self.prev_tok = {}

    def _newsem(self):
        self.nsem += 1
        return self.es.enter_context(self.nc.semaphore("s%d" % self.nsem))

    def sb(self, name, shape, dt, es=None):
        self.nalloc = getattr(self, "nalloc", 0) + 1
        return (es or self.es).enter_context(self.nc.sbuf_tensor("%s_%d" % (name, self.nalloc), list(shape), dt))

    def ps(self, name, shape, dt):
        return self.es.enter_context(self.nc.psum_tensor(name, list(shape), dt))

    def tokens(self, names):
        out = []
        for n in names:
            t = self.last_w.get(n)
            if t is not None:
                out.append(t)
            out.extend(self.readers.get(n, ()))
        return out

    def _deps(self, eng, reads, writes, extra):
        need = {}

        def add(tok):
            s, v = tok
            k = id(s)
            if k not in need or need[k][1] < v:
                need[k] = (s, v)
        for r in reads:
            t = self.last_w.get(r)
            if t is not None:
                add(t)
        for w in writes:
            t = self.last_w.get(w)
            if t is not None:
                add(t)
            for t in self.readers.get(w, ()):
                add(t)
        for t in extra:
            add(t)
        out = []
        wd = self.waited[eng]
        for k, (s, v) in need.items():
            if wd.get(k, 0) >= v:
                continue
            wd[k] = v
            out.append((s, v))
        return out

    def _commit(self, tok, reads, writes):
        for r in reads:
            lst = self.readers.setdefault(r, [])
            lst[:] = [t for t in lst if t[0] is not tok[0]]
            lst.append(tok)
        for w in writes:
            self.last_w[w] = tok
            self.readers[w] = []

    def op(self, eng, fn, reads=(), writes=(), chain=False, extra=()):
        if self.skip:
            return None
        reads = tuple(reads)
        writes = tuple(writes) + tuple(r for r in reads if r.startswith("psb"))
        waits = self._deps(eng, reads, () if chain else writes, extra)
        if self.ccount[eng] >= SEM_LIM:
            self.prev_tok[eng] = (self.csem[eng], self.ccount[eng])
            self.csem[eng] = self._newsem()
            self.ccount[eng] = 0
        self.ccount[eng] += 1
        tok = (self.csem[eng], self.ccount[eng])
        self.ops[eng].append((fn, waits, (self.csem[eng], 1)))
        self._commit(tok, reads, writes)
        return tok

    def dma(self, q, fn, reads=(), writes=(), extra=()):
        if self.skip:
            return None
        reads = tuple(reads)
        writes = tuple(writes)
        i = self.dnext[q]
        self.dnext[q] = (i + 1) % self.nd
        if self.dcnt[q][i] >= SEM_LIM // 16:
            self.dsem[q][i] = self._newsem()
            self.dcnt[q][i] = 0
        s = self.dsem[q][i]
        ex = list(extra)
        if self.dcnt[q][i] > 0:
            ex.append((s, 16 * self.dcnt[q][i]))
        waits = self._deps(q, reads, writes, ex)
        self.dcnt[q][i] += 1
        tok = (s, 16 * self.dcnt[q][i])
        self.ops[q].append((fn, waits, (s, 16)))
        self._commit(tok, reads, writes)
        return tok

    def all_tokens(self):
        toks = [(self.csem[e], self.ccount[e]) if self.ccount[e] > 0 else self.prev_tok[e]
                for e in ENGS if self.ccount[e] > 0 or e in self.prev_tok]
        for q in self.dsem:
            for i in range(self.nd):
                if self.dcnt[q][i] > 0:
                    toks.append((self.dsem[q][i], 16 * self.dcnt[q][i]))
        return toks

    def barrier(self):
        toks = self.all_tokens()
        for e in ENGS:
            waits = self._deps(e, (), (), toks)
            if waits:
                self.ops[e].append((None, waits, None))

    def emit(self):
        nc = self.nc
        engmap = {"pe": "tensor", "act": "scalar", "dve": "vector", "pool": "gpsimd", "sp": "sync"}
        with nc.Block() as block:
            for e in ENGS:
                lst = self.ops[e]
                if not lst:
                    continue

                def body(eobj, lst=lst):
                    for fn, waits, inc in lst:
                        for (s, v) in waits:
                            eobj.wait_ge(s, v)
                        if fn is not None:
                            fn(eobj).then_inc(inc[0], inc[1])
                getattr(block, engmap[e])(body)
        self.es.close()


class Ring:
    def __init__(self, p, name, n, shape, dt, es=None):
        self.t = [p.sb("%s%d" % (name, i), shape, dt, es) for i in range(n)]
        self.names = ["%s%d" % (name, i) for i in range(n)]
        self.i = 0

    def next(self):
        i = self.i
        self.i = (i + 1) % len(self.t)
        return self.t[i], self.names[i]


def _consts():
    c = {}
    c["ident"] = np.eye(128, dtype=np.float32)
    s = np.arange(128)[:, None]
    t = np.arange(128)[None, :]
    caus = np.where(s > t, NEG, 0.0).astype(np.float32)
    winn = np.where(s <= t, NEG, 0.0).astype(np.float32)
    c["causneg"] = np.tile(caus, (1, 4))
    c["winneg"] = np.tile(winn, (1, 4))
    la = np.zeros((2, 16, 128), np.float32)
    la[0] = (np.arange(128) - 127)[None, :]
    la[1] = (-128.0 * np.arange(16))[:, None]
    c["LAl"] = la
    rs = np.zeros((2, 2, 512), np.float32)
    for g in range(2):
        for r in range(4):
            rs[:, g, r * 128:(r + 1) * 128] = 2.0 ** (-(g * 4 + r + 1))
    rsm = np.zeros((10, 2, 512), np.float32)
    rsm[0:2] = rs
    lc = np.zeros((2, 16, 128), np.float32)
    lc[0] = (16.0 * np.arange(128) - 96.0)[None, :]
    lc[1] = (-128.0 * np.arange(16))[:, None]
    lcm = np.zeros((10, 16, 128), np.float32)
    lcm[0:2] = lc
    e = np.zeros((32, 16, 128), np.float32)
    for kt in range(16):
        for si in range(128):
            e[(kt * 128 + si) // 64, kt, si] = 1.0
    c["E"] = e
    lm = np.zeros((8, 16, 128), np.float32)
    for qb in range(16):
        for n in range(127):
            k = n - 8 * qb + 1
            if 0 <= k < 8:
                lm[k, qb, n] = 1.0
    lcm[2:10] = lm
    c["Lcm"] = lcm
    rm = np.zeros((8, 512), np.float32)
    for k in range(8):
        for tt in range(128):
            if 16 * (k - 1) + 31 > tt:
                rm[k, tt::128] = NEG
    rsm[2:10, 0, :] = rm
    rsm[2:10, 1, :] = rm
    c["Rsm"] = rsm
    n = np.arange(128)[:, None]
    j = np.arange(32)[None, :]
    ovl = ((16 * n < 64 * j + 64) & (16 * n + 32 > 64 * j)).astype(np.float32)
    ovl[127] = 0.0
    c["ovl"] = np.concatenate([ovl, np.ones((128, 1), np.float32)], 1)
    mul = np.zeros((128, 16, 32), np.float32)
    add = np.zeros((128, 16, 32), np.float32)
    for qb in range(16):
        for tt in range(128):
            bt = (qb * 128 + tt) // 64
            mul[tt, qb, :bt] = 1.0
            add[tt, qb, bt] = 1e30
            add[tt, qb, bt + 1:] = -1e30
    c["mulm"] = mul
    c["addm"] = add
    gam = [1.0 - 2.0 ** (-5.0 - h) for h in range(4)]
    pos = np.arange(128)
    dm = np.zeros((128, 4, 128), np.float32)
    for h in range(4):
        rel = pos[None, :] - pos[:, None]
        dm[:, h] = np.where(rel >= 0, 0.125 * gam[h] ** np.maximum(rel, 0), 0.0)
    c["dmask"] = dm
    qd = np.zeros((128, 2, 128), np.float32)
    for j2 in range(2):
        for hp in range(2):
            h = 2 * j2 + hp
            qd[64 * hp:64 * hp + 64, j2, :] = (0.125 * gam[h] ** (pos + 1.0))[None, :]
    c["qdec"] = qd
    kd = np.zeros((128, 4), np.float32)
    for h in range(4):
        kd[:, h] = gam[h] ** (127.0 - np.arange(128))
    c["kdec"] = kd
    cm = np.ones((128, 512), np.float32)
    cm[:, ::64] = 0.0
    c["cmask"] = cm
    p128 = np.arange(128)
    c["m01"] = ((p128[:, None] // 64 == p128[None, :] // 64) & (p128[:, None] <= p128[None, :])).astype(np.float32)
    hm = np.zeros((128, 2), np.float32)
    hm[:64, 0] = 1.0
    hm[64:, 1] = 1.0
    c["hmask"] = hm
    c["onesf"] = np.ones((128, 128), np.float32)
    bd = np.zeros((128, 128), np.float32)
    bd[:64, :64] = 1.0
    bd[64:, 64:] = 1.0
    c["onesbd"] = bd
    return c


CONST_SPEC = [
    ("ident", [128, 128], BF16), ("causneg", [128, 512], BF16), ("winneg", [128, 512], BF16),
    ("LAl", [2, 16, 128], BF16), ("Lcm", [10, 16, 128], BF16), ("E", [32, 16, 128], BF16), ("Rsm", [10, 2, 512], BF16),
    ("ovl", [128, 33], BF16), ("mulm", [128, 16, 32], BF16), ("addm", [128, 16, 32], BF16),
    ("dmask", [128, 4, 128], F32), ("qdec", [128, 2, 128], F32), ("kdec", [128, 4], F32),
    ("cmask", [128, 512], F32), ("m01", [128, 128], F32), ("hmask", [128, 2], F32), ("onesf", [128, 128], F32),
    ("onesbd", [128, 128], F32),
]

P_NG, P_AG, P_CW, P_CB, P_BRA, P_BRI, P_LAM, P_DG, P_MB, P_QG, P_KG, P_POS = 0, 8, 12, 28, 32, 36, 40, 44, 48, 80, 81, 82
NPAR = 114


def _params(inp, l):
    P = np.zeros((128, NPAR), np.float32)
    P[:, P_NG:P_NG + 8] = inp["norm_g"][l].reshape(8, 128).T
    P[:, P_AG:P_AG + 4] = inp["a_norm_g"][l].reshape(4, 128).T
    P[:, P_CW:P_CW + 16] = inp["c_conv_w"][l].reshape(4, 4, 128).transpose(2, 1, 0).reshape(128, 16)
    P[:, P_CB:P_CB + 4] = inp["c_conv_b"][l].reshape(4, 128).T
    P[:, P_BRA:P_BRA + 4] = inp["c_b_ra"][l].reshape(4, 128).T
    P[:, P_BRI:P_BRI + 4] = inp["c_b_ri"][l].reshape(4, 128).T
    P[:, P_LAM:P_LAM + 4] = inp["c_lambda"][l].reshape(4, 128).T
    P[:, P_DG:P_DG + 4] = inp["d_norm_g"][l].reshape(4, 128).T
    P[:, P_MB:P_MB + 32] = inp["merge_b"][l].reshape(4, 8, 128).transpose(2, 0, 1).reshape(128, 32)
    P[:, P_QG] = np.tile(inp["b_q_norm_g"][l], 2)
    P[:, P_KG] = np.tile(inp["b_k_norm_g"][l], 2)
    P[:, P_POS:P_POS + 32] = np.tile(inp["b_cmp_pos"][l].T, (2, 1))
    return P


def build_program(NL, NS, dbg=False):
    nc = bass.Bass("TRN2", target_bir_lowering=False)
    dr = {}

    def dten(name, shape, kind="ExternalInput"):
        dr[name] = nc.dram_tensor(name, list(shape), F32, kind=kind).ap()
        return dr[name]
    xT = dten("xT", [NS, D, T])
    outT = dten("outT", [NS, D, T], kind="ExternalOutput")
    w_in = dten("w_in", [NL, D, NIN])
    w_br = dten("w_branch", [NL, 4, W, D])
    w_out = dten("w_out", [NL, D, D])
    wck = dten("b_cmp_wk", [NL, 2048, 64])
    wcv = dten("b_cmp_wv", [NL, 2048, 64])
    wra = dten("c_w_ra", [NL, 8, 64, 64])
    wri = dten("c_w_ri", [NL, 8, 64, 64])
    par = dten("par", [NL, 128, NPAR])
    lbl = dten("lbl", [128, 16])
    lsel = dten("lsel", [128, NL * 4])
    cdr = {}
    for name, shape, _ in CONST_SPEC:
        cdr[name] = dten("c_" + name, shape)
    if dbg:
        dbgT = dten("dbgT", [NS, NL, 4, W, T], kind="ExternalOutput")
        dbg2 = dten("dbg2", [64, 128, 512], kind="ExternalOutput")

    p = Prog(nc)
    PS = [p.ps("psb%d" % i, [128, 512], F32) for i in range(8)]
    PSN = ["psb%d" % i for i in range(8)]

    cst = {}
    for name, shape, dt in CONST_SPEC:
        cst[name] = p.sb("k_" + name, shape, dt)
        q = "pool" if dt == BF16 else "sp"
        p.dma(q, lambda e, a=cst[name], b=cdr[name]: e.dma_start(out=a[:], in_=b), writes=["k_" + name])
    CN = lambda n: "k_" + n
    zer = p.sb("zer", [128, 512], BF16)
    fsc = p.sb("fsc", [128, 2], F32)
    p.op("pool", lambda e: e.memset(zer[:], 0.0), writes=["zer"])
    parT = p.sb("parT", [128, NL, NPAR], F32)
    for l in range(NL):
        p.dma("sp", lambda e, l=l: e.dma_start(out=parT[:, l, :], in_=par[l]), writes=["par"])
    xn = p.sb("xn", [128, 8, T], BF16)
    oT = p.sb("oT", [128, 4, 4, T], BF16)
    WB = Ring(p, "wb", 2, [128, 8, 512], BF16)

    drv = p.sb("drv", [128, NL, 24], F32)
    lbt = p.sb("lbt", [128, 16], F32)
    lst = p.sb("lst", [128, NL * 4], F32)
    lbe = p.sb("lbe", [128, 4, 4], F32)
    lbs = p.sb("lbs", [128, 4], F32)
    lbc = p.sb("lbc", [128, 4, 4], F32)
    tmp4 = p.sb("tmp4", [128, 4, 4], F32)
    p.dma("sp", lambda e: e.dma_start(out=lbt[:], in_=lbl), writes=["lbt"])
    p.dma("sp", lambda e: e.dma_start(out=lst[:], in_=lsel), writes=["lst"])
    lb3 = lbt[:].rearrange("p (h l) -> p h l", l=4)
    p.op("act", lambda e: e.activation(out=lbe[:], in_=lb3, func=AF.Exp), reads=["lbt"], writes=["lbe"])
    p.op("dve", lambda e: e.tensor_reduce(out=lbs[:], in_=lbe[:], axis=AX.X, op=ALU.add), reads=["lbe"], writes=["lbs"])
    p.op("dve", lambda e: e.reciprocal(out=lbs[:], in_=lbs[:]), reads=["lbs"], writes=["lbs"])
    p.op("dve", lambda e: e.tensor_tensor(out=lbe[:], in0=lbe[:], in1=lbs[:].unsqueeze(2).to_broadcast([128, 4, 4]),
                                          op=ALU.mult), reads=["lbe", "lbs"], writes=["lbe"])
    p.op("dve", lambda e: e.memset(lbc[:], 0.0), writes=["lbc"])
    for l in range(1, 4):
        p.op("dve", lambda e, l=l: e.tensor_tensor(out=lbc[:, :, l:l + 1], in0=lbc[:, :, l - 1:l], in1=lbe[:, :, l:l + 1],
                                                   op=ALU.add), reads=["lbc", "lbe"], writes=["lbc"])
    for l in range(NL):
        sel = lst[:, l * 4:(l + 1) * 4]
        p.op("dve", lambda e, sel=sel: e.tensor_tensor(out=tmp4[:], in0=lbc[:], in1=sel.unsqueeze(1).to_broadcast([128, 4, 4]),
                                                       op=ALU.mult), reads=["lbc", "lst"], writes=["tmp4"])
        p.op("dve", lambda e, l=l: e.tensor_reduce(out=drv[:, l, 0:4], in_=tmp4[:], axis=AX.X, op=ALU.add),
             reads=["tmp4"], writes=["drv"])
        p.op("dve", lambda e, l=l: e.tensor_scalar(out=drv[:, l, 4:8], in0=drv[:, l, 0:4], scalar1=-1.0, scalar2=1.0,
                                                   op0=ALU.mult, op1=ALU.add), reads=["drv"], writes=["drv"])
        p.op("act", lambda e, l=l: e.activation(out=drv[:, l, 8:12], in_=parT[:, l, P_LAM:P_LAM + 4], func=AF.Exp, scale=-1.0),
             reads=["par"], writes=["drv"])
        p.op("act", lambda e, l=l: e.activation(out=drv[:, l, 8:12], in_=drv[:, l, 8:12], func=AF.Ln, bias=1.0),
             reads=["drv"], writes=["drv"])
        p.op("dve", lambda e, l=l: e.tensor_scalar(out=drv[:, l, 12:16], in0=drv[:, l, 8:12], scalar1=-16.0, scalar2=None,
                                                   op0=ALU.mult), reads=["drv"], writes=["drv"])
        p.op("dve", lambda e, l=l: e.tensor_scalar(out=drv[:, l, 8:12], in0=drv[:, l, 8:12], scalar1=-8.0, scalar2=None,
                                                   op0=ALU.mult), reads=["drv"], writes=["drv"])
        p.op("dve", lambda e, l=l: e.tensor_scalar(out=drv[:, l, 16:17], in0=parT[:, l, P_QG:P_QG + 1], scalar1=0.125,
                                                   scalar2=None, op0=ALU.mult), reads=["par"], writes=["drv"])

    def DUMP(idx, ap, n, res):
        if dbg:
            DMA("sp", dbg2[idx, :, 0:n], ap, reads=[res], writes=["dbg2_%d" % idx])

    def wload(l, segs):
        wt, wn = WB.next()
        names = ["%ss%d" % (wn, k) for k in range(4)]
        pre = p.tokens(names)
        src = w_in[l].rearrange("(kc q) n -> q kc n", q=128)
        off = 0
        offs = []
        for k, (c0, n) in enumerate(segs):
            DMA("pool", wt[:, :, off:off + n], src[:, :, c0:c0 + n], writes=[names[k]], extra=pre)
            offs.append(off)
            off += n
        return wt, names[:len(segs)], offs

    def proj_fm(ps, psn, wt, wres, off, M, t0, n):
        for kc in range(8):
            p.op("pe", lambda e, kc=kc: e.matmul(ps[0:M, 0:n], lhsT=wt[:, kc, off:off + M], rhs=xn[:, kc, t0:t0 + n],
                                                 start=(kc == 0), stop=(kc == 7)),
                 reads=list(wres) + ["xn"], writes=[psn], chain=(kc > 0))

    def proj_tm(ps, psn, wt, wres, off, N, t0):
        for kc in range(8):
            p.op("pe", lambda e, kc=kc: e.matmul(ps[:, 0:N], lhsT=xn[:, kc, t0:t0 + 128], rhs=wt[:, kc, off:off + N],
                                                 start=(kc == 0), stop=(kc == 7)),
                 reads=list(wres) + ["xn"], writes=[psn], chain=(kc > 0))

    def OP(eng, meth, reads, writes, chain=False, extra=(), **kw):
        return p.op(eng, lambda e: getattr(e, meth)(**kw), reads=reads, writes=writes, chain=chain, extra=extra)

    def DMA(q, out, in_, reads=(), writes=(), extra=()):
        return p.dma(q, lambda e: e.dma_start(out=out, in_=in_), reads=reads, writes=writes, extra=extra)

    def ACT(out, in_, func, reads, writes, **kw):
        return p.op("act", lambda e: e.activation(out=out, in_=in_, func=func, **kw), reads=reads, writes=writes)

    def TT(out, in0, in1, op, reads, writes, eng="dve"):
        eng = POOLMAP if eng == "pool" else eng
        return p.op(eng, lambda e: e.tensor_tensor(out=out, in0=in0, in1=in1, op=op), reads=reads, writes=writes)

    def TS(out, in0, s1, s2, op0, op1, reads, writes, eng="dve"):
        eng = POOLMAP if eng == "pool" else eng
        if s2 is None:
            return p.op(eng, lambda e: e.tensor_scalar(out=out, in0=in0, scalar1=s1, scalar2=None, op0=op0),
                        reads=reads, writes=writes)
        return p.op(eng, lambda e: e.tensor_scalar(out=out, in0=in0, scalar1=s1, scalar2=s2, op0=op0, op1=op1),
                    reads=reads, writes=writes)

    def STT(out, in0, sc, in1, op0, op1, reads, writes):
        return p.op("dve", lambda e: e.scalar_tensor_tensor(out=out, in0=in0, scalar=sc, in1=in1, op0=op0, op1=op1),
                    reads=reads, writes=writes)

    def MM(out, lhsT, rhs, start, stop, reads, writes, chain=False, **kw):
        return p.op("pe", lambda e: e.matmul(out, lhsT=lhsT, rhs=rhs, start=start, stop=stop, **kw),
                    reads=reads, writes=writes, chain=chain)

    def RECIP(out, in_, reads, writes):
        return p.op("dve", lambda e: e.reciprocal(out=out, in_=in_), reads=reads, writes=writes)

    def COPY(eng, out, in_, reads, writes):
        if eng == "act":
            return ACT(out, in_, AF.Copy, reads, writes)
        return p.op(eng, lambda e: e.tensor_copy(out=out, in_=in_), reads=reads, writes=writes)


    for s in range(NS):
        RES = "res%d" % s
        for l in range(NL):
            src_x = xT if l == 0 else outT
            pl = lambda c0, n=1, l=l: parT[:, l, c0:c0 + n]

            p.skip = "0" not in PHASES
            p.barrier()
            with ExitStack() as es:
                XS = Ring(p, "xs", 2, [128, 8, 256], F32, es)
                SQ = Ring(p, "sq0", 2, [128, 8, 256], F32, es)
                RS = Ring(p, "rs0", 2, [128, 256], F32, es)
                for tb in range(8):
                    t0 = tb * 256
                    xs, xsn = XS.next()
                    sq, sqn = SQ.next()
                    rs, rsn = RS.next()
                    DMA("sp", xs[:], src_x[s].rearrange("(c q) t -> q c t", q=128)[:, :, t0:t0 + 256],
                        reads=[RES + "_%d_%d" % (c_, t0 // 512) for c_ in range(8)], writes=[xsn])
                    ACT(sq[:], xs[:], AF.Square, [xsn], [sqn])
                    ps, psn = PS[tb % 2], PSN[tb % 2]
                    for kc in range(8):
                        MM(ps[:, 0:256], cst["onesf"][:], sq[:, kc, :], kc == 0, kc == 7, [CN("onesf"), sqn], [psn], chain=kc > 0)
                    ACT(rs[:], ps[:, 0:256], AF.Sqrt, [psn], [rsn], scale=1.0 / D, bias=EPS)
                    RECIP(rs[:], rs[:], [rsn], [rsn])
                    TT(xs[:], xs[:], rs[:].unsqueeze(1).to_broadcast([128, 8, 256]), ALU.mult, [xsn, rsn], [xsn])
                    TT(xn[:, :, t0:t0 + 256], xs[:], pl(P_NG, 8).unsqueeze(2).to_broadcast([128, 8, 256]), ALU.mult,
                       [xsn, "par"], ["xn"], eng="pool")

            p.skip = "A" not in PHASES
            p.barrier()
            with ExitStack() as es:
                vtm = p.sb("a_vtm", [128, 16, 512], BF16, es)
                F_ = [p.sb("a_f%d" % i, [128, 512], F32, es) for i in range(6)]
                FN = ["a_f%d" % i for i in range(6)]
                qp = Ring(p, "a_qp", 2, [128, 512], BF16, es)
                kp = Ring(p, "a_kp", 2, [128, 512], BF16, es)
                ktm = Ring(p, "a_ktm", 2, [128, 4, 2, 128], BF16, es)
                scs = Ring(p, "a_sc", 2, [128, 4, 128], BF16, es)
                esc = Ring(p, "a_es", 2, [128, 3, 8], F32, es)
                S = p.sb("a_S", [128, 128], F32, es)
                Sp = Ring(p, "a_Sp", 2, [128, 128], BF16, es)
                tkv = p.sb("a_tkv", [128, 128], F32, es)
                E_ = [p.sb("a_e%d" % i, [128, 512], F32, es) for i in range(4)]
                EN = ["a_e%d" % i for i in range(4)]
                wt, wr, wo = wload(l, [(C_AI, 512)])
                for blk in range(16):
                    ps, psn = PS[blk % 2], PSN[blk % 2]
                    proj_tm(ps, psn, wt, wr, 0, 512, blk * 128)
                    COPY("act" if blk % 2 else "dve", vtm[:, blk, :], ps[:, 0:512], [psn], ["a_vtm"])
                p.skip = p.skip or ("a" in PHASES)
                for h in range(4):
                    wt, wr, wo = wload(l, [(C_AQ + 128 * h, 128), (C_AF + 128 * h, 128), (C_AZ + 128 * h, 128)])
                    lbh = drv[:, l, h:h + 1]
                    omh = drv[:, l, 4 + h:5 + h]
                    OP("dve", "memset", [], ["a_S"], ap=S[:], constant=0.0)
                    for tl in range(4):
                        t0 = tl * 512
                        proj_fm(PS[0], PSN[0], wt, wr, wo[1], 128, t0, 512)
                        ACT(F_[0][:], PS[0][:], AF.Sigmoid, [PSN[0]], [FN[0]])
                        TS(F_[0][:], F_[0][:], omh, lbh, ALU.mult, ALU.add, [FN[0], "drv"], [FN[0]])
                        ACT(F_[1][:], F_[0][:], AF.Ln, [FN[0]], [FN[1]])
                        TS(F_[0][:], F_[0][:], -1.0, 1.0, ALU.mult, ALU.add, [FN[0]], [FN[0]], eng="pool")
                        OP("dve", "tensor_tensor_scan", [CN("cmask"), FN[1]], [FN[2]], out=F_[2][:], data0=cst["cmask"][:],
                           data1=F_[1][:], initial=0.0, op0=ALU.mult, op1=ALU.add)
                        b3 = F_[2][:].rearrange("q (c j) -> q c j", j=64)
                        TT(F_[3][:].rearrange("q (c j) -> q c j", j=64), b3, b3[:, :, 31:32].to_broadcast([128, 8, 64]),
                           ALU.subtract, [FN[2]], [FN[3]])
                        ACT(F_[4][:], F_[3][:], AF.Exp, [FN[3]], [FN[4]])
                        ACT(F_[5][:], F_[3][:], AF.Exp, [FN[3]], [FN[5]], scale=-1.0)
                        es_t, es_n = esc.next()
                        ACT(es_t[:, 0, :], b3[:, :, 31], AF.Exp, [FN[2]], [es_n])
                        ACT(es_t[:, 1, :], b3[:, :, 63], AF.Exp, [FN[2]], [es_n])
                        COPY("dve", es_t[:, 2, :], F_[4][:].rearrange("q (c j) -> q c j", j=64)[:, :, 63], [FN[4]], [es_n])
                        if h == 0 and tl == 0 and s == 0 and l == 0:
                            DUMP(0, F_[2][:], 512, FN[2])
                            DUMP(1, es_t[:].rearrange("q a b -> q (a b)"), 24, es_n)
                            DUMP(2, F_[1][:], 512, FN[1])
                        proj_fm(PS[1], PSN[1], wt, wr, wo[0], 128, t0, 512)
                        qpt, qpn = qp.next()
                        kpt, kpn = kp.next()
                        TT(qpt[:], PS[1][:], F_[4][:], ALU.mult, [PSN[1], FN[4]], [qpn])
                        TT(kpt[:], F_[0][:], F_[5][:], ALU.mult, [FN[0], FN[5]], [kpn], eng="pool")
                        p.skip = p.skip or ("b" in PHASES)
                        ktt, ktn = ktm.next()
                        pst = PS[7][:].bitcast(BF16)
                        for bb in range(4):
                            OP("pe", "transpose", [kpn, CN("ident")], [PSN[7]], chain=bb > 0, out=pst[:, bb * 128:(bb + 1) * 128],
                               in_=kpt[:, bb * 128:(bb + 1) * 128], identity=cst["ident"][:])
                        if h == 0 and tl == 0 and s == 0 and l == 0 and dbg:
                            dgp = p.sb("a_dgp", [128, 512], F32, es)
                            COPY("dve", dgp[:], pst[:, 0:512], [PSN[7]], ["a_dgp"]); DUMP(10, dgp[:], 512, "a_dgp")
                        for hf in range(2):
                            TS(ktt[:, :, hf, :], pst[:, 0:512].rearrange("q (b d) -> q b d", d=128), cst["hmask"][:, hf:hf + 1], None,
                               ALU.mult, None, [PSN[7], CN("hmask")], [ktn])
                        p.skip = p.skip or ("c" in PHASES)
                        psS = PS[2][:, 0:512].rearrange("q (b j) -> q b j", j=128)
                        for bb in range(4):
                            MM(psS[:, bb, :], kpt[:, bb * 128:(bb + 1) * 128], qpt[:, bb * 128:(bb + 1) * 128],
                               True, True, [kpn, qpn], [PSN[2]], chain=bb > 0)
                        sct, scn = scs.next()
                        TT(sct[:], psS, cst["m01"][:].unsqueeze(1).to_broadcast([128, 4, 128]), ALU.mult,
                           [PSN[2], CN("m01")], [scn])
                        for c in range(8):
                            hf, bb = c % 2, c // 2
                            pk = PS[3 + c // 4]
                            MM(pk[:, (c % 4) * 128:(c % 4 + 1) * 128], ktt[:, bb, hf, :],
                               vtm[:, tl * 4 + bb, h * 128:(h + 1) * 128], True, True,
                               [ktn, "a_vtm"], [PSN[3 + c // 4]], chain=(c % 4) > 0)
                        p.skip = p.skip or ("d" in PHASES)
                        po, pon = PS[5 + (tl % 2)], PSN[5 + (tl % 2)]
                        for c in range(8):
                            hf, bb = c % 2, c // 2
                            spt, spn = Sp.next()
                            TS(spt[:], S[:], es_t[:, 0, c:c + 1], None, ALU.mult, None, ["a_S", es_n], [spn])
                            MM(po[:, c * 64:(c + 1) * 64], vtm[:, tl * 4 + bb, h * 128:(h + 1) * 128],
                               sct[:, bb, hf * 64:(hf + 1) * 64], True, False, ["a_vtm", scn], [pon], chain=c > 0)
                            MM(po[:, c * 64:(c + 1) * 64], spt[:], qpt[:, c * 64:(c + 1) * 64], False, True,
                               [spn, qpn], [pon], chain=True)
                            pk = PS[3 + c // 4]
                            TS(tkv[:], pk[:, (c % 4) * 128:(c % 4 + 1) * 128], es_t[:, 2, c:c + 1], None, ALU.mult, None,
                               [PSN[3 + c // 4], es_n], ["a_tkv"])
                            STT(S[:], S[:], es_t[:, 1, c:c + 1], tkv[:], ALU.mult, ALU.add, ["a_S", es_n, "a_tkv"], ["a_S"])
                        if h == 0 and tl == 0 and s == 0 and l == 0 and dbg:
                            DUMP(3, S[:], 128, "a_S")
                            DUMP(4, tkv[:], 128, "a_tkv")
                            dg = [p.sb("a_dg%d" % i, [128, 512], F32, es) for i in range(5)]
                            COPY("dve", dg[0][:], kpt[:], [kpn], ["a_dg0"]); DUMP(5, dg[0][:], 512, "a_dg0")
                            COPY("dve", dg[1][:, 0:256], ktt[:, 3, :, :].rearrange("q a d -> q (a d)"), [ktn], ["a_dg1"]); DUMP(6, dg[1][:, 0:256], 256, "a_dg1")
                            COPY("dve", dg[2][:], PS[4][:], [PSN[4]], ["a_dg2"]); DUMP(7, dg[2][:], 512, "a_dg2")
                            COPY("dve", dg[3][:], vtm[:, 3, 0:512], ["a_vtm"], ["a_dg3"]); DUMP(8, dg[3][:], 512, "a_dg3")
                            COPY("dve", dg[4][:], F_[0][:], [FN[0]], ["a_dg4"]); DUMP(9, dg[4][:], 512, FN[0])
                        p.skip = p.skip or ("e" in PHASES)
                        ACT(E_[0][:], po[:], AF.Square, [pon], [EN[0]])
                        MM(PS[7][:], cst["onesf"][:], E_[0][:], True, True, [CN("onesf"), EN[0]], [PSN[7]])
                        ACT(E_[1][:], PS[7][:], AF.Sqrt, [PSN[7]], [EN[1]], scale=1.0 / 128, bias=EPS)
                        RECIP(E_[1][:], E_[1][:], [EN[1]], [EN[1]])
                        STT(E_[2][:], po[:], pl(P_AG + h), E_[1][:], ALU.mult, ALU.mult, [pon, "par", EN[1]], [EN[2]])
                        proj_fm(PS[0], PSN[0], wt, wr, wo[2], 128, t0, 512)
                        ACT(E_[3][:], PS[0][:], AF.Silu, [PSN[0]], [EN[3]])
                        TT(oT[:, 0, h, t0:t0 + 512], E_[2][:], E_[3][:], ALU.mult, [EN[2], EN[3]], ["oT0"], eng="pool")

            p.skip = "D" not in PHASES
            p.barrier()
            with ExitStack() as es:
                vtm = p.sb("d_vtm", [128, 16, 512], BF16, es)
                kdt = p.sb("d_kdt", [128, 16, 256], BF16, es)
                qs = Ring(p, "d_qs", 2, [128, 512], BF16, es)
                qt = Ring(p, "d_qt", 2, [128, 2, 512], BF16, es)
                kk = Ring(p, "d_kk", 2, [128, 2, 512], BF16, es)
                scs = Ring(p, "d_sc", 2, [128, 4, 128], BF16, es)
                S2 = [p.sb("d_S%d" % i, [128, 128], F32, es) for i in range(2)]
                Sb = Ring(p, "d_Sb", 3, [128, 128], BF16, es)
                E_ = [p.sb("d_e%d" % i, [128, 512], F32, es) for i in range(6)]
                EN = ["d_e%d" % i for i in range(6)]
                gam128 = [float((1.0 - 2.0 ** (-5.0 - h)) ** 128) for h in range(4)]
                wt, wr, wo = wload(l, [(C_DV, 512)])
                for blk in range(16):
                    ps, psn = PS[blk % 2], PSN[blk % 2]
                    proj_tm(ps, psn, wt, wr, 0, 512, blk * 128)
                    COPY("act" if blk % 2 else "dve", vtm[:, blk, :], ps[:, 0:512], [psn], ["d_vtm"])
                wt, wr, wo = wload(l, [(C_DK, 256)])
                for blk in range(16):
                    ps, psn = PS[blk % 2], PSN[blk % 2]
                    proj_tm(ps, psn, wt, wr, 0, 256, blk * 128)
                    TT(kdt[:, blk, :].rearrange("q (h d) -> q h d", d=64), ps[:, 0:256].rearrange("q (h d) -> q h d", d=64),
                       cst["kdec"][:].unsqueeze(2).to_broadcast([128, 4, 64]), ALU.mult, [psn, CN("kdec")], ["d_kdt"])
                p.skip = p.skip or ("a" in PHASES)
                for j in range(2):
                    wt, wr, wo = wload(l, [(C_DQ + 128 * j, 128), (C_DK + 128 * j, 128), (C_DZ + 256 * j, 256)])
                    for hp in range(2):
                        OP("dve", "memset", [], ["d_S%d" % hp], ap=S2[hp][:], constant=0.0)
                    for tl in range(4):
                        t0 = tl * 512
                        proj_fm(PS[0], PSN[0], wt, wr, wo[0], 128, t0, 512)
                        qst, qsn = qs.next()
                        qtt, qtn = qt.next()
                        kkt, kkn = kk.next()
                        p.skip = p.skip or ("x" in PHASES)
                        COPY("act", qst[:], PS[0][:], [PSN[0]], [qsn])
                        p.skip = p.skip or ("y" in PHASES)
                        TT(E_[0][:].rearrange("q (c j) -> q c j", j=128), PS[0][:].rearrange("q (c j) -> q c j", j=128),
                           cst["qdec"][:, j, :].unsqueeze(1).to_broadcast([128, 4, 128]), ALU.mult, [PSN[0], CN("qdec")], [EN[0]])
                        p.skip = p.skip or ("w" in PHASES)
                        for hp in range(2):
                            TS(qtt[:, hp, :], E_[0][:], cst["hmask"][:, hp:hp + 1], None, ALU.mult, None, [EN[0], CN("hmask")], [qtn],
                               eng="pool")
                        p.skip = p.skip or ("z" in PHASES)
                        proj_fm(PS[1], PSN[1], wt, wr, wo[1], 128, t0, 512)
                        for hp in range(2):
                            TS(kkt[:, hp, :], PS[1][:], cst["hmask"][:, hp:hp + 1], None, ALU.mult, None, [PSN[1], CN("hmask")], [kkn])
                        p.skip = p.skip or ("b" in PHASES)
                        for hp in range(2):
                            h = 2 * j + hp
                            psS = PS[2][:, 0:512].rearrange("q (b j) -> q b j", j=128)
                            for bb in range(4):
                                MM(psS[:, bb, :], kkt[:, hp, bb * 128:(bb + 1) * 128], qst[:, bb * 128:(bb + 1) * 128], True, True,
                                   [kkn, qsn], [PSN[2]], chain=bb > 0)
                            sct, scn = scs.next()
                            TT(sct[:], psS, cst["dmask"][:, h, :].unsqueeze(1).to_broadcast([128, 4, 128]), ALU.mult,
                               [PSN[2], CN("dmask")], [scn])
                            pk, pkn = PS[3 + hp], PSN[3 + hp]
                            for bb in range(4):
                                MM(pk[:, bb * 128:(bb + 1) * 128], kdt[:, tl * 4 + bb, 128 * j:128 * j + 128],
                                   vtm[:, tl * 4 + bb, h * 128:(h + 1) * 128], True, True, ["d_kdt", "d_vtm"], [pkn], chain=bb > 0)
                            p.skip = p.skip or ("c" in PHASES)
                            po, pon = PS[5 + hp], PSN[5 + hp]
                            Sn = "d_S%d" % hp
                            for bb in range(4):
                                sbt, sbn = Sb.next()
                                COPY("act", sbt[:], S2[hp][:], [Sn], [sbn])
                                MM(po[:, bb * 128:(bb + 1) * 128], vtm[:, tl * 4 + bb, h * 128:(h + 1) * 128], sct[:, bb, :],
                                   True, False, ["d_vtm", scn], [pon], chain=bb > 0)
                                MM(po[:, bb * 128:(bb + 1) * 128], sbt[:], qtt[:, hp, bb * 128:(bb + 1) * 128], False, True,
                                   [sbn, qtn], [pon], chain=True)
                                STT(S2[hp][:], S2[hp][:], gam128[h], pk[:, bb * 128:(bb + 1) * 128], ALU.mult, ALU.add, [Sn, pkn], [Sn])
                            p.skip = p.skip or ("d" in PHASES)
                            COPY("act", E_[0][:], po[:], [pon], [EN[0]])
                            ACT(E_[1][:], po[:], AF.Square, [pon], [EN[1]])
                            MM(PS[7][:], cst["onesf"][:], E_[0][:], True, True, [CN("onesf"), EN[0]], [PSN[7]])
                            ACT(E_[2][:], PS[7][:], AF.Copy, [PSN[7]], [EN[2]], scale=1.0 / 128)
                            MM(PS[7][:], cst["onesf"][:], E_[1][:], True, True, [CN("onesf"), EN[1]], [PSN[7]])
                            TT(E_[3][:], E_[2][:], E_[2][:], ALU.mult, [EN[2]], [EN[3]], eng="pool")
                            STT(E_[3][:], PS[7][:], 1.0 / 128, E_[3][:], ALU.mult, ALU.subtract, [PSN[7], EN[3]], [EN[3]])
                            TS(E_[3][:], E_[3][:], 0.0, None, ALU.max, None, [EN[3]], [EN[3]])
                            ACT(E_[3][:], E_[3][:], AF.Sqrt, [EN[3]], [EN[3]], bias=EPS)
                            RECIP(E_[3][:], E_[3][:], [EN[3]], [EN[3]])
                            TT(E_[0][:], E_[0][:], E_[2][:], ALU.subtract, [EN[0], EN[2]], [EN[0]], eng="pool")
                            STT(E_[4][:], E_[0][:], pl(P_DG + h), E_[3][:], ALU.mult, ALU.mult, [EN[0], "par", EN[3]], [EN[4]])
                            proj_fm(PS[0], PSN[0], wt, wr, wo[2] + 128 * hp, 128, t0, 512)
                            ACT(E_[5][:], PS[0][:], AF.Silu, [PSN[0]], [EN[5]])
                            TT(oT[:, 3, h, t0:t0 + 512], E_[4][:], E_[5][:], ALU.mult, [EN[4], EN[5]], ["oT3"], eng="pool")

            p.skip = "C" not in PHASES
            p.barrier()
            with ExitStack() as es:
                wbd = p.sb("c_wbd", [128, 2, 4, 128], BF16, es)
                xr = [p.sb("c_xr%d" % i, [128, 515], F32, es) for i in range(2)]
                XRN = ["c_xr0", "c_xr1"]
                G_ = [p.sb("c_g%d" % i, [128, 512], F32, es) for i in range(7)]
                GN = ["c_g%d" % i for i in range(7)]
                xcb = p.sb("c_xcb", [128, 512], BF16, es)
                hh = [p.sb("c_h%d" % i, [128, 512], F32, es) for i in range(2)]
                HN = ["c_h0", "c_h1"]
                OP("pool", "memset", [], ["c_wbd"], ap=wbd[:], constant=0.0)
                for wi, wsrc in enumerate((wra, wri)):
                    for hb in range(2):
                        DMA("pool", wbd[64 * hb:64 * hb + 64, wi, :, 64 * hb:64 * hb + 64],
                            wsrc[l].rearrange("(j b) c d -> b c j d", b=2)[hb], reads=[], writes=["c_wbd"])
                for j in range(4):
                    wt, wr, wo = wload(l, [(C_CX + 128 * j, 128), (C_CZ + 128 * j, 128)])
                    for tl in range(4):
                        t0 = tl * 512
                        xa, xan = xr[tl % 2], XRN[tl % 2]
                        xb, xbn = xr[(tl + 1) % 2], XRN[(tl + 1) % 2]
                        if tl == 0:
                            OP("dve", "memset", [], [xan], ap=xa[:, 0:3], constant=0.0)
                        proj_fm(PS[0], PSN[0], wt, wr, wo[0], 128, t0, 512)
                        COPY("act", xa[:, 3:515], PS[0][:], [PSN[0]], [xan])
                        if tl < 3:
                            COPY("dve", xb[:, 0:3], xa[:, 512:515], [xan], [xbn])
                        cw = lambda k, j=j: pl(P_CW + 4 * j + k)
                        TS(G_[0][:], xa[:, 3:515], cw(3), pl(P_CB + j), ALU.mult, ALU.add, [xan, "par"], [GN[0]])
                        STT(G_[0][:], xa[:, 2:514], cw(2), G_[0][:], ALU.mult, ALU.add, [xan, "par", GN[0]], [GN[0]])
                        STT(G_[0][:], xa[:, 1:513], cw(1), G_[0][:], ALU.mult, ALU.add, [xan, "par", GN[0]], [GN[0]])
                        STT(G_[0][:], xa[:, 0:512], cw(0), G_[0][:], ALU.mult, ALU.add, [xan, "par", GN[0]], [GN[0]])
                        COPY("act", xcb[:], G_[0][:], [GN[0]], ["c_xcb"])
                        MM(PS[1][:], wbd[:, 0, j, :], xcb[:], True, True, ["c_wbd", "c_xcb"], [PSN[1]])
                        MM(PS[2][:], wbd[:, 1, j, :], xcb[:], True, True, ["c_wbd", "c_xcb"], [PSN[2]])
                        ACT(G_[1][:], PS[1][:], AF.Sigmoid, [PSN[1], "par"], [GN[1]], bias=pl(P_BRA + j))
                        ACT(G_[2][:], PS[2][:], AF.Sigmoid, [PSN[2], "par"], [GN[2]], bias=pl(P_BRI + j))
                        ACT(G_[3][:], G_[1][:], AF.Exp, [GN[1], "drv"], [GN[3]], scale=drv[:, l, 8 + j:9 + j])
                        ACT(G_[4][:], G_[1][:], AF.Exp, [GN[1], "drv"], [GN[4]], scale=drv[:, l, 12 + j:13 + j])
                        ACT(G_[4][:], G_[4][:], AF.Sqrt, [GN[4]], [GN[4]], scale=-1.0, bias=1.0)
                        TT(G_[2][:], G_[2][:], G_[0][:], ALU.mult, [GN[2], GN[0]], [GN[2]], eng="pool")
                        TT(G_[2][:], G_[2][:], G_[4][:], ALU.mult, [GN[2], GN[4]], [GN[2]])
                        ha, han = hh[tl % 2], HN[tl % 2]
                        hb_, hbn = hh[(tl + 1) % 2], HN[(tl + 1) % 2]
                        init = 0.0 if tl == 0 else hb_[:, 511:512]
                        OP("dve", "tensor_tensor_scan", [GN[3], GN[2], hbn], [han], out=ha[:], data0=G_[3][:], data1=G_[2][:],
                           initial=init, op0=ALU.mult, op1=ALU.add)
                        proj_fm(PS[3], PSN[3], wt, wr, wo[1], 128, t0, 512)
                        ACT(G_[5][:], PS[3][:], AF.Silu, [PSN[3]], [GN[5]])
                        TT(oT[:, 2, j, t0:t0 + 512], ha[:], G_[5][:], ALU.mult, [han, GN[5]], ["oT2"], eng="pool")

            p.skip = "B" not in PHASES
            p.barrier()
            with ExitStack() as es:
                ksT = p.sb("b_ksT", [128, T], BF16, es)
                kwT = p.sb("b_kwT", [128, T], BF16, es)
                vsa = p.sb("b_vsa", [128, 16, 2, 65], BF16, es)
                vwa = p.sb("b_vwa", [128, 16, 2, 65], BF16, es)
                gts = p.sb("b_gt", [128, 16, 24], F32, es)
                kcm = p.sb("b_kcm", [128, 128], BF16, es)
                Rc = p.sb("b_Rc", [128, 2, 97], BF16, es)
                B_ = [p.sb("b_t%d" % i, [128, 512], F32, es) for i in range(3)]
                BN = ["b_t%d" % i for i in range(3)]
                with ExitStack() as es2:
                    kcT = p.sb("b_kcT", [128, T], BF16, es2)
                    vcT = p.sb("b_vcT", [128, T], BF16, es2)
                    wkb = p.sb("b_wkb", [128, 32, 128], BF16, es2)
                    wvb = p.sb("b_wvb", [128, 32, 128], BF16, es2)
                    posb = p.sb("b_posb", [128, 32], BF16, es2)
                    cb = p.sb("b_cb", [128, 2], F32, es2)
                    raw = p.sb("b_raw", [128, 2, 128], F32, es2)
                    vrb = p.sb("b_vrb", [128, 128], BF16, es2)
                    OP("pool", "memset", [], ["b_wkb"], ap=wkb[:], constant=0.0)
                    OP("pool", "memset", [], ["b_wvb"], ap=wvb[:], constant=0.0)
                    for wtile, wn_, wsrc in ((wkb, "b_wkb", wck), (wvb, "b_wvb", wcv)):
                        for g in range(2):
                            for lh in range(2):
                                DMA("pool", wtile[64 * g:64 * g + 64, 16 * lh:16 * lh + 16, 64 * g:64 * g + 64],
                                    wsrc[l].rearrange("(a d) c -> d a c", d=64)[:, 16 * lh:16 * lh + 16, :], writes=[wn_])
                    COPY("dve", posb[:], pl(P_POS, 32), ["par"], ["b_posb"])
                    OP("pool", "memset", [], ["b_vsa"], ap=vsa[:, :, :, 64:65], constant=1.0)
                    OP("pool", "memset", [], ["b_vwa"], ap=vwa[:, :, :, 64:65], constant=1.0)
                    OP("pool", "memset", [], ["b_raw"], ap=raw[:], constant=0.0)
                    OP("pool", "memset", [], ["b_Rc"], ap=Rc[:], constant=0.0)
                    wt, wr, wo = wload(l, [(C_BKC, 128), (C_BVC, 128), (C_BKS, 128), (C_BKW, 128)])
                    for tl in range(4):
                        t0 = tl * 512
                        proj_fm(PS[0], PSN[0], wt, wr, wo[0], 128, t0, 512)
                        COPY("act", kcT[:, t0:t0 + 512], PS[0][:], [PSN[0]], ["b_kcT"])
                        proj_fm(PS[1], PSN[1], wt, wr, wo[1], 128, t0, 512)
                        COPY("dve", vcT[:, t0:t0 + 512], PS[1][:], [PSN[1]], ["b_vcT"])
                        for wi, (dst, dn) in enumerate(((ksT, "b_ksT"), (kwT, "b_kwT"))):
                            ps, psn = PS[2 + wi], PSN[2 + wi]
                            proj_fm(ps, psn, wt, wr, wo[2 + wi], 128, t0, 512)
                            ACT(B_[0][:], ps[:], AF.Square, [psn], [BN[0]])
                            MM(PS[7][:], cst["onesbd"][:], B_[0][:], True, True, [CN("onesbd"), BN[0]], [PSN[7]])
                            ACT(B_[1][:], PS[7][:], AF.Sqrt, [PSN[7]], [BN[1]], scale=1.0 / 64, bias=EPS)
                            RECIP(B_[1][:], B_[1][:], [BN[1]], [BN[1]])
                            STT(dst[:, t0:t0 + 512], ps[:], pl(P_KG), B_[1][:], ALU.mult, ALU.mult, [psn, "par", BN[1]], [dn])
                    wt, wr, wo = wload(l, [(C_BVS, 128), (C_BVW, 128), (C_BG, 24)])
                    for blk in range(16):
                        ps, psn = PS[blk % 2], PSN[blk % 2]
                        proj_tm(ps, psn, wt, wr, 0, 280, blk * 128)
                        COPY("dve", vsa[:, blk, :, 0:64], ps[:, 0:128].rearrange("q (g d) -> q g d", d=64), [psn], ["b_vsa"])
                        COPY("act", vwa[:, blk, :, 0:64], ps[:, 128:256].rearrange("q (g d) -> q g d", d=64), [psn], ["b_vwa"])
                        ACT(gts[:, blk, :], ps[:, 256:280], AF.Sigmoid, [psn], ["b_gt"])
                    for wi, (srcT, sn, wtile, wn_) in enumerate(((kcT, "b_kcT", wkb, "b_wkb"), (vcT, "b_vcT", wvb, "b_wvb"))):
                        ps, psn = PS[2 + wi], PSN[2 + wi]
                        for ll in range(32):
                            MM(ps[:, 0:127], wtile[:, ll, :], srcT[:].rearrange("q (n u) -> q n u", u=16)[:, (ll // 16):(ll // 16) + 127, ll % 16], ll == 0, ll == 31,
                               [wn_, sn], [psn], chain=ll > 0)
                        psb, psbn = PS[4 + wi], PSN[4 + wi]
                        for ll in range(32):
                            MM(psb[:, 0:1], wtile[:, ll, :], posb[:, ll:ll + 1], ll == 0, ll == 31, [wn_, "b_posb"], [psbn],
                               chain=ll > 0)
                        COPY("dve", cb[:, wi:wi + 1], psb[:, 0:1], [psbn], ["b_cb"])
                        ACT(raw[:, wi, 0:127], ps[:, 0:127], AF.Identity, [psn, "b_cb"], ["b_raw"], bias=cb[:, wi:wi + 1])
                    ACT(B_[0][:, 0:128], raw[:, 0, :], AF.Square, ["b_raw"], [BN[0]])
                    MM(PS[7][:, 0:128], cst["onesbd"][:], B_[0][:, 0:128], True, True, [CN("onesbd"), BN[0]], [PSN[7]])
                    ACT(B_[1][:, 0:128], PS[7][:, 0:128], AF.Sqrt, [PSN[7]], [BN[1]], scale=1.0 / 64, bias=EPS)
                    RECIP(B_[1][:, 0:128], B_[1][:, 0:128], [BN[1]], [BN[1]])
                    STT(kcm[:], raw[:, 0, :], pl(P_KG), B_[1][:, 0:128], ALU.mult, ALU.mult, ["b_raw", "par", BN[1]], ["b_kcm"])
                    COPY("dve", vrb[:], raw[:, 1, :], ["b_raw"], ["b_vrb"])
                    pst = PS[6][:].bitcast(BF16)
                    OP("pe", "transpose", ["b_vrb", CN("ident")], [PSN[6]], out=pst[:, 0:128], in_=vrb[:], identity=cst["ident"][:])
                    COPY("dve", Rc[:, :, 0:64], pst[:, 0:128].rearrange("q (g d) -> q g d", d=64), [PSN[6]], ["b_Rc"])
                    for g in range(2):
                        COPY("dve", Rc[:, g, 64:97], cst["ovl"][:], [CN("ovl")], ["b_Rc"])
                    p.barrier()
                qG = p.sb("b_qG", [128, 4, 512], BF16, es)
                qGz = p.sb("b_qGz", [128, 2, 4, 512], BF16, es)
                szT = p.sb("b_sz", [128, 4, 512], BF16, es)
                nsT = p.sb("b_nsT", [32, 4, 128], BF16, es)
                ET = Ring(p, "b_e", 3, [128, 512], BF16, es)
                acc = p.sb("b_acc", [128, 8, 64], F32, es)
                accb = p.sb("b_accb", [128, 512], BF16, es)
                sm = [p.sb("b_sm%d" % i, [128, 4], F32, es) for i in range(3)]
                impw = p.sb("b_impw", [128, 4, 32], F32, es)
                imp = p.sb("b_imp", [128, 32], F32, es)
                mx8 = p.sb("b_mx8", [128, 8], F32, es)
                nsb = p.sb("b_nsb", [128, 32], BF16, es)
                tmpo = p.sb("b_tmpo", [128, 4, 64], F32, es)
                sbank = [2, 3]
                sidx = [0]

                def next_s():
                    i = sbank[sidx[0] % 2]
                    sidx[0] += 1
                    return PS[i], PSN[i]
                for qb in range(16):
                    tl, ql = qb // 4, qb % 4
                    if ql == 0:
                        t0 = tl * 512
                        segs = []
                        for m in range(4):
                            segs += [(C_BQ + 64 * m, 64), (C_BQ + 64 * (4 + m), 64)]
                        wt, wr, wo = wload(l, [(C_BQ, 512)])
                        wtq, wrq = wt, wr
                        wt2, wr2, wo2 = wload(l, [(C_BZ, 512)])
                        for m in range(4):
                            ps, psn = PS[m % 2], PSN[m % 2]
                            for kc in range(8):
                                MM(ps[0:64, 0:512], wtq[:, kc, 64 * m:64 * m + 64], xn[:, kc, t0:t0 + 512], kc == 0, kc == 7,
                                   list(wrq) + ["xn"], [psn], chain=kc > 0)
                            for kc in range(8):
                                MM(ps[64:128, 0:512], wtq[:, kc, 64 * (4 + m):64 * (4 + m) + 64], xn[:, kc, t0:t0 + 512], kc == 0, kc == 7,
                                   list(wrq) + ["xn"], [psn], chain=True)
                            ACT(B_[0][:], ps[:], AF.Square, [psn], [BN[0]])
                            MM(PS[7][:], cst["onesbd"][:], B_[0][:], True, True, [CN("onesbd"), BN[0]], [PSN[7]])
                            ACT(B_[1][:], PS[7][:], AF.Sqrt, [PSN[7]], [BN[1]], scale=1.0 / 64, bias=EPS)
                            RECIP(B_[1][:], B_[1][:], [BN[1]], [BN[1]])
                            STT(qG[:, m, :], ps[:], drv[:, l, 16:17], B_[1][:], ALU.mult, ALU.mult, [psn, "drv", BN[1]], ["b_qG"])
                            for g in range(2):
                                TS(qGz[:, g, m, :], qG[:, m, :], cst["hmask"][:, g:g + 1], None, ALU.mult, None,
                                   ["b_qG", CN("hmask")], ["b_qGz"], eng="pool")
                        for jj in range(4):
                            ps, psn = PS[jj % 2], PSN[jj % 2]
                            proj_fm(ps, psn, wt2, wr2, 128 * jj, 128, t0, 512)
                            ACT(szT[:, jj, :], ps[:], AF.Silu, [psn], ["b_sz"])
                    gq = gts[:, qb, :].rearrange("q (h c) -> q h c", c=3)
                    for g in range(2):
                        gb = 64 * g
                        qrhs = qGz[:, g, :, ql * 128:(ql + 1) * 128]
                        rsl = cst["Rsm"][0:2, g, :]
                        nk = min(127, 8 * qb + 7)
                        ps, psn = next_s()
                        MM(ps[0:nk, :], kcm[:, 0:nk], qrhs, True, False, ["b_kcm", "b_qGz"], [psn])
                        MM(ps[0:nk, :], cst["Lcm"][0:10, qb, 0:nk], cst["Rsm"][0:10, g, :], False, True, [CN("Lcm"), CN("Rsm")], [psn], chain=True)
                        et, etn = ET.next()
                        ACT(et[0:nk, :], ps[0:nk, :], AF.Exp, [psn], [etn])
                        poc = PS[6][:, 0:388].rearrange("q (r c) -> q r c", c=97)
                        for r in range(4):
                            MM(poc[:, r, :], et[0:nk, r * 128:(r + 1) * 128], Rc[0:nk, g, :], True, True, [etn, "b_Rc"], [PSN[6]],
                               chain=r > 0)
                        TS(sm[0][:], poc[:, :, 96], 1e-30, None, ALU.max, None, [PSN[6]], ["b_sm0"])
                        RECIP(sm[0][:], sm[0][:], ["b_sm0"], ["b_sm0"])
                        TT(impw[:], poc[:, :, 64:96], sm[0][:].unsqueeze(2).to_broadcast([128, 4, 32]), ALU.mult,
                           [PSN[6], "b_sm0"], ["b_impw"])
                        OP("dve", "tensor_reduce", ["b_impw"], ["b_imp"], out=imp[:], in_=impw[:].rearrange("q r j -> q j r"),
                           axis=AX.X, op=ALU.add)
                        TT(sm[0][:], sm[0][:], gq[:, 4 * g:4 * g + 4, 0], ALU.mult, ["b_sm0", "b_gt"], ["b_sm0"])
                        TT(acc[:, 4 * g:4 * g + 4, :], poc[:, :, 0:64], sm[0][:].unsqueeze(2).to_broadcast([128, 4, 64]), ALU.mult,
                           [PSN[6], "b_sm0"], ["b_acc"])
                        TT(imp[:], imp[:], cst["mulm"][:, qb, :], ALU.mult, ["b_imp", CN("mulm")], ["b_imp"])
                        TT(imp[:], imp[:], cst["addm"][:, qb, :], ALU.add, ["b_imp", CN("addm")], ["b_imp"])
                        OP("dve", "max", ["b_imp"], ["b_mx8"], out=mx8[:], in_=imp[:])
                        TS(imp[:], imp[:], mx8[:, 7:8], -NEG, ALU.is_ge, ALU.mult, ["b_imp", "b_mx8"], ["b_imp"])
                        TS(nsb[:], imp[:], NEG, None, ALU.add, None, ["b_imp"], ["b_nsb"])
                        pst = PS[7][:].bitcast(BF16)
                        OP("pe", "transpose", ["b_nsb", CN("ident")], [PSN[7]], out=pst[0:32, 0:128], in_=nsb[:], identity=cst["ident"][:])
                        for r in range(4):
                            COPY("act" if r % 2 else "dve", nsT[:, r, :], pst[0:32, 0:128], [PSN[7]], ["b_nsT"])
                        pos_ = PS[4][:, 0:260].rearrange("q (r c) -> q r c", c=65)
                        MM(PS[4][:, 0:260], zer[0:1, 0:128], zer[0:1, 0:260], True, False, ["zer"], [PSN[4]], skip_group_check=True)
                        for kt in range(qb + 1):
                            ps, psn = next_s()
                            MM(ps[:], ksT[:, kt * 128:(kt + 1) * 128], qrhs, True, False, ["b_ksT", "b_qGz"], [psn])
                            MM(ps[:], cst["LAl"][0:2, qb - kt, :], rsl, False, False, [CN("LAl"), CN("Rsm")], [psn], chain=True)
                            MM(ps[:], cst["E"][0:32, kt, :], nsT[:].rearrange("q r t -> q (r t)"), False, kt < qb,
                               [CN("E"), "b_nsT"], [psn], chain=True)
                            if kt == qb:
                                MM(ps[:], cst["ident"][:], cst["causneg"][:], False, True, [CN("ident"), CN("causneg")], [psn],
                                   chain=True)
                            et, etn = ET.next()
                            ACT(et[:], ps[:], AF.Exp, [psn], [etn])
                            for r in range(4):
                                MM(pos_[:, r, :], et[:, r * 128:(r + 1) * 128], vsa[:, kt, g, :], False, kt == qb,
                                   [etn, "b_vsa"], [PSN[4]], chain=True, skip_group_check=True)
                        pow_ = PS[5][:, 0:260].rearrange("q (r c) -> q r c", c=65)
                        MM(PS[5][:, 0:260], zer[0:1, 0:128], zer[0:1, 0:260], True, False, ["zer"], [PSN[5]], skip_group_check=True)
                        for kt in range(max(0, qb - 2), qb + 1):
                            ps, psn = next_s()
                            last_plain = (kt == qb - 1)
                            MM(ps[:], kwT[:, kt * 128:(kt + 1) * 128], qrhs, True, False, ["b_kwT", "b_qGz"], [psn])
                            MM(ps[:], cst["LAl"][0:2, qb - kt, :], rsl, False, last_plain, [CN("LAl"), CN("Rsm")], [psn], chain=True)
                            if kt == qb:
                                MM(ps[:], cst["ident"][:], cst["causneg"][:], False, True, [CN("ident"), CN("causneg")], [psn],
                                   chain=True)
                            elif kt == qb - 2:
                                MM(ps[:], cst["ident"][:], cst["winneg"][:], False, True, [CN("ident"), CN("winneg")], [psn],
                                   chain=True)
                            et, etn = ET.next()
                            ACT(et[:], ps[:], AF.Exp, [psn], [etn])
                            for r in range(4):
                                MM(pow_[:, r, :], et[:, r * 128:(r + 1) * 128], vwa[:, kt, g, :], False, kt == qb,
                                   [etn, "b_vwa"], [PSN[5]], chain=True, skip_group_check=True)
                        for bi, (pso, pson) in enumerate(((pos_, PSN[4]), (pow_, PSN[5]))):
                            smt, smn = sm[1 + bi], "b_sm%d" % (1 + bi)
                            RECIP(smt[:], pso[:, :, 64], [pson], [smn])
                            TT(smt[:], smt[:], gq[:, 4 * g:4 * g + 4, 1 + bi], ALU.mult, [smn, "b_gt"], [smn])
                            TT(tmpo[:], pso[:, :, 0:64], smt[:].unsqueeze(2).to_broadcast([128, 4, 64]), ALU.mult,
                               [pson, smn], ["b_tmpo"])
                            TT(acc[:, 4 * g:4 * g + 4, :], acc[:, 4 * g:4 * g + 4, :], tmpo[:], ALU.add, ["b_acc", "b_tmpo"],
                               ["b_acc"], eng="pool")
                    COPY("act", accb[:], acc[:].rearrange("q h d -> q (h d)"), ["b_acc"], ["b_accb"])
                    pst = PS[7][:].bitcast(BF16)
                    for jj in range(4):
                        OP("pe", "transpose", ["b_accb", CN("ident")], [PSN[7]], chain=jj > 0, out=pst[:, jj * 128:(jj + 1) * 128],
                           in_=accb[:, jj * 128:(jj + 1) * 128], identity=cst["ident"][:])
                    TT(oT[:, 1, :, qb * 128:(qb + 1) * 128], pst[:, 0:512].rearrange("q (j t) -> q j t", t=128),
                       szT[:, :, ql * 128:(ql + 1) * 128], ALU.mult, [PSN[7], "b_sz"], ["oT1"])

            p.skip = False
            if dbg:
                for br in range(4):
                    if "ABCD"[br] not in PHASES or any(ch in PHASES for ch in "abcde"):
                        continue
                    DMA("pool", dbgT[s, l, br].rearrange("(j q) t -> q j t", q=128), oT[:, br, :, :],
                        reads=["oT%d" % br], writes=["dbg"])

            p.skip = "M" not in PHASES
            p.barrier()
            with ExitStack() as es:
                mg = p.sb("m_mg", [128, 8, T], BF16, es)
                macc = p.sb("m_acc", [128, T], F32, es)
                GT = Ring(p, "m_g", 2, [128, 512], F32, es)
                TM = Ring(p, "m_t", 2, [128, 512], F32, es)
                WBR = Ring(p, "m_wb", 2, [128, 4, 128], BF16, es)
                WO = Ring(p, "m_wo", 2, [128, 8, 128], BF16, es)
                XR = Ring(p, "m_xr", 2, [128, 512], F32, es)
                for c in range(8):
                    segs = [(C_MG + br * 1024 + c * 128, 128) for br in range(4)]
                    wt, wr, wo = wload(l, segs)
                    for br in range(4):
                        wbt, wbn = WBR.next()
                        DMA("pool", wbt[:], w_br[l, br].rearrange("(k q) n -> q k n", q=128)[:, :, c * 128:(c + 1) * 128],
                            writes=[wbn])
                        for tl in range(4):
                            t0 = tl * 512
                            psP, psPn = PS[(2 * tl) % 8], PSN[(2 * tl) % 8]
                            psG, psGn = PS[(2 * tl + 1) % 8], PSN[(2 * tl + 1) % 8]
                            for k in range(4):
                                MM(psP[:], wbt[:, k, :], oT[:, br, k, t0:t0 + 512], k == 0, k == 3, [wbn, "oT%d" % br], [psPn],
                                   chain=k > 0)
                            proj_fm(psG, psGn, wt, wr, wo[br], 128, t0, 512)
                            gt_, gtn = GT.next()
                            ACT(gt_[:], psG[:], AF.Sigmoid, [psGn, "par"], [gtn], bias=pl(P_MB + br * 8 + c))
                            if br == 0:
                                TT(macc[:, t0:t0 + 512], gt_[:], psP[:], ALU.mult, [gtn, psPn], ["m_acc%d" % tl])
                            else:
                                tm_, tmn = TM.next()
                                TT(tm_[:], gt_[:], psP[:], ALU.mult, [gtn, psPn], [tmn])
                                if br < 3:
                                    TT(macc[:, t0:t0 + 512], macc[:, t0:t0 + 512], tm_[:], ALU.add, ["m_acc%d" % tl, tmn],
                                       ["m_acc%d" % tl], eng="pool")
                                else:
                                    TT(mg[:, c, t0:t0 + 512], macc[:, t0:t0 + 512], tm_[:], ALU.add, ["m_acc%d" % tl, tmn],
                                       ["m_mg"], eng="pool")
                for c2 in range(8):
                    wot, won = WO.next()
                    DMA("pool", wot[:], w_out[l].rearrange("(k q) n -> q k n", q=128)[:, :, c2 * 128:(c2 + 1) * 128],
                        writes=[won])
                    for tl in range(4):
                        t0 = tl * 512
                        ps, psn = PS[(c2 * 4 + tl) % 8], PSN[(c2 * 4 + tl) % 8]
                        for k in range(8):
                            MM(ps[:], wot[:, k, :], mg[:, k, t0:t0 + 512], k == 0, k == 7, [won, "m_mg"], [psn], chain=k > 0)
                        xr_, xrn = XR.next()
                        DMA("sp", xr_[:], src_x[s, c2 * 128:(c2 + 1) * 128, t0:t0 + 512], reads=[RES + "_%d_%d" % (c2, tl)], writes=[xrn])
                        TT(xr_[:], xr_[:], ps[:], ALU.add, [xrn, psn], [xrn])
                        DMA("sp", outT[s, c2 * 128:(c2 + 1) * 128, t0:t0 + 512], xr_[:], reads=[xrn], writes=[RES + "_%d_%d" % (c2, tl)])
            p.barrier()

    toks = p.all_tokens()
    waits = p._deps("sp", (), (), toks)
    p.ops["sp"].append((None, waits, None))
    p.emit()
    return nc


_PROG_CACHE = {}


def _get_prog(NL, NS, dbg=False):
    key = (NL, NS, dbg)
    if key not in _PROG_CACHE:
        _PROG_CACHE[key] = build_program(NL, NS, dbg)
    return _PROG_CACHE[key]


def _common_maps(inp, layers):
    NL = len(layers)
    m = {}
    for k in ("w_in", "w_branch", "w_out", "b_cmp_wk", "b_cmp_wv", "c_w_ra", "c_w_ri"):
        m[k] = np.ascontiguousarray(np.asarray(inp[k], np.float32)[layers])
    m["par"] = np.stack([_params(inp, l) for l in layers], 0)
    m["lbl"] = np.ascontiguousarray(np.asarray(inp["lb_logits"], np.float32).reshape(4, 4, 128).transpose(2, 1, 0).reshape(128, 16))
    ls = np.zeros((128, NL, 4), np.float32)
    for i, l in enumerate(layers):
        ls[:, i, l] = 1.0
    m["lsel"] = ls.reshape(128, NL * 4)
    for k, v in _consts().items():
        m["c_" + k] = v
    return m


MODE = "layer"
PHASES = "0ADCBM"
POOLMAP = "dve"
NCORES = 8


def kernel(**inputs):
    inp = {k: np.asarray(v) for k, v in inputs.items()}
    x = np.asarray(inp["x"], np.float32)
    B = x.shape[0]
    NSC = B // NCORES
    xTh = np.ascontiguousarray(x.transpose(0, 2, 1))
    if MODE == "fused":
        nc = _get_prog(4, NSC)
        cm = _common_maps(inp, [0, 1, 2, 3])
        maps = [dict(cm, xT=xTh[c * NSC:(c + 1) * NSC]) for c in range(NCORES)]
        res = run_bass_kernel_spmd(nc, maps, core_ids=list(range(NCORES)))
        oTh = np.concatenate([r["outT"] for r in res.results], 0)
    elif MODE == "layer":
        nc = _get_prog(1, NSC)
        cur = xTh
        for l in range(4):
            cm = _common_maps(inp, [l])
            maps = [dict(cm, xT=cur[c * NSC:(c + 1) * NSC]) for c in range(NCORES)]
            res = run_bass_kernel_spmd(nc, maps, core_ids=list(range(NCORES)))
            cur = np.concatenate([r["outT"] for r in res.results], 0)
        oTh = cur
    else:
        nc = _get_prog(4, 1)
        cm = _common_maps(inp, [0, 1, 2, 3])
        outs = []
        for g in range(NSC):
            maps = [dict(cm, xT=xTh[c * NSC + g:c * NSC + g + 1]) for c in range(NCORES)]
            res = run_bass_kernel_spmd(nc, maps, core_ids=list(range(NCORES)))
            outs.append([r["outT"] for r in res.results])
        oTh = np.concatenate([outs[g][c] for c in range(NCORES) for g in range(NSC)], 0)
    return np.ascontiguousarray(oTh.transpose(0, 2, 1)).astype(np.float32)
```

```python
import numpy as np
import concourse.bass as bass
import concourse.mybir as mybir
from concourse.bass_utils import run_bass_kernel_spmd
from contextlib import ExitStack

F32 = mybir.dt.float32
BF16 = mybir.dt.bfloat16
AF = mybir.ActivationFunctionType
ALU = mybir.AluOpType
AX = mybir.AxisListType

D = 1024
T = 2048
NIN = 10520
W = 512
EPS = 1e-6
NEG = -30000.0
ENGS = ("pe", "act", "dve", "pool", "sp")
SEM_LIM = 30000

C_AQ, C_AF, C_AI, C_AZ = 0, 512, 1024, 1536
C_BQ, C_BKC, C_BVC, C_BKS, C_BVS, C_BKW, C_BVW, C_BG, C_BZ = 2048, 2560, 2688, 2816, 2944, 3072, 3200, 3328, 3352
C_CX, C_CZ = 3864, 4376
C_DQ, C_DK, C_DV, C_DZ = 4888, 5144, 5400, 5912
C_MG = 6424


class Prog:
    def __init__(self, nc, n_dma_sems=8):
        self.nc = nc
        self.es = ExitStack()
        self.ops = {e: [] for e in ENGS}
        self.nsem = 0
        self.csem = {e: self._newsem() for e in ENGS}
        self.ccount = {e: 0 for e in ENGS}
        self.nd = n_dma_sems
        self.dsem = {q: [self._newsem() for _ in range(n_dma_sems)] for q in ("sp", "pool", "act")}
        self.dcnt = {q: [0] * n_dma_sems for q in ("sp", "pool", "act")}
        self.dnext = {q: 0 for q in ("sp", "pool", "act")}
        self.last_w = {}
        self.readers = {}
        self.waited = {e: {} for e in ENGS}
        self.skip = False
        self.prev_tok = {}

    def _newsem(self):
        self.nsem += 1
        return self.es.enter_context(self.nc.semaphore("s%d" % self.nsem))

    def sb(self, name, shape, dt, es=None):
        self.nalloc = getattr(self, "nalloc", 0) + 1
        return (es or self.es).enter_context(self.nc.sbuf_tensor("%s_%d" % (name, self.nalloc), list(shape), dt))

    def ps(self, name, shape, dt):
        return self.es.enter_context(self.nc.psum_tensor(name, list(shape), dt))

    def tokens(self, names):
        out = []
        for n in names:
            t = self.last_w.get(n)
            if t is not None:
                out.append(t)
            out.extend(self.readers.get(n, ()))
        return out

    def _deps(self, eng, reads, writes, extra):
        need = {}

        def add(tok):
            s, v = tok
            k = id(s)
            if k not in need or need[k][1] < v:
                need[k] = (s, v)
        for r in reads:
            t = self.last_w.get(r)
            if t is not None:
                add(t)
        for w in writes:
            t = self.last_w.get(w)
            if t is not None:
                add(t)
            for t in self.readers.get(w, ()):
                add(t)
        for t in extra:
            add(t)
        out = []
        wd = self.waited[eng]
        for k, (s, v) in need.items():
            if wd.get(k, 0) >= v:
                continue
            wd[k] = v
            out.append((s, v))
        return out

    def _commit(self, tok, reads, writes):
        for r in reads:
            lst = self.readers.setdefault(r, [])
            lst[:] = [t for t in lst if t[0] is not tok[0]]
            lst.append(tok)
        for w in writes:
            self.last_w[w] = tok
            self.readers[w] = []

    def op(self, eng, fn, reads=(), writes=(), chain=False, extra=()):
        if self.skip:
            return None
        reads = tuple(reads)
        writes = tuple(writes) + tuple(r for r in reads if r.startswith("psb"))
        waits = self._deps(eng, reads, () if chain else writes, extra)
        if self.ccount[eng] >= SEM_LIM:
            self.prev_tok[eng] = (self.csem[eng], self.ccount[eng])
            self.csem[eng] = self._newsem()
            self.ccount[eng] = 0
        self.ccount[eng] += 1
        tok = (self.csem[eng], self.ccount[eng])
        self.ops[eng].append((fn, waits, (self.csem[eng], 1)))
        self._commit(tok, reads, writes)
        return tok

    def dma(self, q, fn, reads=(), writes=(), extra=()):
        if self.skip:
            return None
        reads = tuple(reads)
        writes = tuple(writes)
        i = self.dnext[q]
        self.dnext[q] = (i + 1) % self.nd
        if self.dcnt[q][i] >= SEM_LIM // 16:
            self.dsem[q][i] = self._newsem()
            self.dcnt[q][i] = 0
        s = self.dsem[q][i]
        ex = list(extra)
        if self.dcnt[q][i] > 0:
            ex.append((s, 16 * self.dcnt[q][i]))
        waits = self._deps(q, reads, writes, ex)
        self.dcnt[q][i] += 1
        tok = (s, 16 * self.dcnt[q][i])
        self.ops[q].append((fn, waits, (s, 16)))
        self._commit(tok, reads, writes)
        return tok

    def all_tokens(self):
        toks = [(self.csem[e], self.ccount[e]) if self.ccount[e] > 0 else self.prev_tok[e]
                for e in ENGS if self.ccount[e] > 0 or e in self.prev_tok]
        for q in self.dsem:
            for i in range(self.nd):
                if self.dcnt[q][i] > 0:
                    toks.append((self.dsem[q][i], 16 * self.dcnt[q][i]))
        return toks

    def barrier(self):
        toks = self.all_tokens()
        for e in ENGS:
            waits = self._deps(e, (), (), toks)
            if waits:
                self.ops[e].append((None, waits, None))

    def emit(self):
        nc = self.nc
        engmap = {"pe": "tensor", "act": "scalar", "dve": "vector", "pool": "gpsimd", "sp": "sync"}
        with nc.Block() as block:
            for e in ENGS:
                lst = self.ops[e]
                if not lst:
                    continue

                def body(eobj, lst=lst):
                    for fn, waits, inc in lst:
                        for (s, v) in waits:
                            eobj.wait_ge(s, v)
                        if fn is not None:
                            fn(eobj).then_inc(inc[0], inc[1])
                getattr(block, engmap[e])(body)
        self.es.close()


class Ring:
    def __init__(self, p, name, n, shape, dt, es=None):
        self.t = [p.sb("%s%d" % (name, i), shape, dt, es) for i in range(n)]
        self.names = ["%s%d" % (name, i) for i in range(n)]
        self.i = 0

    def next(self):
        i = self.i
        self.i = (i + 1) % len(self.t)
        return self.t[i], self.names[i]


def _consts():
    c = {}
    c["ident"] = np.eye(128, dtype=np.float32)
    s = np.arange(128)[:, None]
    t = np.arange(128)[None, :]
    caus = np.where(s > t, NEG, 0.0).astype(np.float32)
    winn = np.where(s <= t, NEG, 0.0).astype(np.float32)
    c["causneg"] = np.tile(caus, (1, 4))
    c["winneg"] = np.tile(winn, (1, 4))
    la = np.zeros((2, 16, 128), np.float32)
    la[0] = (np.arange(128) - 127)[None, :]
    la[1] = (-128.0 * np.arange(16))[:, None]
    c["LAl"] = la
    rs = np.zeros((2, 2, 512), np.float32)
    for g in range(2):
        for r in range(4):
            rs[:, g, r * 128:(r + 1) * 128] = 2.0 ** (-(g * 4 + r + 1))
    rsm = np.zeros((10, 2, 512), np.float32)
    rsm[0:2] = rs
    lc = np.zeros((2, 16, 128), np.float32)
    lc[0] = (16.0 * np.arange(128) - 96.0)[None, :]
    lc[1] = (-128.0 * np.arange(16))[:, None]
    lcm = np.zeros((10, 16, 128), np.float32)
    lcm[0:2] = lc
    e = np.zeros((32, 16, 128), np.float32)
    for kt in range(16):
        for si in range(128):
            e[(kt * 128 + si) // 64, kt, si] = 1.0
    c["E"] = e
    lm = np.zeros((8, 16, 128), np.float32)
    for qb in range(16):
        for n in range(127):
            k = n - 8 * qb + 1
            if 0 <= k < 8:
                lm[k, qb, n] = 1.0
    lcm[2:10] = lm
    c["Lcm"] = lcm
    rm = np.zeros((8, 512), np.float32)
    for k in range(8):
        for tt in range(128):
            if 16 * (k - 1) + 31 > tt:
                rm[k, tt::128] = NEG
    rsm[2:10, 0, :] = rm
    rsm[2:10, 1, :] = rm
    c["Rsm"] = rsm
    n = np.arange(128)[:, None]
    j = np.arange(32)[None, :]
    ovl = ((16 * n < 64 * j + 64) & (16 * n + 32 > 64 * j)).astype(np.float32)
    ovl[127] = 0.0
    c["ovl"] = np.concatenate([ovl, np.ones((128, 1), np.float32)], 1)
    mul = np.zeros((128, 16, 32), np.float32)
    add = np.zeros((128, 16, 32), np.float32)
    for qb in range(16):
        for tt in range(128):
            bt = (qb * 128 + tt) // 64
            mul[tt, qb, :bt] = 1.0
            add[tt, qb, bt] = 1e30
            add[tt, qb, bt + 1:] = -1e30
    c["mulm"] = mul
    c["addm"] = add
    gam = [1.0 - 2.0 ** (-5.0 - h) for h in range(4)]
    pos = np.arange(128)
    dm = np.zeros((128, 4, 128), np.float32)
    for h in range(4):
        rel = pos[None, :] - pos[:, None]
        dm[:, h] = np.where(rel >= 0, 0.125 * gam[h] ** np.maximum(rel, 0), 0.0)
    c["dmask"] = dm
    qd = np.zeros((128, 2, 128), np.float32)
    for j2 in range(2):
        for hp in range(2):
            h = 2 * j2 + hp
            qd[64 * hp:64 * hp + 64, j2, :] = (0.125 * gam[h] ** (pos + 1.0))[None, :]
    c["qdec"] = qd
    kd = np.zeros((128, 4), np.float32)
    for h in range(4):
        kd[:, h] = gam[h] ** (127.0 - np.arange(128))
    c["kdec"] = kd
    cm = np.ones((128, 512), np.float32)
    cm[:, ::64] = 0.0
    c["cmask"] = cm
    p128 = np.arange(128)
    c["m01"] = ((p128[:, None] // 64 == p128[None, :] // 64) & (p128[:, None] <= p128[None, :])).astype(np.float32)
    hm = np.zeros((128, 2), np.float32)
    hm[:64, 0] = 1.0
    hm[64:, 1] = 1.0
    c["hmask"] = hm
    c["onesf"] = np.ones((128, 128), np.float32)
    bd = np.zeros((128, 128), np.float32)
    bd[:64, :64] = 1.0
    bd[64:, 64:] = 1.0
    c["onesbd"] = bd
    return c


CONST_SPEC = [
    ("ident", [128, 128], BF16), ("causneg", [128, 512], BF16), ("winneg", [128, 512], BF16),
    ("LAl", [2, 16, 128], BF16), ("Lcm", [10, 16, 128], BF16), ("E", [32, 16, 128], BF16), ("Rsm", [10, 2, 512], BF16),
    ("ovl", [128, 33], BF16), ("mulm", [128, 16, 32], BF16), ("addm", [128, 16, 32], BF16),
    ("dmask", [128, 4, 128], F32), ("qdec", [128, 2, 128], F32), ("kdec", [128, 4], F32),
    ("cmask", [128, 512], F32), ("m01", [128, 128], F32), ("hmask", [128, 2], F32), ("onesf", [128, 128], F32),
    ("onesbd", [128, 128], F32),
]

P_NG, P_AG, P_CW, P_CB, P_BRA, P_BRI, P_LAM, P_DG, P_MB, P_QG, P_KG, P_POS = 0, 8, 12, 28, 32, 36, 40, 44, 48, 80, 81, 82
NPAR = 114


def _params(inp, l):
    P = np.zeros((128, NPAR), np.float32)
    P[:, P_NG:P_NG + 8] = inp["norm_g"][l].reshape(8, 128).T
    P[:, P_AG:P_AG + 4] = inp["a_norm_g"][l].reshape(4, 128).T
    P[:, P_CW:P_CW + 16] = inp["c_conv_w"][l].reshape(4, 4, 128).transpose(2, 1, 0).reshape(128, 16)
    P[:, P_CB:P_CB + 4] = inp["c_conv_b"][l].reshape(4, 128).T
    P[:, P_BRA:P_BRA + 4] = inp["c_b_ra"][l].reshape(4, 128).T
    P[:, P_BRI:P_BRI + 4] = inp["c_b_ri"][l].reshape(4, 128).T
    P[:, P_LAM:P_LAM + 4] = inp["c_lambda"][l].reshape(4, 128).T
    P[:, P_DG:P_DG + 4] = inp["d_norm_g"][l].reshape(4, 128).T
    P[:, P_MB:P_MB + 32] = inp["merge_b"][l].reshape(4, 8, 128).transpose(2, 0, 1).reshape(128, 32)
    P[:, P_QG] = np.tile(inp["b_q_norm_g"][l], 2)
    P[:, P_KG] = np.tile(inp["b_k_norm_g"][l], 2)
    P[:, P_POS:P_POS + 32] = np.tile(inp["b_cmp_pos"][l].T, (2, 1))
    return P


def build_program(NL, NS, dbg=False):
    nc = bass.Bass("TRN2", target_bir_lowering=False)
    dr = {}

    def dten(name, shape, kind="ExternalInput"):
        dr[name] = nc.dram_tensor(name, list(shape), F32, kind=kind).ap()
        return dr[name]
    xT = dten("xT", [NS, D, T])
    outT = dten("outT", [NS, D, T], kind="ExternalOutput")
    w_in = dten("w_in", [NL, D, NIN])
    w_br = dten("w_branch", [NL, 4, W, D])
    w_out = dten("w_out", [NL, D, D])
    wck = dten("b_cmp_wk", [NL, 2048, 64])
    wcv = dten("b_cmp_wv", [NL, 2048, 64])
    wra = dten("c_w_ra", [NL, 8, 64, 64])
    wri = dten("c_w_ri", [NL, 8, 64, 64])
    par = dten("par", [NL, 128, NPAR])
    lbl = dten("lbl", [128, 16])
    lsel = dten("lsel", [128, NL * 4])
    cdr = {}
    for name, shape, _ in CONST_SPEC:
        cdr[name] = dten("c_" + name, shape)
    if dbg:
        dbgT = dten("dbgT", [NS, NL, 4, W, T], kind="ExternalOutput")
        dbg2 = dten("dbg2", [64, 128, 512], kind="ExternalOutput")

    p = Prog(nc)
    PS = [p.ps("psb%d" % i, [128, 512], F32) for i in range(8)]
    PSN = ["psb%d" % i for i in range(8)]

    cst = {}
    for name, shape, dt in CONST_SPEC:
        cst[name] = p.sb("k_" + name, shape, dt)
        q = "pool" if dt == BF16 else "sp"
        p.dma(q, lambda e, a=cst[name], b=cdr[name]: e.dma_start(out=a[:], in_=b), writes=["k_" + name])
    CN = lambda n: "k_" + n
    zer = p.sb("zer", [128, 512], BF16)
    fsc = p.sb("fsc", [128, 2], F32)
    p.op("pool", lambda e: e.memset(zer[:], 0.0), writes=["zer"])
    parT = p.sb("parT", [128, NL, NPAR], F32)
    for l in range(NL):
        p.dma("sp", lambda e, l=l: e.dma_start(out=parT[:, l, :], in_=par[l]), writes=["par"])
    xn = p.sb("xn", [128, 8, T], BF16)
    oT = p.sb("oT", [128, 4, 4, T], BF16)
    WB = Ring(p, "wb", 2, [128, 8, 512], BF16)

    drv = p.sb("drv", [128, NL, 24], F32)
    lbt = p.sb("lbt", [128, 16], F32)
    lst = p.sb("lst", [128, NL * 4], F32)
    lbe = p.sb("lbe", [128, 4, 4], F32)
    lbs = p.sb("lbs", [128, 4], F32)
    lbc = p.sb("lbc", [128, 4, 4], F32)
    tmp4 = p.sb("tmp4", [128, 4, 4], F32)
    p.dma("sp", lambda e: e.dma_start(out=lbt[:], in_=lbl), writes=["lbt"])
    p.dma("sp", lambda e: e.dma_start(out=lst[:], in_=lsel), writes=["lst"])
    lb3 = lbt[:].rearrange("p (h l) -> p h l", l=4)
    p.op("act", lambda e: e.activation(out=lbe[:], in_=lb3, func=AF.Exp), reads=["lbt"], writes=["lbe"])
    p.op("dve", lambda e: e.tensor_reduce(out=lbs[:], in_=lbe[:], axis=AX.X, op=ALU.add), reads=["lbe"], writes=["lbs"])
    p.op("dve", lambda e: e.reciprocal(out=lbs[:], in_=lbs[:]), reads=["lbs"], writes=["lbs"])
    p.op("dve", lambda e: e.tensor_tensor(out=lbe[:], in0=lbe[:], in1=lbs[:].unsqueeze(2).to_broadcast([128, 4, 4]),
                                          op=ALU.mult), reads=["lbe", "lbs"], writes=["lbe"])
    p.op("dve", lambda e: e.memset(lbc[:], 0.0), writes=["lbc"])
    for l in range(1, 4):
        p.op("dve", lambda e, l=l: e.tensor_tensor(out=lbc[:, :, l:l + 1], in0=lbc[:, :, l - 1:l], in1=lbe[:, :, l:l + 1],
                                                   op=ALU.add), reads=["lbc", "lbe"], writes=["lbc"])
    for l in range(NL):
        sel = lst[:, l * 4:(l + 1) * 4]
        p.op("dve", lambda e, sel=sel: e.tensor_tensor(out=tmp4[:], in0=lbc[:], in1=sel.unsqueeze(1).to_broadcast([128, 4, 4]),
                                                       op=ALU.mult), reads=["lbc", "lst"], writes=["tmp4"])
        p.op("dve", lambda e, l=l: e.tensor_reduce(out=drv[:, l, 0:4], in_=tmp4[:], axis=AX.X, op=ALU.add),
             reads=["tmp4"], writes=["drv"])
        p.op("dve", lambda e, l=l: e.tensor_scalar(out=drv[:, l, 4:8], in0=drv[:, l, 0:4], scalar1=-1.0, scalar2=1.0,
                                                   op0=ALU.mult, op1=ALU.add), reads=["drv"], writes=["drv"])
        p.op("act", lambda e, l=l: e.activation(out=drv[:, l, 8:12], in_=parT[:, l, P_LAM:P_LAM + 4], func=AF.Exp, scale=-1.0),
             reads=["par"], writes=["drv"])
        p.op("act", lambda e, l=l: e.activation(out=drv[:, l, 8:12], in_=drv[:, l, 8:12], func=AF.Ln, bias=1.0),
             reads=["drv"], writes=["drv"])
        p.op("dve", lambda e, l=l: e.tensor_scalar(out=drv[:, l, 12:16], in0=drv[:, l, 8:12], scalar1=-16.0, scalar2=None,
                                                   op0=ALU.mult), reads=["drv"], writes=["drv"])
        p.op("dve", lambda e, l=l: e.tensor_scalar(out=drv[:, l, 8:12], in0=drv[:, l, 8:12], scalar1=-8.0, scalar2=None,
                                                   op0=ALU.mult), reads=["drv"], writes=["drv"])
        p.op("dve", lambda e, l=l: e.tensor_scalar(out=drv[:, l, 16:17], in0=parT[:, l, P_QG:P_QG + 1], scalar1=0.125,
                                                   scalar2=None, op0=ALU.mult), reads=["par"], writes=["drv"])

    def DUMP(idx, ap, n, res):
        if dbg:
            DMA("sp", dbg2[idx, :, 0:n], ap, reads=[res], writes=["dbg2_%d" % idx])

    def wload(l, segs):
        wt, wn = WB.next()
        names = ["%ss%d" % (wn, k) for k in range(4)]
        pre = p.tokens(names)
        src = w_in[l].rearrange("(kc q) n -> q kc n", q=128)
        off = 0
        offs = []
        for k, (c0, n) in enumerate(segs):
            DMA("pool", wt[:, :, off:off + n], src[:, :, c0:c0 + n], writes=[names[k]], extra=pre)
            offs.append(off)
            off += n
        return wt, names[:len(segs)], offs

    def proj_fm(ps, psn, wt, wres, off, M, t0, n):
        for kc in range(8):
            p.op("pe", lambda e, kc=kc: e.matmul(ps[0:M, 0:n], lhsT=wt[:, kc, off:off + M], rhs=xn[:, kc, t0:t0 + n],
                                                 start=(kc == 0), stop=(kc == 7)),
                 reads=list(wres) + ["xn"], writes=[psn], chain=(kc > 0))

    def proj_tm(ps, psn, wt, wres, off, N, t0):
        for kc in range(8):
            p.op("pe", lambda e, kc=kc: e.matmul(ps[:, 0:N], lhsT=xn[:, kc, t0:t0 + 128], rhs=wt[:, kc, off:off + N],
                                                 start=(kc == 0), stop=(kc == 7)),
                 reads=list(wres) + ["xn"], writes=[psn], chain=(kc > 0))

    def OP(eng, meth, reads, writes, chain=False, extra=(), **kw):
        return p.op(eng, lambda e: getattr(e, meth)(**kw), reads=reads, writes=writes, chain=chain, extra=extra)

    def DMA(q, out, in_, reads=(), writes=(), extra=()):
        return p.dma(q, lambda e: e.dma_start(out=out, in_=in_), reads=reads, writes=writes, extra=extra)

    def ACT(out, in_, func, reads, writes, **kw):
        return p.op("act", lambda e: e.activation(out=out, in_=in_, func=func, **kw), reads=reads, writes=writes)

    def TT(out, in0, in1, op, reads, writes, eng="dve"):
        eng = POOLMAP if eng == "pool" else eng
        return p.op(eng, lambda e: e.tensor_tensor(out=out, in0=in0, in1=in1, op=op), reads=reads, writes=writes)

    def TS(out, in0, s1, s2, op0, op1, reads, writes, eng="dve"):
        eng = POOLMAP if eng == "pool" else eng
        if s2 is None:
            return p.op(eng, lambda e: e.tensor_scalar(out=out, in0=in0, scalar1=s1, scalar2=None, op0=op0),
                        reads=reads, writes=writes)
        return p.op(eng, lambda e: e.tensor_scalar(out=out, in0=in0, scalar1=s1, scalar2=s2, op0=op0, op1=op1),
                    reads=reads, writes=writes)

    def STT(out, in0, sc, in1, op0, op1, reads, writes):
        return p.op("dve", lambda e: e.scalar_tensor_tensor(out=out, in0=in0, scalar=sc, in1=in1, op0=op0, op1=op1),
                    reads=reads, writes=writes)

    def MM(out, lhsT, rhs, start, stop, reads, writes, chain=False, **kw):
        return p.op("pe", lambda e: e.matmul(out, lhsT=lhsT, rhs=rhs, start=start, stop=stop, **kw),
                    reads=reads, writes=writes, chain=chain)

    def RECIP(out, in_, reads, writes):
        return p.op("dve", lambda e: e.reciprocal(out=out, in_=in_), reads=reads, writes=writes)

    def COPY(eng, out, in_, reads, writes):
        if eng == "act":
            return ACT(out, in_, AF.Copy, reads, writes)
        return p.op(eng, lambda e: e.tensor_copy(out=out, in_=in_), reads=reads, writes=writes)


    for s in range(NS):
        RES = "res%d" % s
        for l in range(NL):
            src_x = xT if l == 0 else outT
            pl = lambda c0, n=1, l=l: parT[:, l, c0:c0 + n]

            p.skip = "0" not in PHASES
            p.barrier()
            with ExitStack() as es:
                XS = Ring(p, "xs", 2, [128, 8, 256], F32, es)
                SQ = Ring(p, "sq0", 2, [128, 8, 256], F32, es)
                RS = Ring(p, "rs0", 2, [128, 256], F32, es)
                for tb in range(8):
                    t0 = tb * 256
                    xs, xsn = XS.next()
                    sq, sqn = SQ.next()
                    rs, rsn = RS.next()
                    DMA("sp", xs[:], src_x[s].rearrange("(c q) t -> q c t", q=128)[:, :, t0:t0 + 256],
                        reads=[RES + "_%d_%d" % (c_, t0 // 512) for c_ in range(8)], writes=[xsn])
                    ACT(sq[:], xs[:], AF.Square, [xsn], [sqn])
                    ps, psn = PS[tb % 2], PSN[tb % 2]
                    for kc in range(8):
                        MM(ps[:, 0:256], cst["onesf"][:], sq[:, kc, :], kc == 0, kc == 7, [CN("onesf"), sqn], [psn], chain=kc > 0)
                    ACT(rs[:], ps[:, 0:256], AF.Sqrt, [psn], [rsn], scale=1.0 / D, bias=EPS)
                    RECIP(rs[:], rs[:], [rsn], [rsn])
                    TT(xs[:], xs[:], rs[:].unsqueeze(1).to_broadcast([128, 8, 256]), ALU.mult, [xsn, rsn], [xsn])
                    TT(xn[:, :, t0:t0 + 256], xs[:], pl(P_NG, 8).unsqueeze(2).to_broadcast([128, 8, 256]), ALU.mult,
                       [xsn, "par"], ["xn"], eng="pool")

            p.skip = "A" not in PHASES
            p.barrier()
            with ExitStack() as es:
                vtm = p.sb("a_vtm", [128, 16, 512], BF16, es)
                F_ = [p.sb("a_f%d" % i, [128, 512], F32, es) for i in range(6)]
                FN = ["a_f%d" % i for i in range(6)]
                qp = Ring(p, "a_qp", 2, [128, 512], BF16, es)
                kp = Ring(p, "a_kp", 2, [128, 512], BF16, es)
                ktm = Ring(p, "a_ktm", 2, [128, 4, 2, 128], BF16, es)
                scs = Ring(p, "a_sc", 2, [128, 4, 128], BF16, es)
                esc = Ring(p, "a_es", 2, [128, 3, 8], F32, es)
                S = p.sb("a_S", [128, 128], F32, es)
                Sp = Ring(p, "a_Sp", 2, [128, 128], BF16, es)
                tkv = p.sb("a_tkv", [128, 128], F32, es)
                E_ = [p.sb("a_e%d" % i, [128, 512], F32, es) for i in range(4)]
                EN = ["a_e%d" % i for i in range(4)]
                wt, wr, wo = wload(l, [(C_AI, 512)])
                for blk in range(16):
                    ps, psn = PS[blk % 2], PSN[blk % 2]
                    proj_tm(ps, psn, wt, wr, 0, 512, blk * 128)
                    COPY("act" if blk % 2 else "dve", vtm[:, blk, :], ps[:, 0:512], [psn], ["a_vtm"])
                p.skip = p.skip or ("a" in PHASES)
                for h in range(4):
                    wt, wr, wo = wload(l, [(C_AQ + 128 * h, 128), (C_AF + 128 * h, 128), (C_AZ + 128 * h, 128)])
                    lbh = drv[:, l, h:h + 1]
                    omh = drv[:, l, 4 + h:5 + h]
                    OP("dve", "memset", [], ["a_S"], ap=S[:], constant=0.0)
                    for tl in range(4):
                        t0 = tl * 512
                        proj_fm(PS[0], PSN[0], wt, wr, wo[1], 128, t0, 512)
                        ACT(F_[0][:], PS[0][:], AF.Sigmoid, [PSN[0]], [FN[0]])
                        TS(F_[0][:], F_[0][:], omh, lbh, ALU.mult, ALU.add, [FN[0], "drv"], [FN[0]])
                        ACT(F_[1][:], F_[0][:], AF.Ln, [FN[0]], [FN[1]])
                        TS(F_[0][:], F_[0][:], -1.0, 1.0, ALU.mult, ALU.add, [FN[0]], [FN[0]], eng="pool")
                        OP("dve", "tensor_tensor_scan", [CN("cmask"), FN[1]], [FN[2]], out=F_[2][:], data0=cst["cmask"][:],
                           data1=F_[1][:], initial=0.0, op0=ALU.mult, op1=ALU.add)
                        b3 = F_[2][:].rearrange("q (c j) -> q c j", j=64)
                        TT(F_[3][:].rearrange("q (c j) -> q c j", j=64), b3, b3[:, :, 31:32].to_broadcast([128, 8, 64]),
                           ALU.subtract, [FN[2]], [FN[3]])
                        ACT(F_[4][:], F_[3][:], AF.Exp, [FN[3]], [FN[4]])
                        ACT(F_[5][:], F_[3][:], AF.Exp, [FN[3]], [FN[5]], scale=-1.0)
                        es_t, es_n = esc.next()
                        ACT(es_t[:, 0, :], b3[:, :, 31], AF.Exp, [FN[2]], [es_n])
                        ACT(es_t[:, 1, :], b3[:, :, 63], AF.Exp, [FN[2]], [es_n])
                        COPY("dve", es_t[:, 2, :], F_[4][:].rearrange("q (c j) -> q c j", j=64)[:, :, 63], [FN[4]], [es_n])
                        if h == 0 and tl == 0 and s == 0 and l == 0:
                            DUMP(0, F_[2][:], 512, FN[2])
                            DUMP(1, es_t[:].rearrange("q a b -> q (a b)"), 24, es_n)
                            DUMP(2, F_[1][:], 512, FN[1])
                        proj_fm(PS[1], PSN[1], wt, wr, wo[0], 128, t0, 512)
                        qpt, qpn = qp.next()
                        kpt, kpn = kp.next()
                        TT(qpt[:], PS[1][:], F_[4][:], ALU.mult, [PSN[1], FN[4]], [qpn])
                        TT(kpt[:], F_[0][:], F_[5][:], ALU.mult, [FN[0], FN[5]], [kpn], eng="pool")
                        p.skip = p.skip or ("b" in PHASES)
                        ktt, ktn = ktm.next()
                        pst = PS[7][:].bitcast(BF16)
                        for bb in range(4):
                            OP("pe", "transpose", [kpn, CN("ident")], [PSN[7]], chain=bb > 0, out=pst[:, bb * 128:(bb + 1) * 128],
                               in_=kpt[:, bb * 128:(bb + 1) * 128], identity=cst["ident"][:])
                        if h == 0 and tl == 0 and s == 0 and l == 0 and dbg:
                            dgp = p.sb("a_dgp", [128, 512], F32, es)
                            COPY("dve", dgp[:], pst[:, 0:512], [PSN[7]], ["a_dgp"]); DUMP(10, dgp[:], 512, "a_dgp")
                        for hf in range(2):
                            TS(ktt[:, :, hf, :], pst[:, 0:512].rearrange("q (b d) -> q b d", d=128), cst["hmask"][:, hf:hf + 1], None,
                               ALU.mult, None, [PSN[7], CN("hmask")], [ktn])
                        p.skip = p.skip or ("c" in PHASES)
                        psS = PS[2][:, 0:512].rearrange("q (b j) -> q b j", j=128)
                        for bb in range(4):
                            MM(psS[:, bb, :], kpt[:, bb * 128:(bb + 1) * 128], qpt[:, bb * 128:(bb + 1) * 128],
                               True, True, [kpn, qpn], [PSN[2]], chain=bb > 0)
                        sct, scn = scs.next()
                        TT(sct[:], psS, cst["m01"][:].unsqueeze(1).to_broadcast([128, 4, 128]), ALU.mult,
                           [PSN[2], CN("m01")], [scn])
                        for c in range(8):
                            hf, bb = c % 2, c // 2
                            pk = PS[3 + c // 4]
                            MM(pk[:, (c % 4) * 128:(c % 4 + 1) * 128], ktt[:, bb, hf, :],
                               vtm[:, tl * 4 + bb, h * 128:(h + 1) * 128], True, True,
                               [ktn, "a_vtm"], [PSN[3 + c // 4]], chain=(c % 4) > 0)
                        p.skip = p.skip or ("d" in PHASES)
                        po, pon = PS[5 + (tl % 2)], PSN[5 + (tl % 2)]
                        for c in range(8):
                            hf, bb = c % 2, c // 2
                            spt, spn = Sp.next()
                            TS(spt[:], S[:], es_t[:, 0, c:c + 1], None, ALU.mult, None, ["a_S", es_n], [spn])
                            MM(po[:, c * 64:(c + 1) * 64], vtm[:, tl * 4 + bb, h * 128:(h + 1) * 128],
                               sct[:, bb, hf * 64:(hf + 1) * 64], True, False, ["a_vtm", scn], [pon], chain=c > 0)
                            MM(po[:, c * 64:(c + 1) * 64], spt[:], qpt[:, c * 64:(c + 1) * 64], False, True,
                               [spn, qpn], [pon], chain=True)
                            pk = PS[3 + c // 4]
                            TS(tkv[:], pk[:, (c % 4) * 128:(c % 4 + 1) * 128], es_t[:, 2, c:c + 1], None, ALU.mult, None,
                               [PSN[3 + c // 4], es_n], ["a_tkv"])
                            STT(S[:], S[:], es_t[:, 1, c:c + 1], tkv[:], ALU.mult, ALU.add, ["a_S", es_n, "a_tkv"], ["a_S"])
                        if h == 0 and tl == 0 and s == 0 and l == 0 and dbg:
                            DUMP(3, S[:], 128, "a_S")
                            DUMP(4, tkv[:], 128, "a_tkv")
                            dg = [p.sb("a_dg%d" % i, [128, 512], F32, es) for i in range(5)]
                            COPY("dve", dg[0][:], kpt[:], [kpn], ["a_dg0"]); DUMP(5, dg[0][:], 512, "a_dg0")
                            COPY("dve", dg[1][:, 0:256], ktt[:, 3, :, :].rearrange("q a d -> q (a d)"), [ktn], ["a_dg1"]); DUMP(6, dg[1][:, 0:256], 256, "a_dg1")
                            COPY("dve", dg[2][:], PS[4][:], [PSN[4]], ["a_dg2"]); DUMP(7, dg[2][:], 512, "a_dg2")
                            COPY("dve", dg[3][:], vtm[:, 3, 0:512], ["a_vtm"], ["a_dg3"]); DUMP(8, dg[3][:], 512, "a_dg3")
                            COPY("dve", dg[4][:], F_[0][:], [FN[0]], ["a_dg4"]); DUMP(9, dg[4][:], 512, FN[0])
                        p.skip = p.skip or ("e" in PHASES)
                        ACT(E_[0][:], po[:], AF.Square, [pon], [EN[0]])
                        MM(PS[7][:], cst["onesf"][:], E_[0][:], True, True, [CN("onesf"), EN[0]], [PSN[7]])
                        ACT(E_[1][:], PS[7][:], AF.Sqrt, [PSN[7]], [EN[1]], scale=1.0 / 128, bias=EPS)
                        RECIP(E_[1][:], E_[1][:], [EN[1]], [EN[1]])
                        STT(E_[2][:], po[:], pl(P_AG + h), E_[1][:], ALU.mult, ALU.mult, [pon, "par", EN[1]], [EN[2]])
                        proj_fm(PS[0], PSN[0], wt, wr, wo[2], 128, t0, 512)
                        ACT(E_[3][:], PS[0][:], AF.Silu, [PSN[0]], [EN[3]])
                        TT(oT[:, 0, h, t0:t0 + 512], E_[2][:], E_[3][:], ALU.mult, [EN[2], EN[3]], ["oT0"], eng="pool")

            p.skip = "D" not in PHASES
            p.barrier()
            with ExitStack() as es:
                vtm = p.sb("d_vtm", [128, 16, 512], BF16, es)
                kdt = p.sb("d_kdt", [128, 16, 256], BF16, es)
                qs = Ring(p, "d_qs", 2, [128, 512], BF16, es)
                qt = Ring(p, "d_qt", 2, [128, 2, 512], BF16, es)
                kk = Ring(p, "d_kk", 2, [128, 2, 512], BF16, es)
                scs = Ring(p, "d_sc", 2, [128, 4, 128], BF16, es)
                S2 = [p.sb("d_S%d" % i, [128, 128], F32, es) for i in range(2)]
                Sb = Ring(p, "d_Sb", 3, [128, 128], BF16, es)
                E_ = [p.sb("d_e%d" % i, [128, 512], F32, es) for i in range(6)]
                EN = ["d_e%d" % i for i in range(6)]
                gam128 = [float((1.0 - 2.0 ** (-5.0 - h)) ** 128) for h in range(4)]
                wt, wr, wo = wload(l, [(C_DV, 512)])
                for blk in range(16):
                    ps, psn = PS[blk % 2], PSN[blk % 2]
                    proj_tm(ps, psn, wt, wr, 0, 512, blk * 128)
                    COPY("act" if blk % 2 else "dve", vtm[:, blk, :], ps[:, 0:512], [psn], ["d_vtm"])
                wt, wr, wo = wload(l, [(C_DK, 256)])
                for blk in range(16):
                    ps, psn = PS[blk % 2], PSN[blk % 2]
                    proj_tm(ps, psn, wt, wr, 0, 256, blk * 128)
                    TT(kdt[:, blk, :].rearrange("q (h d) -> q h d", d=64), ps[:, 0:256].rearrange("q (h d) -> q h d", d=64),
                       cst["kdec"][:].unsqueeze(2).to_broadcast([128, 4, 64]), ALU.mult, [psn, CN("kdec")], ["d_kdt"])
                p.skip = p.skip or ("a" in PHASES)
                for j in range(2):
                    wt, wr, wo = wload(l, [(C_DQ + 128 * j, 128), (C_DK + 128 * j, 128), (C_DZ + 256 * j, 256)])
                    for hp in range(2):
                        OP("dve", "memset", [], ["d_S%d" % hp], ap=S2[hp][:], constant=0.0)
                    for tl in range(4):
                        t0 = tl * 512
                        proj_fm(PS[0], PSN[0], wt, wr, wo[0], 128, t0, 512)
                        qst, qsn = qs.next()
                        qtt, qtn = qt.next()
                        kkt, kkn = kk.next()
                        p.skip = p.skip or ("x" in PHASES)
                        COPY("act", qst[:], PS[0][:], [PSN[0]], [qsn])
                        p.skip = p.skip or ("y" in PHASES)
                        TT(E_[0][:].rearrange("q (c j) -> q c j", j=128), PS[0][:].rearrange("q (c j) -> q c j", j=128),
                           cst["qdec"][:, j, :].unsqueeze(1).to_broadcast([128, 4, 128]), ALU.mult, [PSN[0], CN("qdec")], [EN[0]])
                        p.skip = p.skip or ("w" in PHASES)
                        for hp in range(2):
                            TS(qtt[:, hp, :], E_[0][:], cst["hmask"][:, hp:hp + 1], None, ALU.mult, None, [EN[0], CN("hmask")], [qtn],
                               eng="pool")
                        p.skip = p.skip or ("z" in PHASES)
                        proj_fm(PS[1], PSN[1], wt, wr, wo[1], 128, t0, 512)
                        for hp in range(2):
                            TS(kkt[:, hp, :], PS[1][:], cst["hmask"][:, hp:hp + 1], None, ALU.mult, None, [PSN[1], CN("hmask")], [kkn])
                        p.skip = p.skip or ("b" in PHASES)
                        for hp in range(2):
                            h = 2 * j + hp
                            psS = PS[2][:, 0:512].rearrange("q (b j) -> q b j", j=128)
                            for bb in range(4):
                                MM(psS[:, bb, :], kkt[:, hp, bb * 128:(bb + 1) * 128], qst[:, bb * 128:(bb + 1) * 128], True, True,
                                   [kkn, qsn], [PSN[2]], chain=bb > 0)
                            sct, scn = scs.next()
                            TT(sct[:], psS, cst["dmask"][:, h, :].unsqueeze(1).to_broadcast([128, 4, 128]), ALU.mult,
                               [PSN[2], CN("dmask")], [scn])
                            pk, pkn = PS[3 + hp], PSN[3 + hp]
                            for bb in range(4):
                                MM(pk[:, bb * 128:(bb + 1) * 128], kdt[:, tl * 4 + bb, 128 * j:128 * j + 128],
                                   vtm[:, tl * 4 + bb, h * 128:(h + 1) * 128], True, True, ["d_kdt", "d_vtm"], [pkn], chain=bb > 0)
                            p.skip = p.skip or ("c" in PHASES)
                            po, pon = PS[5 + hp], PSN[5 + hp]
                            Sn = "d_S%d" % hp
                            for bb in range(4):
                                sbt, sbn = Sb.next()
                                COPY("act", sbt[:], S2[hp][:], [Sn], [sbn])
                                MM(po[:, bb * 128:(bb + 1) * 128], vtm[:, tl * 4 + bb, h * 128:(h + 1) * 128], sct[:, bb, :],
                                   True, False, ["d_vtm", scn], [pon], chain=bb > 0)
                                MM(po[:, bb * 128:(bb + 1) * 128], sbt[:], qtt[:, hp, bb * 128:(bb + 1) * 128], False, True,
                                   [sbn, qtn], [pon], chain=True)
                                STT(S2[hp][:], S2[hp][:], gam128[h], pk[:, bb * 128:(bb + 1) * 128], ALU.mult, ALU.add, [Sn, pkn], [Sn])
                            p.skip = p.skip or ("d" in PHASES)
                            COPY("act", E_[0][:], po[:], [pon], [EN[0]])
                            ACT(E_[1][:], po[:], AF.Square, [pon], [EN[1]])
                            MM(PS[7][:], cst["onesf"][:], E_[0][:], True, True, [CN("onesf"), EN[0]], [PSN[7]])
                            ACT(E_[2][:], PS[7][:], AF.Copy, [PSN[7]], [EN[2]], scale=1.0 / 128)
                            MM(PS[7][:], cst["onesf"][:], E_[1][:], True, True, [CN("onesf"), EN[1]], [PSN[7]])
                            TT(E_[3][:], E_[2][:], E_[2][:], ALU.mult, [EN[2]], [EN[3]], eng="pool")
                            STT(E_[3][:], PS[7][:], 1.0 / 128, E_[3][:], ALU.mult, ALU.subtract, [PSN[7], EN[3]], [EN[3]])
                            TS(E_[3][:], E_[3][:], 0.0, None, ALU.max, None, [EN[3]], [EN[3]])
                            ACT(E_[3][:], E_[3][:], AF.Sqrt, [EN[3]], [EN[3]], bias=EPS)
                            RECIP(E_[3][:], E_[3][:], [EN[3]], [EN[3]])
                            TT(E_[0][:], E_[0][:], E_[2][:], ALU.subtract, [EN[0], EN[2]], [EN[0]], eng="pool")
                            STT(E_[4][:], E_[0][:], pl(P_DG + h), E_[3][:], ALU.mult, ALU.mult, [EN[0], "par", EN[3]], [EN[4]])
                            proj_fm(PS[0], PSN[0], wt, wr, wo[2] + 128 * hp, 128, t0, 512)
                            ACT(E_[5][:], PS[0][:], AF.Silu, [PSN[0]], [EN[5]])
                            TT(oT[:, 3, h, t0:t0 + 512], E_[4][:], E_[5][:], ALU.mult, [EN[4], EN[5]], ["oT3"], eng="pool")

            p.skip = "C" not in PHASES
            p.barrier()
            with ExitStack() as es:
                wbd = p.sb("c_wbd", [128, 2, 4, 128], BF16, es)
                xr = [p.sb("c_xr%d" % i, [128, 515], F32, es) for i in range(2)]
                XRN = ["c_xr0", "c_xr1"]
                G_ = [p.sb("c_g%d" % i, [128, 512], F32, es) for i in range(7)]
                GN = ["c_g%d" % i for i in range(7)]
                xcb = p.sb("c_xcb", [128, 512], BF16, es)
                hh = [p.sb("c_h%d" % i, [128, 512], F32, es) for i in range(2)]
                HN = ["c_h0", "c_h1"]
                OP("pool", "memset", [], ["c_wbd"], ap=wbd[:], constant=0.0)
                for wi, wsrc in enumerate((wra, wri)):
                    for hb in range(2):
                        DMA("pool", wbd[64 * hb:64 * hb + 64, wi, :, 64 * hb:64 * hb + 64],
                            wsrc[l].rearrange("(j b) c d -> b c j d", b=2)[hb], reads=[], writes=["c_wbd"])
                for j in range(4):
                    wt, wr, wo = wload(l, [(C_CX + 128 * j, 128), (C_CZ + 128 * j, 128)])
                    for tl in range(4):
                        t0 = tl * 512
                        xa, xan = xr[tl % 2], XRN[tl % 2]
                        xb, xbn = xr[(tl + 1) % 2], XRN[(tl + 1) % 2]
                        if tl == 0:
                            OP("dve", "memset", [], [xan], ap=xa[:, 0:3], constant=0.0)
                        proj_fm(PS[0], PSN[0], wt, wr, wo[0], 128, t0, 512)
                        COPY("act", xa[:, 3:515], PS[0][:], [PSN[0]], [xan])
                        if tl < 3:
                            COPY("dve", xb[:, 0:3], xa[:, 512:515], [xan], [xbn])
                        cw = lambda k, j=j: pl(P_CW + 4 * j + k)
                        TS(G_[0][:], xa[:, 3:515], cw(3), pl(P_CB + j), ALU.mult, ALU.add, [xan, "par"], [GN[0]])
                        STT(G_[0][:], xa[:, 2:514], cw(2), G_[0][:], ALU.mult, ALU.add, [xan, "par", GN[0]], [GN[0]])
                        STT(G_[0][:], xa[:, 1:513], cw(1), G_[0][:], ALU.mult, ALU.add, [xan, "par", GN[0]], [GN[0]])
                        STT(G_[0][:], xa[:, 0:512], cw(0), G_[0][:], ALU.mult, ALU.add, [xan, "par", GN[0]], [GN[0]])
                        COPY("act", xcb[:], G_[0][:], [GN[0]], ["c_xcb"])
                        MM(PS[1][:], wbd[:, 0, j, :], xcb[:], True, True, ["c_wbd", "c_xcb"], [PSN[1]])
                        MM(PS[2][:], wbd[:, 1, j, :], xcb[:], True, True, ["c_wbd", "c_xcb"], [PSN[2]])
                        ACT(G_[1][:], PS[1][:], AF.Sigmoid, [PSN[1], "par"], [GN[1]], bias=pl(P_BRA + j))
                        ACT(G_[2][:], PS[2][:], AF.Sigmoid, [PSN[2], "par"], [GN[2]], bias=pl(P_BRI + j))
                        ACT(G_[3][:], G_[1][:], AF.Exp, [GN[1], "drv"], [GN[3]], scale=drv[:, l, 8 + j:9 + j])
                        ACT(G_[4][:], G_[1][:], AF.Exp, [GN[1], "drv"], [GN[4]], scale=drv[:, l, 12 + j:13 + j])
                        ACT(G_[4][:], G_[4][:], AF.Sqrt, [GN[4]], [GN[4]], scale=-1.0, bias=1.0)
                        TT(G_[2][:], G_[2][:], G_[0][:], ALU.mult, [GN[2], GN[0]], [GN[2]], eng="pool")
                        TT(G_[2][:], G_[2][:], G_[4][:], ALU.mult, [GN[2], GN[4]], [GN[2]])
                        ha, han = hh[tl % 2], HN[tl % 2]
                        hb_, hbn = hh[(tl + 1) % 2], HN[(tl + 1) % 2]
                        init = 0.0 if tl == 0 else hb_[:, 511:512]
                        OP("dve", "tensor_tensor_scan", [GN[3], GN[2], hbn], [han], out=ha[:], data0=G_[3][:], data1=G_[2][:],
                           initial=init, op0=ALU.mult, op1=ALU.add)
                        proj_fm(PS[3], PSN[3], wt, wr, wo[1], 128, t0, 512)
                        ACT(G_[5][:], PS[3][:], AF.Silu, [PSN[3]], [GN[5]])
                        TT(oT[:, 2, j, t0:t0 + 512], ha[:], G_[5][:], ALU.mult, [han, GN[5]], ["oT2"], eng="pool")

            p.skip = "B" not in PHASES
            p.barrier()
            with ExitStack() as es:
                ksT = p.sb("b_ksT", [128, T], BF16, es)
                kwT = p.sb("b_kwT", [128, T], BF16, es)
                vsa = p.sb("b_vsa", [128, 16, 2, 65], BF16, es)
                vwa = p.sb("b_vwa", [128, 16, 2, 65], BF16, es)
                gts = p.sb("b_gt", [128, 16, 24], F32, es)
                kcm = p.sb("b_kcm", [128, 128], BF16, es)
                Rc = p.sb("b_Rc", [128, 2, 97], BF16, es)
                B_ = [p.sb("b_t%d" % i, [128, 512], F32, es) for i in range(3)]
                BN = ["b_t%d" % i for i in range(3)]
                with ExitStack() as es2:
                    kcT = p.sb("b_kcT", [128, T], BF16, es2)
                    vcT = p.sb("b_vcT", [128, T], BF16, es2)
                    wkb = p.sb("b_wkb", [128, 32, 128], BF16, es2)
                    wvb = p.sb("b_wvb", [128, 32, 128], BF16, es2)
                    posb = p.sb("b_posb", [128, 32], BF16, es2)
                    cb = p.sb("b_cb", [128, 2], F32, es2)
                    raw = p.sb("b_raw", [128, 2, 128], F32, es2)
                    vrb = p.sb("b_vrb", [128, 128], BF16, es2)
                    OP("pool", "memset", [], ["b_wkb"], ap=wkb[:], constant=0.0)
                    OP("pool", "memset", [], ["b_wvb"], ap=wvb[:], constant=0.0)
                    for wtile, wn_, wsrc in ((wkb, "b_wkb", wck), (wvb, "b_wvb", wcv)):
                        for g in range(2):
                            for lh in range(2):
                                DMA("pool", wtile[64 * g:64 * g + 64, 16 * lh:16 * lh + 16, 64 * g:64 * g + 64],
                                    wsrc[l].rearrange("(a d) c -> d a c", d=64)[:, 16 * lh:16 * lh + 16, :], writes=[wn_])
                    COPY("dve", posb[:], pl(P_POS, 32), ["par"], ["b_posb"])
                    OP("pool", "memset", [], ["b_vsa"], ap=vsa[:, :, :, 64:65], constant=1.0)
                    OP("pool", "memset", [], ["b_vwa"], ap=vwa[:, :, :, 64:65], constant=1.0)
                    OP("pool", "memset", [], ["b_raw"], ap=raw[:], constant=0.0)
                    OP("pool", "memset", [], ["b_Rc"], ap=Rc[:], constant=0.0)
                    wt, wr, wo = wload(l, [(C_BKC, 128), (C_BVC, 128), (C_BKS, 128), (C_BKW, 128)])
                    for tl in range(4):
                        t0 = tl * 512
                        proj_fm(PS[0], PSN[0], wt, wr, wo[0], 128, t0, 512)
                        COPY("act", kcT[:, t0:t0 + 512], PS[0][:], [PSN[0]], ["b_kcT"])
                        proj_fm(PS[1], PSN[1], wt, wr, wo[1], 128, t0, 512)
                        COPY("dve", vcT[:, t0:t0 + 512], PS[1][:], [PSN[1]], ["b_vcT"])
                        for wi, (dst, dn) in enumerate(((ksT, "b_ksT"), (kwT, "b_kwT"))):
                            ps, psn = PS[2 + wi], PSN[2 + wi]
                            proj_fm(ps, psn, wt, wr, wo[2 + wi], 128, t0, 512)
                            ACT(B_[0][:], ps[:], AF.Square, [psn], [BN[0]])
                            MM(PS[7][:], cst["onesbd"][:], B_[0][:], True, True, [CN("onesbd"), BN[0]], [PSN[7]])
                            ACT(B_[1][:], PS[7][:], AF.Sqrt, [PSN[7]], [BN[1]], scale=1.0 / 64, bias=EPS)
                            RECIP(B_[1][:], B_[1][:], [BN[1]], [BN[1]])
                            STT(dst[:, t0:t0 + 512], ps[:], pl(P_KG), B_[1][:], ALU.mult, ALU.mult, [psn, "par", BN[1]], [dn])
                    wt, wr, wo = wload(l, [(C_BVS, 128), (C_BVW, 128), (C_BG, 24)])
                    for blk in range(16):
                        ps, psn = PS[blk % 2], PSN[blk % 2]
                        proj_tm(ps, psn, wt, wr, 0, 280, blk * 128)
                        COPY("dve", vsa[:, blk, :, 0:64], ps[:, 0:128].rearrange("q (g d) -> q g d", d=64), [psn], ["b_vsa"])
                        COPY("act", vwa[:, blk, :, 0:64], ps[:, 128:256].rearrange("q (g d) -> q g d", d=64), [psn], ["b_vwa"])
                        ACT(gts[:, blk, :], ps[:, 256:280], AF.Sigmoid, [psn], ["b_gt"])
                    for wi, (srcT, sn, wtile, wn_) in enumerate(((kcT, "b_kcT", wkb, "b_wkb"), (vcT, "b_vcT", wvb, "b_wvb"))):
                        ps, psn = PS[2 + wi], PSN[2 + wi]
                        for ll in range(32):
                            MM(ps[:, 0:127], wtile[:, ll, :], srcT[:].rearrange("q (n u) -> q n u", u=16)[:, (ll // 16):(ll // 16) + 127, ll % 16], ll == 0, ll == 31,
                               [wn_, sn], [psn], chain=ll > 0)
                        psb, psbn = PS[4 + wi], PSN[4 + wi]
                        for ll in range(32):
                            MM(psb[:, 0:1], wtile[:, ll, :], posb[:, ll:ll + 1], ll == 0, ll == 31, [wn_, "b_posb"], [psbn],
                               chain=ll > 0)
                        COPY("dve", cb[:, wi:wi + 1], psb[:, 0:1], [psbn], ["b_cb"])
                        ACT(raw[:, wi, 0:127], ps[:, 0:127], AF.Identity, [psn, "b_cb"], ["b_raw"], bias=cb[:, wi:wi + 1])
                    ACT(B_[0][:, 0:128], raw[:, 0, :], AF.Square, ["b_raw"], [BN[0]])
                    MM(PS[7][:, 0:128], cst["onesbd"][:], B_[0][:, 0:128], True, True, [CN("onesbd"), BN[0]], [PSN[7]])
                    ACT(B_[1][:, 0:128], PS[7][:, 0:128], AF.Sqrt, [PSN[7]], [BN[1]], scale=1.0 / 64, bias=EPS)
                    RECIP(B_[1][:, 0:128], B_[1][:, 0:128], [BN[1]], [BN[1]])
                    STT(kcm[:], raw[:, 0, :], pl(P_KG), B_[1][:, 0:128], ALU.mult, ALU.mult, ["b_raw", "par", BN[1]], ["b_kcm"])
                    COPY("dve", vrb[:], raw[:, 1, :], ["b_raw"], ["b_vrb"])
                    pst = PS[6][:].bitcast(BF16)
                    OP("pe", "transpose", ["b_vrb", CN("ident")], [PSN[6]], out=pst[:, 0:128], in_=vrb[:], identity=cst["ident"][:])
                    COPY("dve", Rc[:, :, 0:64], pst[:, 0:128].rearrange("q (g d) -> q g d", d=64), [PSN[6]], ["b_Rc"])
                    for g in range(2):
                        COPY("dve", Rc[:, g, 64:97], cst["ovl"][:], [CN("ovl")], ["b_Rc"])
                    p.barrier()
                qG = p.sb("b_qG", [128, 4, 512], BF16, es)
                qGz = p.sb("b_qGz", [128, 2, 4, 512], BF16, es)
                szT = p.sb("b_sz", [128, 4, 512], BF16, es)
                nsT = p.sb("b_nsT", [32, 4, 128], BF16, es)
                ET = Ring(p, "b_e", 3, [128, 512], BF16, es)
                acc = p.sb("b_acc", [128, 8, 64], F32, es)
                accb = p.sb("b_accb", [128, 512], BF16, es)
                sm = [p.sb("b_sm%d" % i, [128, 4], F32, es) for i in range(3)]
                impw = p.sb("b_impw", [128, 4, 32], F32, es)
                imp = p.sb("b_imp", [128, 32], F32, es)
                mx8 = p.sb("b_mx8", [128, 8], F32, es)
                nsb = p.sb("b_nsb", [128, 32], BF16, es)
                tmpo = p.sb("b_tmpo", [128, 4, 64], F32, es)
                sbank = [2, 3]
                sidx = [0]

                def next_s():
                    i = sbank[sidx[0] % 2]
                    sidx[0] += 1
                    return PS[i], PSN[i]
                for qb in range(16):
                    tl, ql = qb // 4, qb % 4
                    if ql == 0:
                        t0 = tl * 512
                        segs = []
                        for m in range(4):
                            segs += [(C_BQ + 64 * m, 64), (C_BQ + 64 * (4 + m), 64)]
                        wt, wr, wo = wload(l, [(C_BQ, 512)])
                        wtq, wrq = wt, wr
                        wt2, wr2, wo2 = wload(l, [(C_BZ, 512)])
                        for m in range(4):
                            ps, psn = PS[m % 2], PSN[m % 2]
                            for kc in range(8):
                                MM(ps[0:64, 0:512], wtq[:, kc, 64 * m:64 * m + 64], xn[:, kc, t0:t0 + 512], kc == 0, kc == 7,
                                   list(wrq) + ["xn"], [psn], chain=kc > 0)
                            for kc in range(8):
                                MM(ps[64:128, 0:512], wtq[:, kc, 64 * (4 + m):64 * (4 + m) + 64], xn[:, kc, t0:t0 + 512], kc == 0, kc == 7,
                                   list(wrq) + ["xn"], [psn], chain=True)
                            ACT(B_[0][:], ps[:], AF.Square, [psn], [BN[0]])
                            MM(PS[7][:], cst["onesbd"][:], B_[0][:], True, True, [CN("onesbd"), BN[0]], [PSN[7]])
                            ACT(B_[1][:], PS[7][:], AF.Sqrt, [PSN[7]], [BN[1]], scale=1.0 / 64, bias=EPS)
                            RECIP(B_[1][:], B_[1][:], [BN[1]], [BN[1]])
                            STT(qG[:, m, :], ps[:], drv[:, l, 16:17], B_[1][:], ALU.mult, ALU.mult, [psn, "drv", BN[1]], ["b_qG"])
                            for g in range(2):
                                TS(qGz[:, g, m, :], qG[:, m, :], cst["hmask"][:, g:g + 1], None, ALU.mult, None,
                                   ["b_qG", CN("hmask")], ["b_qGz"], eng="pool")
                        for jj in range(4):
                            ps, psn = PS[jj % 2], PSN[jj % 2]
                            proj_fm(ps, psn, wt2, wr2, 128 * jj, 128, t0, 512)
                            ACT(szT[:, jj, :], ps[:], AF.Silu, [psn], ["b_sz"])
                    gq = gts[:, qb, :].rearrange("q (h c) -> q h c", c=3)
                    for g in range(2):
                        gb = 64 * g
                        qrhs = qGz[:, g, :, ql * 128:(ql + 1) * 128]
                        rsl = cst["Rsm"][0:2, g, :]
                        nk = min(127, 8 * qb + 7)
                        ps, psn = next_s()
                        MM(ps[0:nk, :], kcm[:, 0:nk], qrhs, True, False, ["b_kcm", "b_qGz"], [psn])
                        MM(ps[0:nk, :], cst["Lcm"][0:10, qb, 0:nk], cst["Rsm"][0:10, g, :], False, True, [CN("Lcm"), CN("Rsm")], [psn], chain=True)
                        et, etn = ET.next()
                        ACT(et[0:nk, :], ps[0:nk, :], AF.Exp, [psn], [etn])
                        poc = PS[6][:, 0:388].rearrange("q (r c) -> q r c", c=97)
                        for r in range(4):
                            MM(poc[:, r, :], et[0:nk, r * 128:(r + 1) * 128], Rc[0:nk, g, :], True, True, [etn, "b_Rc"], [PSN[6]],
                               chain=r > 0)
                        TS(sm[0][:], poc[:, :, 96], 1e-30, None, ALU.max, None, [PSN[6]], ["b_sm0"])
                        RECIP(sm[0][:], sm[0][:], ["b_sm0"], ["b_sm0"])
                        TT(impw[:], poc[:, :, 64:96], sm[0][:].unsqueeze(2).to_broadcast([128, 4, 32]), ALU.mult,
                           [PSN[6], "b_sm0"], ["b_impw"])
                        OP("dve", "tensor_reduce", ["b_impw"], ["b_imp"], out=imp[:], in_=impw[:].rearrange("q r j -> q j r"),
                           axis=AX.X, op=ALU.add)
                        TT(sm[0][:], sm[0][:], gq[:, 4 * g:4 * g + 4, 0], ALU.mult, ["b_sm0", "b_gt"], ["b_sm0"])
                        TT(acc[:, 4 * g:4 * g + 4, :], poc[:, :, 0:64], sm[0][:].unsqueeze(2).to_broadcast([128, 4, 64]), ALU.mult,
                           [PSN[6], "b_sm0"], ["b_acc"])
                        TT(imp[:], imp[:], cst["mulm"][:, qb, :], ALU.mult, ["b_imp", CN("mulm")], ["b_imp"])
                        TT(imp[:], imp[:], cst["addm"][:, qb, :], ALU.add, ["b_imp", CN("addm")], ["b_imp"])
                        OP("dve", "max", ["b_imp"], ["b_mx8"], out=mx8[:], in_=imp[:])
                        TS(imp[:], imp[:], mx8[:, 7:8], -NEG, ALU.is_ge, ALU.mult, ["b_imp", "b_mx8"], ["b_imp"])
                        TS(nsb[:], imp[:], NEG, None, ALU.add, None, ["b_imp"], ["b_nsb"])
                        pst = PS[7][:].bitcast(BF16)
                        OP("pe", "transpose", ["b_nsb", CN("ident")], [PSN[7]], out=pst[0:32, 0:128], in_=nsb[:], identity=cst["ident"][:])
                        for r in range(4):
                            COPY("act" if r % 2 else "dve", nsT[:, r, :], pst[0:32, 0:128], [PSN[7]], ["b_nsT"])
                        pos_ = PS[4][:, 0:260].rearrange("q (r c) -> q r c", c=65)
                        MM(PS[4][:, 0:260], zer[0:1, 0:128], zer[0:1, 0:260], True, False, ["zer"], [PSN[4]], skip_group_check=True)
                        for kt in range(qb + 1):
                            ps, psn = next_s()
                            MM(ps[:], ksT[:, kt * 128:(kt + 1) * 128], qrhs, True, False, ["b_ksT", "b_qGz"], [psn])
                            MM(ps[:], cst["LAl"][0:2, qb - kt, :], rsl, False, False, [CN("LAl"), CN("Rsm")], [psn], chain=True)
                            MM(ps[:], cst["E"][0:32, kt, :], nsT[:].rearrange("q r t -> q (r t)"), False, kt < qb,
                               [CN("E"), "b_nsT"], [psn], chain=True)
                            if kt == qb:
                                MM(ps[:], cst["ident"][:], cst["causneg"][:], False, True, [CN("ident"), CN("causneg")], [psn],
                                   chain=True)
                            et, etn = ET.next()
                            ACT(et[:], ps[:], AF.Exp, [psn], [etn])
                            for r in range(4):
                                MM(pos_[:, r, :], et[:, r * 128:(r + 1) * 128], vsa[:, kt, g, :], False, kt == qb,
                                   [etn, "b_vsa"], [PSN[4]], chain=True, skip_group_check=True)
                        pow_ = PS[5][:, 0:260].rearrange("q (r c) -> q r c", c=65)
                        MM(PS[5][:, 0:260], zer[0:1, 0:128], zer[0:1, 0:260], True, False, ["zer"], [PSN[5]], skip_group_check=True)
                        for kt in range(max(0, qb - 2), qb + 1):
                            ps, psn = next_s()
                            last_plain = (kt == qb - 1)
                            MM(ps[:], kwT[:, kt * 128:(kt + 1) * 128], qrhs, True, False, ["b_kwT", "b_qGz"], [psn])
                            MM(ps[:], cst["LAl"][0:2, qb - kt, :], rsl, False, last_plain, [CN("LAl"), CN("Rsm")], [psn], chain=True)
                            if kt == qb:
                                MM(ps[:], cst["ident"][:], cst["causneg"][:], False, True, [CN("ident"), CN("causneg")], [psn],
                                   chain=True)
                            elif kt == qb - 2:
                                MM(ps[:], cst["ident"][:], cst["winneg"][:], False, True, [CN("ident"), CN("winneg")], [psn],
                                   chain=True)
                            et, etn = ET.next()
                            ACT(et[:], ps[:], AF.Exp, [psn], [etn])
                            for r in range(4):
                                MM(pow_[:, r, :], et[:, r * 128:(r + 1) * 128], vwa[:, kt, g, :], False, kt == qb,
                                   [etn, "b_vwa"], [PSN[5]], chain=True, skip_group_check=True)
                        for bi, (pso, pson) in enumerate(((pos_, PSN[4]), (pow_, PSN[5]))):
                            smt, smn = sm[1 + bi], "b_sm%d" % (1 + bi)
                            RECIP(smt[:], pso[:, :, 64], [pson], [smn])
                            TT(smt[:], smt[:], gq[:, 4 * g:4 * g + 4, 1 + bi], ALU.mult, [smn, "b_gt"], [smn])
                            TT(tmpo[:], pso[:, :, 0:64], smt[:].unsqueeze(2).to_broadcast([128, 4, 64]), ALU.mult,
                               [pson, smn], ["b_tmpo"])
                            TT(acc[:, 4 * g:4 * g + 4, :], acc[:, 4 * g:4 * g + 4, :], tmpo[:], ALU.add, ["b_acc", "b_tmpo"],
                               ["b_acc"], eng="pool")
                    COPY("act", accb[:], acc[:].rearrange("q h d -> q (h d)"), ["b_acc"], ["b_accb"])
                    pst = PS[7][:].bitcast(BF16)
                    for jj in range(4):
                        OP("pe", "transpose", ["b_accb", CN("ident")], [PSN[7]], chain=jj > 0, out=pst[:, jj * 128:(jj + 1) * 128],
                           in_=accb[:, jj * 128:(jj + 1) * 128], identity=cst["ident"][:])
                    TT(oT[:, 1, :, qb * 128:(qb + 1) * 128], pst[:, 0:512].rearrange("q (j t) -> q j t", t=128),
                       szT[:, :, ql * 128:(ql + 1) * 128], ALU.mult, [PSN[7], "b_sz"], ["oT1"])

            p.skip = False
            if dbg:
                for br in range(4):
                    if "ABCD"[br] not in PHASES or any(ch in PHASES for ch in "abcde"):
                        continue
                    DMA("pool", dbgT[s, l, br].rearrange("(j q) t -> q j t", q=128), oT[:, br, :, :],
                        reads=["oT%d" % br], writes=["dbg"])

            p.skip = "M" not in PHASES
            p.barrier()
            with ExitStack() as es:
                mg = p.sb("m_mg", [128, 8, T], BF16, es)
                macc = p.sb("m_acc", [128, T], F32, es)
                GT = Ring(p, "m_g", 2, [128, 512], F32, es)
                TM = Ring(p, "m_t", 2, [128, 512], F32, es)
                WBR = Ring(p, "m_wb", 2, [128, 4, 128], BF16, es)
                WO = Ring(p, "m_wo", 2, [128, 8, 128], BF16, es)
                XR = Ring(p, "m_xr", 2, [128, 512], F32, es)
                for c in range(8):
                    segs = [(C_MG + br * 1024 + c * 128, 128) for br in range(4)]
                    wt, wr, wo = wload(l, segs)
                    for br in range(4):
                        wbt, wbn = WBR.next()
                        DMA("pool", wbt[:], w_br[l, br].rearrange("(k q) n -> q k n", q=128)[:, :, c * 128:(c + 1) * 128],
                            writes=[wbn])
                        for tl in range(4):
                            t0 = tl * 512
                            psP, psPn = PS[(2 * tl) % 8], PSN[(2 * tl) % 8]
                            psG, psGn = PS[(2 * tl + 1) % 8], PSN[(2 * tl + 1) % 8]
                            for k in range(4):
                                MM(psP[:], wbt[:, k, :], oT[:, br, k, t0:t0 + 512], k == 0, k == 3, [wbn, "oT%d" % br], [psPn],
                                   chain=k > 0)
                            proj_fm(psG, psGn, wt, wr, wo[br], 128, t0, 512)
                            gt_, gtn = GT.next()
                            ACT(gt_[:], psG[:], AF.Sigmoid, [psGn, "par"], [gtn], bias=pl(P_MB + br * 8 + c))
                            if br == 0:
                                TT(macc[:, t0:t0 + 512], gt_[:], psP[:], ALU.mult, [gtn, psPn], ["m_acc%d" % tl])
                            else:
                                tm_, tmn = TM.next()
                                TT(tm_[:], gt_[:], psP[:], ALU.mult, [gtn, psPn], [tmn])
                                if br < 3:
                                    TT(macc[:, t0:t0 + 512], macc[:, t0:t0 + 512], tm_[:], ALU.add, ["m_acc%d" % tl, tmn],
                                       ["m_acc%d" % tl], eng="pool")
                                else:
                                    TT(mg[:, c, t0:t0 + 512], macc[:, t0:t0 + 512], tm_[:], ALU.add, ["m_acc%d" % tl, tmn],
                                       ["m_mg"], eng="pool")
                for c2 in range(8):
                    wot, won = WO.next()
                    DMA("pool", wot[:], w_out[l].rearrange("(k q) n -> q k n", q=128)[:, :, c2 * 128:(c2 + 1) * 128],
                        writes=[won])
                    for tl in range(4):
                        t0 = tl * 512
                        ps, psn = PS[(c2 * 4 + tl) % 8], PSN[(c2 * 4 + tl) % 8]
                        for k in range(8):
                            MM(ps[:], wot[:, k, :], mg[:, k, t0:t0 + 512], k == 0, k == 7, [won, "m_mg"], [psn], chain=k > 0)
                        xr_, xrn = XR.next()
                        DMA("sp", xr_[:], src_x[s, c2 * 128:(c2 + 1) * 128, t0:t0 + 512], reads=[RES + "_%d_%d" % (c2, tl)], writes=[xrn])
                        TT(xr_[:], xr_[:], ps[:], ALU.add, [xrn, psn], [xrn])
                        DMA("sp", outT[s, c2 * 128:(c2 + 1) * 128, t0:t0 + 512], xr_[:], reads=[xrn], writes=[RES + "_%d_%d" % (c2, tl)])
            p.barrier()

    toks = p.all_tokens()
    waits = p._deps("sp", (), (), toks)
    p.ops["sp"].append((None, waits, None))
    p.emit()
    return nc


_PROG_CACHE = {}


def _get_prog(NL, NS, dbg=False):
    key = (NL, NS, dbg)
    if key not in _PROG_CACHE:
        _PROG_CACHE[key] = build_program(NL, NS, dbg)
    return _PROG_CACHE[key]


def _common_maps(inp, layers):
    NL = len(layers)
    m = {}
    for k in ("w_in", "w_branch", "w_out", "b_cmp_wk", "b_cmp_wv", "c_w_ra", "c_w_ri"):
        m[k] = np.ascontiguousarray(np.asarray(inp[k], np.float32)[layers])
    m["par"] = np.stack([_params(inp, l) for l in layers], 0)
    m["lbl"] = np.ascontiguousarray(np.asarray(inp["lb_logits"], np.float32).reshape(4, 4, 128).transpose(2, 1, 0).reshape(128, 16))
    ls = np.zeros((128, NL, 4), np.float32)
    for i, l in enumerate(layers):
        ls[:, i, l] = 1.0
    m["lsel"] = ls.reshape(128, NL * 4)
    for k, v in _consts().items():
        m["c_" + k] = v
    return m


MODE = "fused"
PHASES = "0ADCBM"
POOLMAP = "dve"
NCORES = 8


def kernel(**inputs):
    inp = {k: np.asarray(v) for k, v in inputs.items()}
    x = np.asarray(inp["x"], np.float32)
    B = x.shape[0]
    NSC = B // NCORES
    xTh = np.ascontiguousarray(x.transpose(0, 2, 1))
    if MODE == "fused":
        nc = _get_prog(4, NSC)
        cm = _common_maps(inp, [0, 1, 2, 3])
        maps = [dict(cm, xT=xTh[c * NSC:(c + 1) * NSC]) for c in range(NCORES)]
        res = run_bass_kernel_spmd(nc, maps, core_ids=list(range(NCORES)))
        oTh = np.concatenate([r["outT"] for r in res.results], 0)
    elif MODE == "layer":
        nc = _get_prog(1, NSC)
        cur = xTh
        for l in range(4):
            cm = _common_maps(inp, [l])
            maps = [dict(cm, xT=cur[c * NSC:(c + 1) * NSC]) for c in range(NCORES)]
            res = run_bass_kernel_spmd(nc, maps, core_ids=list(range(NCORES)))
            cur = np.concatenate([r["outT"] for r in res.results], 0)
        oTh = cur
    else:
        nc = _get_prog(4, 1)
        cm = _common_maps(inp, [0, 1, 2, 3])
        outs = []
        for g in range(NSC):
            maps = [dict(cm, xT=xTh[c * NSC + g:c * NSC + g + 1]) for c in range(NCORES)]
            res = run_bass_kernel_spmd(nc, maps, core_ids=list(range(NCORES)))
            outs.append([r["outT"] for r in res.results])
        oTh = np.concatenate([outs[g][c] for c in range(NCORES) for g in range(NSC)], 0)
    return np.ascontiguousarray(oTh.transpose(0, 2, 1)).astype(np.float32)
```

```python
import numpy as np
import concourse.bass as bass
import concourse.mybir as mybir
from concourse.bass_utils import run_bass_kernel_spmd
from contextlib import ExitStack

F32 = mybir.dt.float32
BF16 = mybir.dt.bfloat16
AF = mybir.ActivationFunctionType
ALU = mybir.AluOpType
AX = mybir.AxisListType

D = 1024
T = 2048
NIN = 10520
W = 512
EPS = 1e-6
NEG = -30000.0
ENGS = ("pe", "act", "dve", "pool", "sp")
SEM_LIM = 30000

C_AQ, C_AF, C_AI, C_AZ = 0, 512, 1024, 1536
C_BQ, C_BKC, C_BVC, C_BKS, C_BVS, C_BKW, C_BVW, C_BG, C_BZ = 2048, 2560, 2688, 2816, 2944, 3072, 3200, 3328, 3352
C_CX, C_CZ = 3864, 4376
C_DQ, C_DK, C_DV, C_DZ = 4888, 5144, 5400, 5912
C_MG = 6424


class Prog:
    def __init__(self, nc, n_dma_sems=8):
        self.nc = nc
        self.es = ExitStack()
        self.ops = {e: [] for e in ENGS}
        self.nsem = 0
        self.csem = {e: self._newsem() for e in ENGS}
        self.ccount = {e: 0 for e in ENGS}
        self.nd = n_dma_sems
        self.dsem = {q: [self._newsem() for _ in range(n_dma_sems)] for q in ("sp", "pool", "act")}
        self.dcnt = {q: [0] * n_dma_sems for q in ("sp", "pool", "act")}
        self.dnext = {q: 0 for q in ("sp", "pool", "act")}
        self.last_w = {}
        self.readers = {}
        self.waited = {e: {} for e in ENGS}
        self.skip = False
        self.prev_tok = {}

    def _newsem(self):
        self.nsem += 1
        return self.es.enter_context(self.nc.semaphore("s%d" % self.nsem))

    def sb(self, name, shape, dt, es=None):
        self.nalloc = getattr(self, "nalloc", 0) + 1
        return (es or self.es).enter_context(self.nc.sbuf_tensor("%s_%d" % (name, self.nalloc), list(shape), dt))

    def ps(self, name, shape, dt):
        return self.es.enter_context(self.nc.psum_tensor(name, list(shape), dt))

    def tokens(self, names):
        out = []
        for n in names:
            t = self.last_w.get(n)
            if t is not None:
                out.append(t)
            out.extend(self.readers.get(n, ()))
        return out

    def _deps(self, eng, reads, writes, extra):
        need = {}

        def add(tok):
            s, v = tok
            k = id(s)
            if k not in need or need[k][1] < v:
                need[k] = (s, v)
        for r in reads:
            t = self.last_w.get(r)
            if t is not None:
                add(t)
        for w in writes:
            t = self.last_w.get(w)
            if t is not None:
                add(t)
            for t in self.readers.get(w, ()):
                add(t)
        for t in extra:
            add(t)
        out = []
        wd = self.waited[eng]
        for k, (s, v) in need.items():
            if wd.get(k, 0) >= v:
                continue
            wd[k] = v
            out.append((s, v))
        return out

    def _commit(self, tok, reads, writes):
        for r in reads:
            lst = self.readers.setdefault(r, [])
            lst[:] = [t for t in lst if t[0] is not tok[0]]
            lst.append(tok)
        for w in writes:
            self.last_w[w] = tok
            self.readers[w] = []

    def op(self, eng, fn, reads=(), writes=(), chain=False, extra=()):
        if self.skip:
            return None
        reads = tuple(reads)
        writes = tuple(writes) + tuple(r for r in reads if r.startswith("psb"))
        waits = self._deps(eng, reads, () if chain else writes, extra)
        if self.ccount[eng] >= SEM_LIM:
            self.prev_tok[eng] = (self.csem[eng], self.ccount[eng])
            self.csem[eng] = self._newsem()
            self.ccount[eng] = 0
        self.ccount[eng] += 1
        tok = (self.csem[eng], self.ccount[eng])
        self.ops[eng].append((fn, waits, (self.csem[eng], 1)))
        self._commit(tok, reads, writes)
        return tok

    def dma(self, q, fn, reads=(), writes=(), extra=()):
        if self.skip:
            return None
        reads = tuple(reads)
        writes = tuple(writes)
        i = self.dnext[q]
        self.dnext[q] = (i + 1) % self.nd
        if self.dcnt[q][i] >= SEM_LIM // 16:
            self.dsem[q][i] = self._newsem()
            self.dcnt[q][i] = 0
        s = self.dsem[q][i]
        ex = list(extra)
        if self.dcnt[q][i] > 0:
            ex.append((s, 16 * self.dcnt[q][i]))
        waits = self._deps(q, reads, writes, ex)
        self.dcnt[q][i] += 1
        tok = (s, 16 * self.dcnt[q][i])
        self.ops[q].append((fn, waits, (s, 16)))
        self._commit(tok, reads, writes)
        return tok

    def all_tokens(self):
        toks = [(self.csem[e], self.ccount[e]) if self.ccount[e] > 0 else self.prev_tok[e]
                for e in ENGS if self.ccount[e] > 0 or e in self.prev_tok]
        for q in self.dsem:
            for i in range(self.nd):
                if self.dcnt[q][i] > 0:
                    toks.append((self.dsem[q][i], 16 * self.dcnt[q][i]))
        return toks

    def barrier(self):
        toks = self.all_tokens()
        for e in ENGS:
            waits = self._deps(e, (), (), toks)
            if waits:
                self.ops[e].append((None, waits, None))

    def emit(self):
        nc = self.nc
        engmap = {"pe": "tensor", "act": "scalar", "dve": "vector", "pool": "gpsimd", "sp": "sync"}
        with nc.Block() as block:
            for e in ENGS:
                lst = self.ops[e]
                if not lst:
                    continue

                def body(eobj, lst=lst):
                    for fn, waits, inc in lst:
                        for (s, v) in waits:
                            eobj.wait_ge(s, v)
                        if fn is not None:
                            fn(eobj).then_inc(inc[0], inc[1])
                getattr(block, engmap[e])(body)
        self.es.close()


class Ring:
    def __init__(self, p, name, n, shape, dt, es=None):
        self.t = [p.sb("%s%d" % (name, i), shape, dt, es) for i in range(n)]
        self.names = ["%s%d" % (name, i) for i in range(n)]
        self.i = 0

    def next(self):
        i = self.i
        self.i = (i + 1) % len(self.t)
        return self.t[i], self.names[i]


def _consts():
    c = {}
    c["ident"] = np.eye(128, dtype=np.float32)
    s = np.arange(128)[:, None]
    t = np.arange(128)[None, :]
    caus = np.where(s > t, NEG, 0.0).astype(np.float32)
    winn = np.where(s <= t, NEG, 0.0).astype(np.float32)
    c["causneg"] = np.tile(caus, (1, 4))
    c["winneg"] = np.tile(winn, (1, 4))
    la = np.zeros((2, 16, 128), np.float32)
    la[0] = (np.arange(128) - 127)[None, :]
    la[1] = (-128.0 * np.arange(16))[:, None]
    c["LAl"] = la
    rs = np.zeros((2, 2, 512), np.float32)
    for g in range(2):
        for r in range(4):
            rs[:, g, r * 128:(r + 1) * 128] = 2.0 ** (-(g * 4 + r + 1))
    rsm = np.zeros((10, 2, 512), np.float32)
    rsm[0:2] = rs
    lc = np.zeros((2, 16, 128), np.float32)
    lc[0] = (16.0 * np.arange(128) - 96.0)[None, :]
    lc[1] = (-128.0 * np.arange(16))[:, None]
    lcm = np.zeros((10, 16, 128), np.float32)
    lcm[0:2] = lc
    e = np.zeros((32, 16, 128), np.float32)
    for kt in range(16):
        for si in range(128):
            e[(kt * 128 + si) // 64, kt, si] = 1.0
    c["E"] = e
    lm = np.zeros((8, 16, 128), np.float32)
    for qb in range(16):
        for n in range(127):
            k = n - 8 * qb + 1
            if 0 <= k < 8:
                lm[k, qb, n] = 1.0
    lcm[2:10] = lm
    c["Lcm"] = lcm
    rm = np.zeros((8, 512), np.float32)
    for k in range(8):
        for tt in range(128):
            if 16 * (k - 1) + 31 > tt:
                rm[k, tt::128] = NEG
    rsm[2:10, 0, :] = rm
    rsm[2:10, 1, :] = rm
    c["Rsm"] = rsm
    n = np.arange(128)[:, None]
    j = np.arange(32)[None, :]
    ovl = ((16 * n < 64 * j + 64) & (16 * n + 32 > 64 * j)).astype(np.float32)
    ovl[127] = 0.0
    c["ovl"] = np.concatenate([ovl, np.ones((128, 1), np.float32)], 1)
    mul = np.zeros((128, 16, 32), np.float32)
    add = np.zeros((128, 16, 32), np.float32)
    for qb in range(16):
        for tt in range(128):
            bt = (qb * 128 + tt) // 64
            mul[tt, qb, :bt] = 1.0
            add[tt, qb, bt] = 1e30
            add[tt, qb, bt + 1:] = -1e30
    c["mulm"] = mul
    c["addm"] = add
    gam = [1.0 - 2.0 ** (-5.0 - h) for h in range(4)]
    pos = np.arange(128)
    dm = np.zeros((128, 4, 128), np.float32)
    for h in range(4):
        rel = pos[None, :] - pos[:, None]
        dm[:, h] = np.where(rel >= 0, 0.125 * gam[h] ** np.maximum(rel, 0), 0.0)
    c["dmask"] = dm
    qd = np.zeros((128, 2, 128), np.float32)
    for j2 in range(2):
        for hp in range(2):
            h = 2 * j2 + hp
            qd[64 * hp:64 * hp + 64, j2, :] = (0.125 * gam[h] ** (pos + 1.0))[None, :]
    c["qdec"] = qd
    kd = np.zeros((128, 4), np.float32)
    for h in range(4):
        kd[:, h] = gam[h] ** (127.0 - np.arange(128))
    c["kdec"] = kd
    cm = np.ones((128, 512), np.float32)
    cm[:, ::64] = 0.0
    c["cmask"] = cm
    p128 = np.arange(128)
    c["m01"] = ((p128[:, None] // 64 == p128[None, :] // 64) & (p128[:, None] <= p128[None, :])).astype(np.float32)
    hm = np.zeros((128, 2), np.float32)
    hm[:64, 0] = 1.0
    hm[64:, 1] = 1.0
    c["hmask"] = hm
    c["onesf"] = np.ones((128, 128), np.float32)
    bd = np.zeros((128, 128), np.float32)
    bd[:64, :64] = 1.0
    bd[64:, 64:] = 1.0
    c["onesbd"] = bd
    return c


CONST_SPEC = [
    ("ident", [128, 128], BF16), ("causneg", [128, 512], BF16), ("winneg", [128, 512], BF16),
    ("LAl", [2, 16, 128], BF16), ("Lcm", [10, 16, 128], BF16), ("E", [32, 16, 128], BF16), ("Rsm", [10, 2, 512], BF16),
    ("ovl", [128, 33], BF16), ("mulm", [128, 16, 32], BF16), ("addm", [128, 16, 32], BF16),
    ("dmask", [128, 4, 128], F32), ("qdec", [128, 2, 128], F32), ("kdec", [128, 4], F32),
    ("cmask", [128, 512], F32), ("m01", [128, 128], F32), ("hmask", [128, 2], F32), ("onesf", [128, 128], F32),
    ("onesbd", [128, 128], F32),
]

P_NG, P_AG, P_CW, P_CB, P_BRA, P_BRI, P_LAM, P_DG, P_MB, P_QG, P_KG, P_POS = 0, 8, 12, 28, 32, 36, 40, 44, 48, 80, 81, 82
NPAR = 114


def _params(inp, l):
    P = np.zeros((128, NPAR), np.float32)
    P[:, P_NG:P_NG + 8] = inp["norm_g"][l].reshape(8, 128).T
    P[:, P_AG:P_AG + 4] = inp["a_norm_g"][l].reshape(4, 128).T
    P[:, P_CW:P_CW + 16] = inp["c_conv_w"][l].reshape(4, 4, 128).transpose(2, 1, 0).reshape(128, 16)
    P[:, P_CB:P_CB + 4] = inp["c_conv_b"][l].reshape(4, 128).T
    P[:, P_BRA:P_BRA + 4] = inp["c_b_ra"][l].reshape(4, 128).T
    P[:, P_BRI:P_BRI + 4] = inp["c_b_ri"][l].reshape(4, 128).T
    P[:, P_LAM:P_LAM + 4] = inp["c_lambda"][l].reshape(4, 128).T
    P[:, P_DG:P_DG + 4] = inp["d_norm_g"][l].reshape(4, 128).T
    P[:, P_MB:P_MB + 32] = inp["merge_b"][l].reshape(4, 8, 128).transpose(2, 0, 1).reshape(128, 32)
    P[:, P_QG] = np.tile(inp["b_q_norm_g"][l], 2)
    P[:, P_KG] = np.tile(inp["b_k_norm_g"][l], 2)
    P[:, P_POS:P_POS + 32] = np.tile(inp["b_cmp_pos"][l].T, (2, 1))
    return P


def build_program(NL, NS, dbg=False):
    nc = bass.Bass("TRN2", target_bir_lowering=False)
    dr = {}

    def dten(name, shape, kind="ExternalInput"):
        dr[name] = nc.dram_tensor(name, list(shape), F32, kind=kind).ap()
        return dr[name]
    xT = dten("xT", [NS, D, T])
    outT = dten("outT", [NS, D, T], kind="ExternalOutput")
    w_in = dten("w_in", [NL, D, NIN])
    w_br = dten("w_branch", [NL, 4, W, D])
    w_out = dten("w_out", [NL, D, D])
    wck = dten("b_cmp_wk", [NL, 2048, 64])
    wcv = dten("b_cmp_wv", [NL, 2048, 64])
    wra = dten("c_w_ra", [NL, 8, 64, 64])
    wri = dten("c_w_ri", [NL, 8, 64, 64])
    par = dten("par", [NL, 128, NPAR])
    lbl = dten("lbl", [128, 16])
    lsel = dten("lsel", [128, NL * 4])
    cdr = {}
    for name, shape, _ in CONST_SPEC:
        cdr[name] = dten("c_" + name, shape)
    if dbg:
        dbgT = dten("dbgT", [NS, NL, 4, W, T], kind="ExternalOutput")
        dbg2 = dten("dbg2", [64, 128, 512], kind="ExternalOutput")

    p = Prog(nc)
    PS = [p.ps("psb%d" % i, [128, 512], F32) for i in range(8)]
    PSN = ["psb%d" % i for i in range(8)]

    cst = {}
    for name, shape, dt in CONST_SPEC:
        cst[name] = p.sb("k_" + name, shape, dt)
        q = "pool" if dt == BF16 else "sp"
        p.dma(q, lambda e, a=cst[name], b=cdr[name]: e.dma_start(out=a[:], in_=b), writes=["k_" + name])
    CN = lambda n: "k_" + n
    zer = p.sb("zer", [128, 512], BF16)
    fsc = p.sb("fsc", [128, 2], F32)
    p.op("pool", lambda e: e.memset(zer[:], 0.0), writes=["zer"])
    parT = p.sb("parT", [128, NL, NPAR], F32)
    for l in range(NL):
        p.dma("sp", lambda e, l=l: e.dma_start(out=parT[:, l, :], in_=par[l]), writes=["par"])
    xn = p.sb("xn", [128, 8, T], BF16)
    oT = p.sb("oT", [128, 4, 4, T], BF16)
    WB = Ring(p, "wb", 3, [128, 8, 512], BF16)

    drv = p.sb("drv", [128, NL, 24], F32)
    lbt = p.sb("lbt", [128, 16], F32)
    lst = p.sb("lst", [128, NL * 4], F32)
    lbe = p.sb("lbe", [128, 4, 4], F32)
    lbs = p.sb("lbs", [128, 4], F32)
    lbc = p.sb("lbc", [128, 4, 4], F32)
    tmp4 = p.sb("tmp4", [128, 4, 4], F32)
    p.dma("sp", lambda e: e.dma_start(out=lbt[:], in_=lbl), writes=["lbt"])
    p.dma("sp", lambda e: e.dma_start(out=lst[:], in_=lsel), writes=["lst"])
    lb3 = lbt[:].rearrange("p (h l) -> p h l", l=4)
    p.op("act", lambda e: e.activation(out=lbe[:], in_=lb3, func=AF.Exp), reads=["lbt"], writes=["lbe"])
    p.op("dve", lambda e: e.tensor_reduce(out=lbs[:], in_=lbe[:], axis=AX.X, op=ALU.add), reads=["lbe"], writes=["lbs"])
    p.op("dve", lambda e: e.reciprocal(out=lbs[:], in_=lbs[:]), reads=["lbs"], writes=["lbs"])
    p.op("dve", lambda e: e.tensor_tensor(out=lbe[:], in0=lbe[:], in1=lbs[:].unsqueeze(2).to_broadcast([128, 4, 4]),
                                          op=ALU.mult), reads=["lbe", "lbs"], writes=["lbe"])
    p.op("dve", lambda e: e.memset(lbc[:], 0.0), writes=["lbc"])
    for l in range(1, 4):
        p.op("dve", lambda e, l=l: e.tensor_tensor(out=lbc[:, :, l:l + 1], in0=lbc[:, :, l - 1:l], in1=lbe[:, :, l:l + 1],
                                                   op=ALU.add), reads=["lbc", "lbe"], writes=["lbc"])
    for l in range(NL):
        sel = lst[:, l * 4:(l + 1) * 4]
        p.op("dve", lambda e, sel=sel: e.tensor_tensor(out=tmp4[:], in0=lbc[:], in1=sel.unsqueeze(1).to_broadcast([128, 4, 4]),
                                                       op=ALU.mult), reads=["lbc", "lst"], writes=["tmp4"])
        p.op("dve", lambda e, l=l: e.tensor_reduce(out=drv[:, l, 0:4], in_=tmp4[:], axis=AX.X, op=ALU.add),
             reads=["tmp4"], writes=["drv"])
        p.op("dve", lambda e, l=l: e.tensor_scalar(out=drv[:, l, 4:8], in0=drv[:, l, 0:4], scalar1=-1.0, scalar2=1.0,
                                                   op0=ALU.mult, op1=ALU.add), reads=["drv"], writes=["drv"])
        p.op("act", lambda e, l=l: e.activation(out=drv[:, l, 8:12], in_=parT[:, l, P_LAM:P_LAM + 4], func=AF.Exp, scale=-1.0),
             reads=["par"], writes=["drv"])
        p.op("act", lambda e, l=l: e.activation(out=drv[:, l, 8:12], in_=drv[:, l, 8:12], func=AF.Ln, bias=1.0),
             reads=["drv"], writes=["drv"])
        p.op("dve", lambda e, l=l: e.tensor_scalar(out=drv[:, l, 12:16], in0=drv[:, l, 8:12], scalar1=-16.0, scalar2=None,
                                                   op0=ALU.mult), reads=["drv"], writes=["drv"])
        p.op("dve", lambda e, l=l: e.tensor_scalar(out=drv[:, l, 8:12], in0=drv[:, l, 8:12], scalar1=-8.0, scalar2=None,
                                                   op0=ALU.mult), reads=["drv"], writes=["drv"])
        p.op("dve", lambda e, l=l: e.tensor_scalar(out=drv[:, l, 16:17], in0=parT[:, l, P_QG:P_QG + 1], scalar1=0.125,
                                                   scalar2=None, op0=ALU.mult), reads=["par"], writes=["drv"])

    def DUMP(idx, ap, n, res):
        if dbg:
            DMA("sp", dbg2[idx, :, 0:n], ap, reads=[res], writes=["dbg2_%d" % idx])

    def wload(l, segs):
        wt, wn = WB.next()
        names = ["%ss%d" % (wn, k) for k in range(4)]
        pre = p.tokens(names)
        src = w_in[l].rearrange("(kc q) n -> q kc n", q=128)
        off = 0
        offs = []
        for k, (c0, n) in enumerate(segs):
            DMA("pool", wt[:, :, off:off + n], src[:, :, c0:c0 + n], writes=[names[k]], extra=pre)
            offs.append(off)
            off += n
        return wt, names[:len(segs)], offs

    def proj_fm(ps, psn, wt, wres, off, M, t0, n):
        for kc in range(8):
            p.op("pe", lambda e, kc=kc: e.matmul(ps[0:M, 0:n], lhsT=wt[:, kc, off:off + M], rhs=xn[:, kc, t0:t0 + n],
                                                 start=(kc == 0), stop=(kc == 7)),
                 reads=list(wres) + ["xn"], writes=[psn], chain=(kc > 0))

    def proj_tm(ps, psn, wt, wres, off, N, t0):
        for kc in range(8):
            p.op("pe", lambda e, kc=kc: e.matmul(ps[:, 0:N], lhsT=xn[:, kc, t0:t0 + 128], rhs=wt[:, kc, off:off + N],
                                                 start=(kc == 0), stop=(kc == 7)),
                 reads=list(wres) + ["xn"], writes=[psn], chain=(kc > 0))

    def OP(eng, meth, reads, writes, chain=False, extra=(), **kw):
        return p.op(eng, lambda e: getattr(e, meth)(**kw), reads=reads, writes=writes, chain=chain, extra=extra)

    def DMA(q, out, in_, reads=(), writes=(), extra=()):
        return p.dma(q, lambda e: e.dma_start(out=out, in_=in_), reads=reads, writes=writes, extra=extra)

    def ACT(out, in_, func, reads, writes, **kw):
        return p.op("act", lambda e: e.activation(out=out, in_=in_, func=func, **kw), reads=reads, writes=writes)

    def TT(out, in0, in1, op, reads, writes, eng="dve"):
        eng = POOLMAP if eng == "pool" else eng
        return p.op(eng, lambda e: e.tensor_tensor(out=out, in0=in0, in1=in1, op=op), reads=reads, writes=writes)

    def TS(out, in0, s1, s2, op0, op1, reads, writes, eng="dve"):
        eng = POOLMAP if eng == "pool" else eng
        if s2 is None:
            return p.op(eng, lambda e: e.tensor_scalar(out=out, in0=in0, scalar1=s1, scalar2=None, op0=op0),
                        reads=reads, writes=writes)
        return p.op(eng, lambda e: e.tensor_scalar(out=out, in0=in0, scalar1=s1, scalar2=s2, op0=op0, op1=op1),
                    reads=reads, writes=writes)

    def STT(out, in0, sc, in1, op0, op1, reads, writes):
        return p.op("dve", lambda e: e.scalar_tensor_tensor(out=out, in0=in0, scalar=sc, in1=in1, op0=op0, op1=op1),
                    reads=reads, writes=writes)

    def MM(out, lhsT, rhs, start, stop, reads, writes, chain=False, **kw):
        return p.op("pe", lambda e: e.matmul(out, lhsT=lhsT, rhs=rhs, start=start, stop=stop, **kw),
                    reads=reads, writes=writes, chain=chain)

    def RECIP(out, in_, reads, writes):
        return p.op("dve", lambda e: e.reciprocal(out=out, in_=in_), reads=reads, writes=writes)

    def COPY(eng, out, in_, reads, writes):
        if eng == "act":
            return ACT(out, in_, AF.Copy, reads, writes)
        return p.op(eng, lambda e: e.tensor_copy(out=out, in_=in_), reads=reads, writes=writes)


    for s in range(NS):
        RES = "res%d" % s
        for l in range(NL):
            src_x = xT if l == 0 else outT
            pl = lambda c0, n=1, l=l: parT[:, l, c0:c0 + n]

            p.skip = "0" not in PHASES
            p.barrier()
            with ExitStack() as es:
                XS = Ring(p, "xs", 2, [128, 8, 256], F32, es)
                SQ = Ring(p, "sq0", 2, [128, 8, 256], F32, es)
                RS = Ring(p, "rs0", 2, [128, 256], F32, es)
                for tb in range(8):
                    t0 = tb * 256
                    xs, xsn = XS.next()
                    sq, sqn = SQ.next()
                    rs, rsn = RS.next()
                    DMA("sp", xs[:], src_x[s].rearrange("(c q) t -> q c t", q=128)[:, :, t0:t0 + 256],
                        reads=[RES + "_%d_%d" % (c_, t0 // 512) for c_ in range(8)], writes=[xsn])
                    ACT(sq[:], xs[:], AF.Square, [xsn], [sqn])
                    ps, psn = PS[tb % 2], PSN[tb % 2]
                    for kc in range(8):
                        MM(ps[:, 0:256], cst["onesf"][:], sq[:, kc, :], kc == 0, kc == 7, [CN("onesf"), sqn], [psn], chain=kc > 0)
                    ACT(rs[:], ps[:, 0:256], AF.Sqrt, [psn], [rsn], scale=1.0 / D, bias=EPS)
                    RECIP(rs[:], rs[:], [rsn], [rsn])
                    TT(xs[:], xs[:], rs[:].unsqueeze(1).to_broadcast([128, 8, 256]), ALU.mult, [xsn, rsn], [xsn])
                    TT(xn[:, :, t0:t0 + 256], xs[:], pl(P_NG, 8).unsqueeze(2).to_broadcast([128, 8, 256]), ALU.mult,
                       [xsn, "par"], ["xn"], eng="pool")

            p.skip = "A" not in PHASES
            p.barrier()
            with ExitStack() as es:
                vtm = p.sb("a_vtm", [128, 16, 512], BF16, es)
                F_ = [p.sb("a_f%d" % i, [128, 512], F32, es) for i in range(6)]
                FN = ["a_f%d" % i for i in range(6)]
                qp = Ring(p, "a_qp", 2, [128, 512], BF16, es)
                kp = Ring(p, "a_kp", 2, [128, 512], BF16, es)
                ktm = Ring(p, "a_ktm", 2, [128, 4, 2, 128], BF16, es)
                scs = Ring(p, "a_sc", 2, [128, 4, 128], BF16, es)
                esc = Ring(p, "a_es", 2, [128, 3, 8], F32, es)
                S = p.sb("a_S", [128, 128], F32, es)
                Sp = Ring(p, "a_Sp", 2, [128, 128], BF16, es)
                tkv = p.sb("a_tkv", [128, 128], F32, es)
                E_ = [p.sb("a_e%d" % i, [128, 512], F32, es) for i in range(4)]
                EN = ["a_e%d" % i for i in range(4)]
                wt, wr, wo = wload(l, [(C_AI, 512)])
                for blk in range(16):
                    ps, psn = PS[blk % 2], PSN[blk % 2]
                    proj_tm(ps, psn, wt, wr, 0, 512, blk * 128)
                    COPY("act" if blk % 2 else "dve", vtm[:, blk, :], ps[:, 0:512], [psn], ["a_vtm"])
                p.skip = p.skip or ("a" in PHASES)
                for h in range(4):
                    wt, wr, wo = wload(l, [(C_AQ + 128 * h, 128), (C_AF + 128 * h, 128), (C_AZ + 128 * h, 128)])
                    lbh = drv[:, l, h:h + 1]
                    omh = drv[:, l, 4 + h:5 + h]
                    OP("dve", "memset", [], ["a_S"], ap=S[:], constant=0.0)
                    for tl in range(4):
                        t0 = tl * 512
                        proj_fm(PS[0], PSN[0], wt, wr, wo[1], 128, t0, 512)
                        ACT(F_[0][:], PS[0][:], AF.Sigmoid, [PSN[0]], [FN[0]])
                        TS(F_[0][:], F_[0][:], omh, lbh, ALU.mult, ALU.add, [FN[0], "drv"], [FN[0]])
                        ACT(F_[1][:], F_[0][:], AF.Ln, [FN[0]], [FN[1]])
                        TS(F_[0][:], F_[0][:], -1.0, 1.0, ALU.mult, ALU.add, [FN[0]], [FN[0]], eng="pool")
                        OP("dve", "tensor_tensor_scan", [CN("cmask"), FN[1]], [FN[2]], out=F_[2][:], data0=cst["cmask"][:],
                           data1=F_[1][:], initial=0.0, op0=ALU.mult, op1=ALU.add)
                        b3 = F_[2][:].rearrange("q (c j) -> q c j", j=64)
                        TT(F_[3][:].rearrange("q (c j) -> q c j", j=64), b3, b3[:, :, 31:32].to_broadcast([128, 8, 64]),
                           ALU.subtract, [FN[2]], [FN[3]])
                        ACT(F_[4][:], F_[3][:], AF.Exp, [FN[3]], [FN[4]])
                        ACT(F_[5][:], F_[3][:], AF.Exp, [FN[3]], [FN[5]], scale=-1.0)
                        es_t, es_n = esc.next()
                        ACT(es_t[:, 0, :], b3[:, :, 31], AF.Exp, [FN[2]], [es_n])
                        ACT(es_t[:, 1, :], b3[:, :, 63], AF.Exp, [FN[2]], [es_n])
                        COPY("dve", es_t[:, 2, :], F_[4][:].rearrange("q (c j) -> q c j", j=64)[:, :, 63], [FN[4]], [es_n])
                        if h == 0 and tl == 0 and s == 0 and l == 0:
                            DUMP(0, F_[2][:], 512, FN[2])
                            DUMP(1, es_t[:].rearrange("q a b -> q (a b)"), 24, es_n)
                            DUMP(2, F_[1][:], 512, FN[1])
                        proj_fm(PS[1], PSN[1], wt, wr, wo[0], 128, t0, 512)
                        qpt, qpn = qp.next()
                        kpt, kpn = kp.next()
                        TT(qpt[:], PS[1][:], F_[4][:], ALU.mult, [PSN[1], FN[4]], [qpn])
                        TT(kpt[:], F_[0][:], F_[5][:], ALU.mult, [FN[0], FN[5]], [kpn], eng="pool")
                        p.skip = p.skip or ("b" in PHASES)
                        ktt, ktn = ktm.next()
                        pst = PS[7][:].bitcast(BF16)
                        for bb in range(4):
                            OP("pe", "transpose", [kpn, CN("ident")], [PSN[7]], chain=bb > 0, out=pst[:, bb * 128:(bb + 1) * 128],
                               in_=kpt[:, bb * 128:(bb + 1) * 128], identity=cst["ident"][:])
                        if h == 0 and tl == 0 and s == 0 and l == 0 and dbg:
                            dgp = p.sb("a_dgp", [128, 512], F32, es)
                            COPY("dve", dgp[:], pst[:, 0:512], [PSN[7]], ["a_dgp"]); DUMP(10, dgp[:], 512, "a_dgp")
                        for hf in range(2):
                            TS(ktt[:, :, hf, :], pst[:, 0:512].rearrange("q (b d) -> q b d", d=128), cst["hmask"][:, hf:hf + 1], None,
                               ALU.mult, None, [PSN[7], CN("hmask")], [ktn])
                        p.skip = p.skip or ("c" in PHASES)
                        psS = PS[2][:, 0:512].rearrange("q (b j) -> q b j", j=128)
                        for bb in range(4):
                            MM(psS[:, bb, :], kpt[:, bb * 128:(bb + 1) * 128], qpt[:, bb * 128:(bb + 1) * 128],
                               True, True, [kpn, qpn], [PSN[2]], chain=bb > 0)
                        sct, scn = scs.next()
                        TT(sct[:], psS, cst["m01"][:].unsqueeze(1).to_broadcast([128, 4, 128]), ALU.mult,
                           [PSN[2], CN("m01")], [scn])
                        for c in range(8):
                            hf, bb = c % 2, c // 2
                            pk = PS[3 + c // 4]
                            MM(pk[:, (c % 4) * 128:(c % 4 + 1) * 128], ktt[:, bb, hf, :],
                               vtm[:, tl * 4 + bb, h * 128:(h + 1) * 128], True, True,
                               [ktn, "a_vtm"], [PSN[3 + c // 4]], chain=(c % 4) > 0)
                        p.skip = p.skip or ("d" in PHASES)
                        po, pon = PS[5 + (tl % 2)], PSN[5 + (tl % 2)]
                        for c in range(8):
                            hf, bb = c % 2, c // 2
                            spt, spn = Sp.next()
                            TS(spt[:], S[:], es_t[:, 0, c:c + 1], None, ALU.mult, None, ["a_S", es_n], [spn])
                            MM(po[:, c * 64:(c + 1) * 64], vtm[:, tl * 4 + bb, h * 128:(h + 1) * 128],
                               sct[:, bb, hf * 64:(hf + 1) * 64], True, False, ["a_vtm", scn], [pon], chain=c > 0)
                            MM(po[:, c * 64:(c + 1) * 64], spt[:], qpt[:, c * 64:(c + 1) * 64], False, True,
                               [spn, qpn], [pon], chain=True)
                            pk = PS[3 + c // 4]
                            TS(tkv[:], pk[:, (c % 4) * 128:(c % 4 + 1) * 128], es_t[:, 2, c:c + 1], None, ALU.mult, None,
                               [PSN[3 + c // 4], es_n], ["a_tkv"])
                            STT(S[:], S[:], es_t[:, 1, c:c + 1], tkv[:], ALU.mult, ALU.add, ["a_S", es_n, "a_tkv"], ["a_S"])
                        if h == 0 and tl == 0 and s == 0 and l == 0 and dbg:
                            DUMP(3, S[:], 128, "a_S")
                            DUMP(4, tkv[:], 128, "a_tkv")
                            dg = [p.sb("a_dg%d" % i, [128, 512], F32, es) for i in range(5)]
                            COPY("dve", dg[0][:], kpt[:], [kpn], ["a_dg0"]); DUMP(5, dg[0][:], 512, "a_dg0")
                            COPY("dve", dg[1][:, 0:256], ktt[:, 3, :, :].rearrange("q a d -> q (a d)"), [ktn], ["a_dg1"]); DUMP(6, dg[1][:, 0:256], 256, "a_dg1")
                            COPY("dve", dg[2][:], PS[4][:], [PSN[4]], ["a_dg2"]); DUMP(7, dg[2][:], 512, "a_dg2")
                            COPY("dve", dg[3][:], vtm[:, 3, 0:512], ["a_vtm"], ["a_dg3"]); DUMP(8, dg[3][:], 512, "a_dg3")
                            COPY("dve", dg[4][:], F_[0][:], [FN[0]], ["a_dg4"]); DUMP(9, dg[4][:], 512, FN[0])
                        p.skip = p.skip or ("e" in PHASES)
                        ACT(E_[0][:], po[:], AF.Square, [pon], [EN[0]])
                        MM(PS[7][:], cst["onesf"][:], E_[0][:], True, True, [CN("onesf"), EN[0]], [PSN[7]])
                        ACT(E_[1][:], PS[7][:], AF.Sqrt, [PSN[7]], [EN[1]], scale=1.0 / 128, bias=EPS)
                        RECIP(E_[1][:], E_[1][:], [EN[1]], [EN[1]])
                        STT(E_[2][:], po[:], pl(P_AG + h), E_[1][:], ALU.mult, ALU.mult, [pon, "par", EN[1]], [EN[2]])
                        proj_fm(PS[0], PSN[0], wt, wr, wo[2], 128, t0, 512)
                        ACT(E_[3][:], PS[0][:], AF.Silu, [PSN[0]], [EN[3]])
                        TT(oT[:, 0, h, t0:t0 + 512], E_[2][:], E_[3][:], ALU.mult, [EN[2], EN[3]], ["oT0"], eng="pool")

            p.skip = "D" not in PHASES
            p.barrier()
            with ExitStack() as es:
                vtm = p.sb("d_vtm", [128, 16, 512], BF16, es)
                kdt = p.sb("d_kdt", [128, 16, 256], BF16, es)
                qs = Ring(p, "d_qs", 2, [128, 512], BF16, es)
                qt = Ring(p, "d_qt", 2, [128, 2, 512], BF16, es)
                kk = Ring(p, "d_kk", 2, [128, 2, 512], BF16, es)
                scs = Ring(p, "d_sc", 2, [128, 4, 128], BF16, es)
                S2 = [p.sb("d_S%d" % i, [128, 128], F32, es) for i in range(2)]
                Sb = Ring(p, "d_Sb", 3, [128, 128], BF16, es)
                E_ = [p.sb("d_e%d" % i, [128, 512], F32, es) for i in range(6)]
                EN = ["d_e%d" % i for i in range(6)]
                gam128 = [float((1.0 - 2.0 ** (-5.0 - h)) ** 128) for h in range(4)]
                wt, wr, wo = wload(l, [(C_DV, 512)])
                for blk in range(16):
                    ps, psn = PS[blk % 2], PSN[blk % 2]
                    proj_tm(ps, psn, wt, wr, 0, 512, blk * 128)
                    COPY("act" if blk % 2 else "dve", vtm[:, blk, :], ps[:, 0:512], [psn], ["d_vtm"])
                wt, wr, wo = wload(l, [(C_DK, 256)])
                for blk in range(16):
                    ps, psn = PS[blk % 2], PSN[blk % 2]
                    proj_tm(ps, psn, wt, wr, 0, 256, blk * 128)
                    TT(kdt[:, blk, :].rearrange("q (h d) -> q h d", d=64), ps[:, 0:256].rearrange("q (h d) -> q h d", d=64),
                       cst["kdec"][:].unsqueeze(2).to_broadcast([128, 4, 64]), ALU.mult, [psn, CN("kdec")], ["d_kdt"])
                p.skip = p.skip or ("a" in PHASES)
                for j in range(2):
                    wt, wr, wo = wload(l, [(C_DQ + 128 * j, 128), (C_DK + 128 * j, 128), (C_DZ + 256 * j, 256)])
                    for hp in range(2):
                        OP("dve", "memset", [], ["d_S%d" % hp], ap=S2[hp][:], constant=0.0)
                    for tl in range(4):
                        t0 = tl * 512
                        proj_fm(PS[0], PSN[0], wt, wr, wo[0], 128, t0, 512)
                        qst, qsn = qs.next()
                        qtt, qtn = qt.next()
                        kkt, kkn = kk.next()
                        p.skip = p.skip or ("x" in PHASES)
                        COPY("act", qst[:], PS[0][:], [PSN[0]], [qsn])
                        p.skip = p.skip or ("y" in PHASES)
                        TT(E_[0][:].rearrange("q (c j) -> q c j", j=128), PS[0][:].rearrange("q (c j) -> q c j", j=128),
                           cst["qdec"][:, j, :].unsqueeze(1).to_broadcast([128, 4, 128]), ALU.mult, [PSN[0], CN("qdec")], [EN[0]])
                        p.skip = p.skip or ("w" in PHASES)
                        for hp in range(2):
                            TS(qtt[:, hp, :], E_[0][:], cst["hmask"][:, hp:hp + 1], None, ALU.mult, None, [EN[0], CN("hmask")], [qtn],
                               eng="pool")
                        p.skip = p.skip or ("z" in PHASES)
                        proj_fm(PS[1], PSN[1], wt, wr, wo[1], 128, t0, 512)
                        for hp in range(2):
                            TS(kkt[:, hp, :], PS[1][:], cst["hmask"][:, hp:hp + 1], None, ALU.mult, None, [PSN[1], CN("hmask")], [kkn])
                        p.skip = p.skip or ("b" in PHASES)
                        for hp in range(2):
                            h = 2 * j + hp
                            psS = PS[2][:, 0:512].rearrange("q (b j) -> q b j", j=128)
                            for bb in range(4):
                                MM(psS[:, bb, :], kkt[:, hp, bb * 128:(bb + 1) * 128], qst[:, bb * 128:(bb + 1) * 128], True, True,
                                   [kkn, qsn], [PSN[2]], chain=bb > 0)
                            sct, scn = scs.next()
                            TT(sct[:], psS, cst["dmask"][:, h, :].unsqueeze(1).to_broadcast([128, 4, 128]), ALU.mult,
                               [PSN[2], CN("dmask")], [scn])
                            pk, pkn = PS[3 + hp], PSN[3 + hp]
                            for bb in range(4):
                                MM(pk[:, bb * 128:(bb + 1) * 128], kdt[:, tl * 4 + bb, 128 * j:128 * j + 128],
                                   vtm[:, tl * 4 + bb, h * 128:(h + 1) * 128], True, True, ["d_kdt", "d_vtm"], [pkn], chain=bb > 0)
                            p.skip = p.skip or ("c" in PHASES)
                            po, pon = PS[5 + hp], PSN[5 + hp]
                            Sn = "d_S%d" % hp
                            for bb in range(4):
                                sbt, sbn = Sb.next()
                                COPY("act", sbt[:], S2[hp][:], [Sn], [sbn])
                                MM(po[:, bb * 128:(bb + 1) * 128], vtm[:, tl * 4 + bb, h * 128:(h + 1) * 128], sct[:, bb, :],
                                   True, False, ["d_vtm", scn], [pon], chain=bb > 0)
                                MM(po[:, bb * 128:(bb + 1) * 128], sbt[:], qtt[:, hp, bb * 128:(bb + 1) * 128], False, True,
                                   [sbn, qtn], [pon], chain=True)
                                STT(S2[hp][:], S2[hp][:], gam128[h], pk[:, bb * 128:(bb + 1) * 128], ALU.mult, ALU.add, [Sn, pkn], [Sn])
                            p.skip = p.skip or ("d" in PHASES)
                            COPY("act", E_[0][:], po[:], [pon], [EN[0]])
                            ACT(E_[1][:], po[:], AF.Square, [pon], [EN[1]])
                            MM(PS[7][:], cst["onesf"][:], E_[0][:], True, True, [CN("onesf"), EN[0]], [PSN[7]])
                            ACT(E_[2][:], PS[7][:], AF.Copy, [PSN[7]], [EN[2]], scale=1.0 / 128)
                            MM(PS[7][:], cst["onesf"][:], E_[1][:], True, True, [CN("onesf"), EN[1]], [PSN[7]])
                            TT(E_[3][:], E_[2][:], E_[2][:], ALU.mult, [EN[2]], [EN[3]], eng="pool")
                            STT(E_[3][:], PS[7][:], 1.0 / 128, E_[3][:], ALU.mult, ALU.subtract, [PSN[7], EN[3]], [EN[3]])
                            TS(E_[3][:], E_[3][:], 0.0, None, ALU.max, None, [EN[3]], [EN[3]])
                            ACT(E_[3][:], E_[3][:], AF.Sqrt, [EN[3]], [EN[3]], bias=EPS)
                            RECIP(E_[3][:], E_[3][:], [EN[3]], [EN[3]])
                            TT(E_[0][:], E_[0][:], E_[2][:], ALU.subtract, [EN[0], EN[2]], [EN[0]], eng="pool")
                            STT(E_[4][:], E_[0][:], pl(P_DG + h), E_[3][:], ALU.mult, ALU.mult, [EN[0], "par", EN[3]], [EN[4]])
                            proj_fm(PS[0], PSN[0], wt, wr, wo[2] + 128 * hp, 128, t0, 512)
                            ACT(E_[5][:], PS[0][:], AF.Silu, [PSN[0]], [EN[5]])
                            TT(oT[:, 3, h, t0:t0 + 512], E_[4][:], E_[5][:], ALU.mult, [EN[4], EN[5]], ["oT3"], eng="pool")

            p.skip = "C" not in PHASES
            p.barrier()
            with ExitStack() as es:
                wbd = p.sb("c_wbd", [128, 2, 4, 128], BF16, es)
                xr = [p.sb("c_xr%d" % i, [128, 515], F32, es) for i in range(2)]
                XRN = ["c_xr0", "c_xr1"]
                G_ = [p.sb("c_g%d" % i, [128, 512], F32, es) for i in range(7)]
                GN = ["c_g%d" % i for i in range(7)]
                xcb = p.sb("c_xcb", [128, 512], BF16, es)
                hh = [p.sb("c_h%d" % i, [128, 512], F32, es) for i in range(2)]
                HN = ["c_h0", "c_h1"]
                OP("pool", "memset", [], ["c_wbd"], ap=wbd[:], constant=0.0)
                for wi, wsrc in enumerate((wra, wri)):
                    for hb in range(2):
                        DMA("pool", wbd[64 * hb:64 * hb + 64, wi, :, 64 * hb:64 * hb + 64],
                            wsrc[l].rearrange("(j b) c d -> b c j d", b=2)[hb], reads=[], writes=["c_wbd"])
                for j in range(4):
                    wt, wr, wo = wload(l, [(C_CX + 128 * j, 128), (C_CZ + 128 * j, 128)])
                    for tl in range(4):
                        t0 = tl * 512
                        xa, xan = xr[tl % 2], XRN[tl % 2]
                        xb, xbn = xr[(tl + 1) % 2], XRN[(tl + 1) % 2]
                        if tl == 0:
                            OP("dve", "memset", [], [xan], ap=xa[:, 0:3], constant=0.0)
                        proj_fm(PS[0], PSN[0], wt, wr, wo[0], 128, t0, 512)
                        COPY("act", xa[:, 3:515], PS[0][:], [PSN[0]], [xan])
                        if tl < 3:
                            COPY("dve", xb[:, 0:3], xa[:, 512:515], [xan], [xbn])
                        cw = lambda k, j=j: pl(P_CW + 4 * j + k)
                        TS(G_[0][:], xa[:, 3:515], cw(3), pl(P_CB + j), ALU.mult, ALU.add, [xan, "par"], [GN[0]])
                        STT(G_[0][:], xa[:, 2:514], cw(2), G_[0][:], ALU.mult, ALU.add, [xan, "par", GN[0]], [GN[0]])
                        STT(G_[0][:], xa[:, 1:513], cw(1), G_[0][:], ALU.mult, ALU.add, [xan, "par", GN[0]], [GN[0]])
                        STT(G_[0][:], xa[:, 0:512], cw(0), G_[0][:], ALU.mult, ALU.add, [xan, "par", GN[0]], [GN[0]])
                        COPY("act", xcb[:], G_[0][:], [GN[0]], ["c_xcb"])
                        MM(PS[1][:], wbd[:, 0, j, :], xcb[:], True, True, ["c_wbd", "c_xcb"], [PSN[1]])
                        MM(PS[2][:], wbd[:, 1, j, :], xcb[:], True, True, ["c_wbd", "c_xcb"], [PSN[2]])
                        ACT(G_[1][:], PS[1][:], AF.Sigmoid, [PSN[1], "par"], [GN[1]], bias=pl(P_BRA + j))
                        ACT(G_[2][:], PS[2][:], AF.Sigmoid, [PSN[2], "par"], [GN[2]], bias=pl(P_BRI + j))
                        ACT(G_[3][:], G_[1][:], AF.Exp, [GN[1], "drv"], [GN[3]], scale=drv[:, l, 8 + j:9 + j])
                        ACT(G_[4][:], G_[1][:], AF.Exp, [GN[1], "drv"], [GN[4]], scale=drv[:, l, 12 + j:13 + j])
                        ACT(G_[4][:], G_[4][:], AF.Sqrt, [GN[4]], [GN[4]], scale=-1.0, bias=1.0)
                        TT(G_[2][:], G_[2][:], G_[0][:], ALU.mult, [GN[2], GN[0]], [GN[2]], eng="pool")
                        TT(G_[2][:], G_[2][:], G_[4][:], ALU.mult, [GN[2], GN[4]], [GN[2]])
                        ha, han = hh[tl % 2], HN[tl % 2]
                        hb_, hbn = hh[(tl + 1) % 2], HN[(tl + 1) % 2]
                        init = 0.0 if tl == 0 else hb_[:, 511:512]
                        OP("dve", "tensor_tensor_scan", [GN[3], GN[2], hbn], [han], out=ha[:], data0=G_[3][:], data1=G_[2][:],
                           initial=init, op0=ALU.mult, op1=ALU.add)
                        proj_fm(PS[3], PSN[3], wt, wr, wo[1], 128, t0, 512)
                        ACT(G_[5][:], PS[3][:], AF.Silu, [PSN[3]], [GN[5]])
                        TT(oT[:, 2, j, t0:t0 + 512], ha[:], G_[5][:], ALU.mult, [han, GN[5]], ["oT2"], eng="pool")

            p.skip = "B" not in PHASES
            p.barrier()
            with ExitStack() as es:
                ksT = p.sb("b_ksT", [128, T], BF16, es)
                kwT = p.sb("b_kwT", [128, T], BF16, es)
                vsa = p.sb("b_vsa", [128, 16, 2, 65], BF16, es)
                vwa = p.sb("b_vwa", [128, 16, 2, 65], BF16, es)
                gts = p.sb("b_gt", [128, 16, 24], F32, es)
                kcm = p.sb("b_kcm", [128, 128], BF16, es)
                Rc = p.sb("b_Rc", [128, 2, 97], BF16, es)
                B_ = [p.sb("b_t%d" % i, [128, 512], F32, es) for i in range(3)]
                BN = ["b_t%d" % i for i in range(3)]
                with ExitStack() as es2:
                    kcT = p.sb("b_kcT", [128, T], BF16, es2)
                    vcT = p.sb("b_vcT", [128, T], BF16, es2)
                    wkb = p.sb("b_wkb", [128, 32, 128], BF16, es2)
                    wvb = p.sb("b_wvb", [128, 32, 128], BF16, es2)
                    posb = p.sb("b_posb", [128, 32], BF16, es2)
                    cb = p.sb("b_cb", [128, 2], F32, es2)
                    raw = p.sb("b_raw", [128, 2, 128], F32, es2)
                    vrb = p.sb("b_vrb", [128, 128], BF16, es2)
                    OP("pool", "memset", [], ["b_wkb"], ap=wkb[:], constant=0.0)
                    OP("pool", "memset", [], ["b_wvb"], ap=wvb[:], constant=0.0)
                    for wtile, wn_, wsrc in ((wkb, "b_wkb", wck), (wvb, "b_wvb", wcv)):
                        for g in range(2):
                            for lh in range(2):
                                DMA("pool", wtile[64 * g:64 * g + 64, 16 * lh:16 * lh + 16, 64 * g:64 * g + 64],
                                    wsrc[l].rearrange("(a d) c -> d a c", d=64)[:, 16 * lh:16 * lh + 16, :], writes=[wn_])
                    COPY("dve", posb[:], pl(P_POS, 32), ["par"], ["b_posb"])
                    OP("pool", "memset", [], ["b_vsa"], ap=vsa[:, :, :, 64:65], constant=1.0)
                    OP("pool", "memset", [], ["b_vwa"], ap=vwa[:, :, :, 64:65], constant=1.0)
                    OP("pool", "memset", [], ["b_raw"], ap=raw[:], constant=0.0)
                    OP("pool", "memset", [], ["b_Rc"], ap=Rc[:], constant=0.0)
                    wt, wr, wo = wload(l, [(C_BKC, 128), (C_BVC, 128), (C_BKS, 128), (C_BKW, 128)])
                    for tl in range(4):
                        t0 = tl * 512
                        proj_fm(PS[0], PSN[0], wt, wr, wo[0], 128, t0, 512)
                        COPY("act", kcT[:, t0:t0 + 512], PS[0][:], [PSN[0]], ["b_kcT"])
                        proj_fm(PS[1], PSN[1], wt, wr, wo[1], 128, t0, 512)
                        COPY("dve", vcT[:, t0:t0 + 512], PS[1][:], [PSN[1]], ["b_vcT"])
                        for wi, (dst, dn) in enumerate(((ksT, "b_ksT"), (kwT, "b_kwT"))):
                            ps, psn = PS[2 + wi], PSN[2 + wi]
                            proj_fm(ps, psn, wt, wr, wo[2 + wi], 128, t0, 512)
                            ACT(B_[0][:], ps[:], AF.Square, [psn], [BN[0]])
                            MM(PS[7][:], cst["onesbd"][:], B_[0][:], True, True, [CN("onesbd"), BN[0]], [PSN[7]])
                            ACT(B_[1][:], PS[7][:], AF.Sqrt, [PSN[7]], [BN[1]], scale=1.0 / 64, bias=EPS)
                            RECIP(B_[1][:], B_[1][:], [BN[1]], [BN[1]])
                            STT(dst[:, t0:t0 + 512], ps[:], pl(P_KG), B_[1][:], ALU.mult, ALU.mult, [psn, "par", BN[1]], [dn])
                    wt, wr, wo = wload(l, [(C_BVS, 128), (C_BVW, 128), (C_BG, 24)])
                    for blk in range(16):
                        ps, psn = PS[blk % 2], PSN[blk % 2]
                        proj_tm(ps, psn, wt, wr, 0, 280, blk * 128)
                        COPY("dve", vsa[:, blk, :, 0:64], ps[:, 0:128].rearrange("q (g d) -> q g d", d=64), [psn], ["b_vsa"])
                        COPY("act", vwa[:, blk, :, 0:64], ps[:, 128:256].rearrange("q (g d) -> q g d", d=64), [psn], ["b_vwa"])
                        ACT(gts[:, blk, :], ps[:, 256:280], AF.Sigmoid, [psn], ["b_gt"])
                    for wi, (srcT, sn, wtile, wn_) in enumerate(((kcT, "b_kcT", wkb, "b_wkb"), (vcT, "b_vcT", wvb, "b_wvb"))):
                        ps, psn = PS[2 + wi], PSN[2 + wi]
                        for ll in range(32):
                            MM(ps[:, 0:127], wtile[:, ll, :], srcT[:].rearrange("q (n u) -> q n u", u=16)[:, (ll // 16):(ll // 16) + 127, ll % 16], ll == 0, ll == 31,
                               [wn_, sn], [psn], chain=ll > 0)
                        psb, psbn = PS[4 + wi], PSN[4 + wi]
                        for ll in range(32):
                            MM(psb[:, 0:1], wtile[:, ll, :], posb[:, ll:ll + 1], ll == 0, ll == 31, [wn_, "b_posb"], [psbn],
                               chain=ll > 0)
                        COPY("dve", cb[:, wi:wi + 1], psb[:, 0:1], [psbn], ["b_cb"])
                        ACT(raw[:, wi, 0:127], ps[:, 0:127], AF.Identity, [psn, "b_cb"], ["b_raw"], bias=cb[:, wi:wi + 1])
                    ACT(B_[0][:, 0:128], raw[:, 0, :], AF.Square, ["b_raw"], [BN[0]])
                    MM(PS[7][:, 0:128], cst["onesbd"][:], B_[0][:, 0:128], True, True, [CN("onesbd"), BN[0]], [PSN[7]])
                    ACT(B_[1][:, 0:128], PS[7][:, 0:128], AF.Sqrt, [PSN[7]], [BN[1]], scale=1.0 / 64, bias=EPS)
                    RECIP(B_[1][:, 0:128], B_[1][:, 0:128], [BN[1]], [BN[1]])
                    STT(kcm[:], raw[:, 0, :], pl(P_KG), B_[1][:, 0:128], ALU.mult, ALU.mult, ["b_raw", "par", BN[1]], ["b_kcm"])
                    COPY("dve", vrb[:], raw[:, 1, :], ["b_raw"], ["b_vrb"])
                    pst = PS[6][:].bitcast(BF16)
                    OP("pe", "transpose", ["b_vrb", CN("ident")], [PSN[6]], out=pst[:, 0:128], in_=vrb[:], identity=cst["ident"][:])
                    COPY("dve", Rc[:, :, 0:64], pst[:, 0:128].rearrange("q (g d) -> q g d", d=64), [PSN[6]], ["b_Rc"])
                    for g in range(2):
                        COPY("dve", Rc[:, g, 64:97], cst["ovl"][:], [CN("ovl")], ["b_Rc"])
                    p.barrier()
                qG = p.sb("b_qG", [128, 4, 512], BF16, es)
                qGz = p.sb("b_qGz", [128, 2, 4, 512], BF16, es)
                szT = p.sb("b_sz", [128, 4, 512], BF16, es)
                nsT = p.sb("b_nsT", [32, 4, 128], BF16, es)
                ET = Ring(p, "b_e", 3, [128, 512], BF16, es)
                acc = p.sb("b_acc", [128, 8, 64], F32, es)
                accb = p.sb("b_accb", [128, 512], BF16, es)
                sm = [p.sb("b_sm%d" % i, [128, 4], F32, es) for i in range(3)]
                impw = p.sb("b_impw", [128, 4, 32], F32, es)
                imp = p.sb("b_imp", [128, 32], F32, es)
                mx8 = p.sb("b_mx8", [128, 8], F32, es)
                nsb = p.sb("b_nsb", [128, 32], BF16, es)
                tmpo = p.sb("b_tmpo", [128, 4, 64], F32, es)
                sbank = [2, 3]
                sidx = [0]

                def next_s():
                    i = sbank[sidx[0] % 2]
                    sidx[0] += 1
                    return PS[i], PSN[i]
                for qb in range(16):
                    tl, ql = qb // 4, qb % 4
                    if ql == 0:
                        t0 = tl * 512
                        segs = []
                        for m in range(4):
                            segs += [(C_BQ + 64 * m, 64), (C_BQ + 64 * (4 + m), 64)]
                        wt, wr, wo = wload(l, [(C_BQ, 512)])
                        wtq, wrq = wt, wr
                        wt2, wr2, wo2 = wload(l, [(C_BZ, 512)])
                        for m in range(4):
                            ps, psn = PS[m % 2], PSN[m % 2]
                            for kc in range(8):
                                MM(ps[0:64, 0:512], wtq[:, kc, 64 * m:64 * m + 64], xn[:, kc, t0:t0 + 512], kc == 0, kc == 7,
                                   list(wrq) + ["xn"], [psn], chain=kc > 0)
                            for kc in range(8):
                                MM(ps[64:128, 0:512], wtq[:, kc, 64 * (4 + m):64 * (4 + m) + 64], xn[:, kc, t0:t0 + 512], kc == 0, kc == 7,
                                   list(wrq) + ["xn"], [psn], chain=True)
                            ACT(B_[0][:], ps[:], AF.Square, [psn], [BN[0]])
                            MM(PS[7][:], cst["onesbd"][:], B_[0][:], True, True, [CN("onesbd"), BN[0]], [PSN[7]])
                            ACT(B_[1][:], PS[7][:], AF.Sqrt, [PSN[7]], [BN[1]], scale=1.0 / 64, bias=EPS)
                            RECIP(B_[1][:], B_[1][:], [BN[1]], [BN[1]])
                            STT(qG[:, m, :], ps[:], drv[:, l, 16:17], B_[1][:], ALU.mult, ALU.mult, [psn, "drv", BN[1]], ["b_qG"])
                            for g in range(2):
                                TS(qGz[:, g, m, :], qG[:, m, :], cst["hmask"][:, g:g + 1], None, ALU.mult, None,
                                   ["b_qG", CN("hmask")], ["b_qGz"], eng="pool")
                        for jj in range(4):
                            ps, psn = PS[jj % 2], PSN[jj % 2]
                            proj_fm(ps, psn, wt2, wr2, 128 * jj, 128, t0, 512)
                            ACT(szT[:, jj, :], ps[:], AF.Silu, [psn], ["b_sz"])
                    gq = gts[:, qb, :].rearrange("q (h c) -> q h c", c=3)
                    for g in range(2):
                        gb = 64 * g
                        qrhs = qGz[:, g, :, ql * 128:(ql + 1) * 128]
                        rsl = cst["Rsm"][0:2, g, :]
                        nk = min(127, 8 * qb + 7)
                        ps, psn = next_s()
                        MM(ps[0:nk, :], kcm[:, 0:nk], qrhs, True, False, ["b_kcm", "b_qGz"], [psn])
                        MM(ps[0:nk, :], cst["Lcm"][0:10, qb, 0:nk], cst["Rsm"][0:10, g, :], False, True, [CN("Lcm"), CN("Rsm")], [psn], chain=True)
                        et, etn = ET.next()
                        ACT(et[0:nk, :], ps[0:nk, :], AF.Exp, [psn], [etn])
                        poc = PS[6][:, 0:388].rearrange("q (r c) -> q r c", c=97)
                        for r in range(4):
                            MM(poc[:, r, :], et[0:nk, r * 128:(r + 1) * 128], Rc[0:nk, g, :], True, True, [etn, "b_Rc"], [PSN[6]],
                               chain=r > 0)
                        TS(sm[0][:], poc[:, :, 96], 1e-30, None, ALU.max, None, [PSN[6]], ["b_sm0"])
                        RECIP(sm[0][:], sm[0][:], ["b_sm0"], ["b_sm0"])
                        TT(impw[:], poc[:, :, 64:96], sm[0][:].unsqueeze(2).to_broadcast([128, 4, 32]), ALU.mult,
                           [PSN[6], "b_sm0"], ["b_impw"])
                        OP("dve", "tensor_reduce", ["b_impw"], ["b_imp"], out=imp[:], in_=impw[:].rearrange("q r j -> q j r"),
                           axis=AX.X, op=ALU.add)
                        TT(sm[0][:], sm[0][:], gq[:, 4 * g:4 * g + 4, 0], ALU.mult, ["b_sm0", "b_gt"], ["b_sm0"])
                        TT(acc[:, 4 * g:4 * g + 4, :], poc[:, :, 0:64], sm[0][:].unsqueeze(2).to_broadcast([128, 4, 64]), ALU.mult,
                           [PSN[6], "b_sm0"], ["b_acc"])
                        TT(imp[:], imp[:], cst["mulm"][:, qb, :], ALU.mult, ["b_imp", CN("mulm")], ["b_imp"])
                        TT(imp[:], imp[:], cst["addm"][:, qb, :], ALU.add, ["b_imp", CN("addm")], ["b_imp"])
                        OP("dve", "max", ["b_imp"], ["b_mx8"], out=mx8[:], in_=imp[:])
                        TS(imp[:], imp[:], mx8[:, 7:8], -NEG, ALU.is_ge, ALU.mult, ["b_imp", "b_mx8"], ["b_imp"])
                        TS(nsb[:], imp[:], NEG, None, ALU.add, None, ["b_imp"], ["b_nsb"])
                        pst = PS[7][:].bitcast(BF16)
                        OP("pe", "transpose", ["b_nsb", CN("ident")], [PSN[7]], out=pst[0:32, 0:128], in_=nsb[:], identity=cst["ident"][:])
                        for r in range(4):
                            COPY("act" if r % 2 else "dve", nsT[:, r, :], pst[0:32, 0:128], [PSN[7]], ["b_nsT"])
                        pos_ = PS[4][:, 0:260].rearrange("q (r c) -> q r c", c=65)
                        MM(PS[4][:, 0:260], zer[0:1, 0:128], zer[0:1, 0:260], True, False, ["zer"], [PSN[4]], skip_group_check=True)
                        for kt in range(qb + 1):
                            ps, psn = next_s()
                            MM(ps[:], ksT[:, kt * 128:(kt + 1) * 128], qrhs, True, False, ["b_ksT", "b_qGz"], [psn])
                            MM(ps[:], cst["LAl"][0:2, qb - kt, :], rsl, False, False, [CN("LAl"), CN("Rsm")], [psn], chain=True)
                            MM(ps[:], cst["E"][0:32, kt, :], nsT[:].rearrange("q r t -> q (r t)"), False, kt < qb,
                               [CN("E"), "b_nsT"], [psn], chain=True)
                            if kt == qb:
                                MM(ps[:], cst["ident"][:], cst["causneg"][:], False, True, [CN("ident"), CN("causneg")], [psn],
                                   chain=True)
                            et, etn = ET.next()
                            ACT(et[:], ps[:], AF.Exp, [psn], [etn])
                            for r in range(4):
                                MM(pos_[:, r, :], et[:, r * 128:(r + 1) * 128], vsa[:, kt, g, :], False, kt == qb,
                                   [etn, "b_vsa"], [PSN[4]], chain=True, skip_group_check=True)
                        pow_ = PS[5][:, 0:260].rearrange("q (r c) -> q r c", c=65)
                        MM(PS[5][:, 0:260], zer[0:1, 0:128], zer[0:1, 0:260], True, False, ["zer"], [PSN[5]], skip_group_check=True)
                        for kt in range(max(0, qb - 2), qb + 1):
                            ps, psn = next_s()
                            last_plain = (kt == qb - 1)
                            MM(ps[:], kwT[:, kt * 128:(kt + 1) * 128], qrhs, True, False, ["b_kwT", "b_qGz"], [psn])
                            MM(ps[:], cst["LAl"][0:2, qb - kt, :], rsl, False, last_plain, [CN("LAl"), CN("Rsm")], [psn], chain=True)
                            if kt == qb:
                                MM(ps[:], cst["ident"][:], cst["causneg"][:], False, True, [CN("ident"), CN("causneg")], [psn],
                                   chain=True)
                            elif kt == qb - 2:
                                MM(ps[:], cst["ident"][:], cst["winneg"][:], False, True, [CN("ident"), CN("winneg")], [psn],
                                   chain=True)
                            et, etn = ET.next()
                            ACT(et[:], ps[:], AF.Exp, [psn], [etn])
                            for r in range(4):
                                MM(pow_[:, r, :], et[:, r * 128:(r + 1) * 128], vwa[:, kt, g, :], False, kt == qb,
                                   [etn, "b_vwa"], [PSN[5]], chain=True, skip_group_check=True)
                        for bi, (pso, pson) in enumerate(((pos_, PSN[4]), (pow_, PSN[5]))):
                            smt, smn = sm[1 + bi], "b_sm%d" % (1 + bi)
                            RECIP(smt[:], pso[:, :, 64], [pson], [smn])
                            TT(smt[:], smt[:], gq[:, 4 * g:4 * g + 4, 1 + bi], ALU.mult, [smn, "b_gt"], [smn])
                            TT(tmpo[:], pso[:, :, 0:64], smt[:].unsqueeze(2).to_broadcast([128, 4, 64]), ALU.mult,
                               [pson, smn], ["b_tmpo"])
                            TT(acc[:, 4 * g:4 * g + 4, :], acc[:, 4 * g:4 * g + 4, :], tmpo[:], ALU.add, ["b_acc", "b_tmpo"],
                               ["b_acc"], eng="pool")
                    COPY("act", accb[:], acc[:].rearrange("q h d -> q (h d)"), ["b_acc"], ["b_accb"])
                    pst = PS[7][:].bitcast(BF16)
                    for jj in range(4):
                        OP("pe", "transpose", ["b_accb", CN("ident")], [PSN[7]], chain=jj > 0, out=pst[:, jj * 128:(jj + 1) * 128],
                           in_=accb[:, jj * 128:(jj + 1) * 128], identity=cst["ident"][:])
                    TT(oT[:, 1, :, qb * 128:(qb + 1) * 128], pst[:, 0:512].rearrange("q (j t) -> q j t", t=128),
                       szT[:, :, ql * 128:(ql + 1) * 128], ALU.mult, [PSN[7], "b_sz"], ["oT1"])

            p.skip = False
            if dbg:
                for br in range(4):
                    if "ABCD"[br] not in PHASES or any(ch in PHASES for ch in "abcde"):
                        continue
                    DMA("pool", dbgT[s, l, br].rearrange("(j q) t -> q j t", q=128), oT[:, br, :, :],
                        reads=["oT%d" % br], writes=["dbg"])

            p.skip = "M" not in PHASES
            p.barrier()
            with ExitStack() as es:
                mg = p.sb("m_mg", [128, 8, T], BF16, es)
                macc = p.sb("m_acc", [128, T], F32, es)
                GT = Ring(p, "m_g", 2, [128, 512], F32, es)
                TM = Ring(p, "m_t", 2, [128, 512], F32, es)
                WBR = Ring(p, "m_wb", 2, [128, 4, 128], BF16, es)
                WO = Ring(p, "m_wo", 2, [128, 8, 128], BF16, es)
                XR = Ring(p, "m_xr", 2, [128, 512], F32, es)
                for c in range(8):
                    segs = [(C_MG + br * 1024 + c * 128, 128) for br in range(4)]
                    wt, wr, wo = wload(l, segs)
                    for br in range(4):
                        wbt, wbn = WBR.next()
                        DMA("pool", wbt[:], w_br[l, br].rearrange("(k q) n -> q k n", q=128)[:, :, c * 128:(c + 1) * 128],
                            writes=[wbn])
                        for tl in range(4):
                            t0 = tl * 512
                            psP, psPn = PS[(2 * tl) % 8], PSN[(2 * tl) % 8]
                            psG, psGn = PS[(2 * tl + 1) % 8], PSN[(2 * tl + 1) % 8]
                            for k in range(4):
                                MM(psP[:], wbt[:, k, :], oT[:, br, k, t0:t0 + 512], k == 0, k == 3, [wbn, "oT%d" % br], [psPn],
                                   chain=k > 0)
                            proj_fm(psG, psGn, wt, wr, wo[br], 128, t0, 512)
                            gt_, gtn = GT.next()
                            ACT(gt_[:], psG[:], AF.Sigmoid, [psGn, "par"], [gtn], bias=pl(P_MB + br * 8 + c))
                            if br == 0:
                                TT(macc[:, t0:t0 + 512], gt_[:], psP[:], ALU.mult, [gtn, psPn], ["m_acc%d" % tl])
                            else:
                                tm_, tmn = TM.next()
                                TT(tm_[:], gt_[:], psP[:], ALU.mult, [gtn, psPn], [tmn])
                                if br < 3:
                                    TT(macc[:, t0:t0 + 512], macc[:, t0:t0 + 512], tm_[:], ALU.add, ["m_acc%d" % tl, tmn],
                                       ["m_acc%d" % tl], eng="pool")
                                else:
                                    TT(mg[:, c, t0:t0 + 512], macc[:, t0:t0 + 512], tm_[:], ALU.add, ["m_acc%d" % tl, tmn],
                                       ["m_mg"], eng="pool")
                for c2 in range(8):
                    wot, won = WO.next()
                    DMA("pool", wot[:], w_out[l].rearrange("(k q) n -> q k n", q=128)[:, :, c2 * 128:(c2 + 1) * 128],
                        writes=[won])
                    for tl in range(4):
                        t0 = tl * 512
                        ps, psn = PS[(c2 * 4 + tl) % 8], PSN[(c2 * 4 + tl) % 8]
                        for k in range(8):
                            MM(ps[:], wot[:, k, :], mg[:, k, t0:t0 + 512], k == 0, k == 7, [won, "m_mg"], [psn], chain=k > 0)
                        xr_, xrn = XR.next()
                        DMA("sp", xr_[:], src_x[s, c2 * 128:(c2 + 1) * 128, t0:t0 + 512], reads=[RES + "_%d_%d" % (c2, tl)], writes=[xrn])
                        TT(xr_[:], xr_[:], ps[:], ALU.add, [xrn, psn], [xrn])
                        DMA("sp", outT[s, c2 * 128:(c2 + 1) * 128, t0:t0 + 512], xr_[:], reads=[xrn], writes=[RES + "_%d_%d" % (c2, tl)])
            p.barrier()

    toks = p.all_tokens()
    waits = p._deps("sp", (), (), toks)
    p.ops["sp"].append((None, waits, None))
    p.emit()
    return nc


_PROG_CACHE = {}


def _get_prog(NL, NS, dbg=False):
    key = (NL, NS, dbg)
    if key not in _PROG_CACHE:
        _PROG_CACHE[key] = build_program(NL, NS, dbg)
    return _PROG_CACHE[key]


def _common_maps(inp, layers):
    NL = len(layers)
    m = {}
    for k in ("w_in", "w_branch", "w_out", "b_cmp_wk", "b_cmp_wv", "c_w_ra", "c_w_ri"):
        m[k] = np.ascontiguousarray(np.asarray(inp[k], np.float32)[layers])
    m["par"] = np.stack([_params(inp, l) for l in layers], 0)
    m["lbl"] = np.ascontiguousarray(np.asarray(inp["lb_logits"], np.float32).reshape(4, 4, 128).transpose(2, 1, 0).reshape(128, 16))
    ls = np.zeros((128, NL, 4), np.float32)
    for i, l in enumerate(layers):
        ls[:, i, l] = 1.0
    m["lsel"] = ls.reshape(128, NL * 4)
    for k, v in _consts().items():
        m["c_" + k] = v
    return m


MODE = "fused"
PHASES = "0ADCBM"
POOLMAP = "dve"
NCORES = 8


def kernel(**inputs):
    inp = {k: np.asarray(v) for k, v in inputs.items()}
    x = np.asarray(inp["x"], np.float32)
    B = x.shape[0]
    NSC = B // NCORES
    xTh = np.ascontiguousarray(x.transpose(0, 2, 1))
    if MODE == "fused":
        nc = _get_prog(4, NSC)
        cm = _common_maps(inp, [0, 1, 2, 3])
        maps = [dict(cm, xT=xTh[c * NSC:(c + 1) * NSC]) for c in range(NCORES)]
        res = run_bass_kernel_spmd(nc, maps, core_ids=list(range(NCORES)))
        oTh = np.concatenate([r["outT"] for r in res.results], 0)
    elif MODE == "layer":
        nc = _get_prog(1, NSC)
        cur = xTh
        for l in range(4):
            cm = _common_maps(inp, [l])
            maps = [dict(cm, xT=cur[c * NSC:(c + 1) * NSC]) for c in range(NCORES)]
            res = run_bass_kernel_spmd(nc, maps, core_ids=list(range(NCORES)))
            cur = np.concatenate([r["outT"] for r in res.results], 0)
        oTh = cur
    else:
        nc = _get_prog(4, 1)
        cm = _common_maps(inp, [0, 1, 2, 3])
        outs = []
        for g in range(NSC):
            maps = [dict(cm, xT=xTh[c * NSC + g:c * NSC + g + 1]) for c in range(NCORES)]
            res = run_bass_kernel_spmd(nc, maps, core_ids=list(range(NCORES)))
            outs.append([r["outT"] for r in res.results])
        oTh = np.concatenate([outs[g][c] for c in range(NCORES) for g in range(NSC)], 0)
    return np.ascontiguousarray(oTh.transpose(0, 2, 1)).astype(np.float32)
```
